# Optimizing a Trainium2 kernel written in Bass

```python
import math
import jax, jax.numpy as jnp
from jax import lax
import numpy as np

D_MODEL = 2048
BATCH = 4
SEQ = 4096
DEPTH = 2

GRID_W = 64
CTX_LEN = 256
EPS = 1e-6
CONV_DIM = 1024
CONV_GROUPS = 16
FOURIER_DIM = 1024
FOURIER_GROUPS = 4
FOURIER_GROUP_DIM = FOURIER_DIM // FOURIER_GROUPS
CONV_WIDTH = 3
EVEN_IN_DIM = 3 * CONV_DIM + FOURIER_DIM
EVEN_OUT_DIM = CONV_DIM + FOURIER_DIM
DIFF_HEADS = 8
DIFF_HEAD_DIM = 128
DIFF_V_DIM = 2 * DIFF_HEAD_DIM
QK_DIM = DIFF_HEADS * 2 * DIFF_HEAD_DIM
V_DIM = DIFF_HEADS * DIFF_V_DIM
ROPE_THETA = 10000.0
ROPE_FREQS = DIFF_HEAD_DIM // 4
Q_BLOCK = 128
LAMBDA_STD = 0.1
N_GROUPS = 4
EXPERTS_PER_GROUP = 4
N_EXPERTS = N_GROUPS * EXPERTS_PER_GROUP
TOP_K = 2
EXPERT_FF = 512
MOE_BLOCK = 128
MOD_SCALE = 0.5

kernel_name = "hybrid_conv_fourier_diffattn_hmoe_dit"


def rmsnorm(x, w):
    xf = x.astype(jnp.float32)
    y = xf * lax.rsqrt(jnp.mean(xf * xf, axis=-1, keepdims=True) + EPS)
    return (y * w.astype(jnp.float32)).astype(x.dtype)


def modulation(cond, w_mod, b_mod):
    m = jax.nn.silu(cond) @ w_mod + b_mod
    return jnp.split(m[..., None, :], 6, axis=-1)


def short_conv3(u, w):
    up = jnp.pad(u, ((0, 0), (1, 1), (0, 0)))
    return up[:, :-2] * w[0] + up[:, 1:-1] * w[1] + up[:, 2:] * w[2]


def conv_fourier_mixer(u, w_in, conv_w, w_out):
    b, n, _ = u.shape
    p = u @ w_in
    b_gate, c_gate, v_in, f_in = jnp.split(p, [CONV_DIM, 2 * CONV_DIM, 3 * CONV_DIM], axis=-1)
    y_conv = b_gate * short_conv3(c_gate * v_in, conv_w)
    fg = f_in.reshape(b, n, FOURIER_GROUPS, FOURIER_GROUP_DIM).astype(jnp.float32)
    y_fourier = jnp.fft.fft2(fg, axes=(1, 3), norm="ortho").real
    y_fourier = y_fourier.reshape(b, n, FOURIER_DIM).astype(u.dtype)
    return jnp.concatenate([y_conv, y_fourier], axis=-1) @ w_out


def axial_rope_tables(n_tok):
    rows = n_tok // GRID_W
    row = jnp.repeat(jnp.arange(rows, dtype=jnp.float32), GRID_W)
    col = jnp.tile(jnp.arange(GRID_W, dtype=jnp.float32), rows)
    inv_freq = ROPE_THETA ** (-jnp.arange(ROPE_FREQS, dtype=jnp.float32) / ROPE_FREQS)
    ang = jnp.stack([row, col], axis=-1)[:, :, None] * inv_freq
    return jnp.cos(ang), jnp.sin(ang)


def apply_axial_rope(x, cos, sin):
    sh = x.shape
    xr = x.reshape(sh[:-1] + (2, 2, ROPE_FREQS)).astype(jnp.float32)
    x1, x2 = xr[..., 0, :], xr[..., 1, :]
    cc = cos[None, :, None, None]
    ss = sin[None, :, None, None]
    out = jnp.stack([x1 * cc - x2 * ss, x2 * cc + x1 * ss], axis=-2)
    return out.reshape(sh).astype(x.dtype)


def diff_attend(q, k, v, lam):
    s = jnp.einsum('bqhcd,bkhcd->bhcqk', q, k).astype(jnp.float32) * (DIFF_HEAD_DIM ** -0.5)
    p = jax.nn.softmax(s, axis=-1)
    a = p[:, :, 0] - lam * p[:, :, 1]
    return jnp.einsum('bhqk,bkhe->bqhe', a.astype(v.dtype), v)


def blockwise_diff_attend(q, k, v, lam):
    b, nq = q.shape[:2]
    nb = nq // Q_BLOCK
    qb = jnp.moveaxis(q.reshape((b, nb, Q_BLOCK) + q.shape[2:]), 1, 0)
    ob = lax.map(lambda qq: diff_attend(qq, k, v, lam), qb)
    return jnp.moveaxis(ob, 0, 1).reshape((b, nq) + ob.shape[3:])


def diff_attention_mixer(u_lat, u_ctx, layer_idx, cos, sin, w_qkv, q_norm, k_norm,
                         lam_q1, lam_k1, lam_q2, lam_k2, subln_w, w_out, need_ctx):
    f32 = jnp.float32
    lam_init = 0.8 - 0.6 * math.exp(-0.3 * layer_idx)
    lam = (jnp.exp(jnp.sum(lam_q1.astype(f32) * lam_k1.astype(f32)))
           - jnp.exp(jnp.sum(lam_q2.astype(f32) * lam_k2.astype(f32))) + lam_init)
    w_q, w_kv = w_qkv[:, :QK_DIM], w_qkv[:, QK_DIM:]

    def split_qk(t, gain):
        b, n, _ = t.shape
        return rmsnorm(t.reshape(b, n, DIFF_HEADS, 2, DIFF_HEAD_DIM), gain)

    def keys_values(u):
        b, n, _ = u.shape
        kv = u @ w_kv
        return split_qk(kv[..., :QK_DIM], k_norm), kv[..., QK_DIM:].reshape(b, n, DIFF_HEADS, DIFF_V_DIM)

    def merge(o):
        b, n = o.shape[:2]
        o = rmsnorm(o, subln_w) * (1.0 - lam_init)
        return o.reshape(b, n, V_DIM) @ w_out

    k_ctx, v_ctx = keys_values(u_ctx)
    k_lat, v_lat = keys_values(u_lat)
    k_lat = apply_axial_rope(k_lat, cos, sin)
    q_lat = apply_axial_rope(split_qk(u_lat @ w_q, q_norm), cos, sin)
    k_all = jnp.concatenate([k_ctx, k_lat], axis=1)
    v_all = jnp.concatenate([v_ctx, v_lat], axis=1)
    out_lat = merge(blockwise_diff_attend(q_lat, k_all, v_all, lam))
    if need_ctx:
        q_ctx = split_qk(u_ctx @ w_q, q_norm)
        out_ctx = merge(diff_attend(q_ctx, k_ctx, v_ctx, lam))
    else:
        out_ctx = None
    return out_lat, out_ctx


def hier_moe(h, w_rg, b_rg, w_re, b_re, w_gate, w_up, w_down):
    f32 = jnp.float32
    n, d = h.shape
    g_logits = (h @ w_rg).astype(f32) + b_rg.astype(f32)
    g_idx = jnp.argmax(g_logits, axis=-1)
    g_w = jnp.max(jax.nn.softmax(g_logits, axis=-1), axis=-1)
    e_logits = ((h @ w_re).astype(f32) + b_re.astype(f32)).reshape(n, N_GROUPS, EXPERTS_PER_GROUP)
    e_sel = e_logits[jnp.arange(n), g_idx]
    top_v, top_i = lax.top_k(e_sel, TOP_K)
    top_w = jax.nn.softmax(top_v, axis=-1) * g_w[:, None]
    expert = (g_idx[:, None] * EXPERTS_PER_GROUP + top_i).reshape(-1).astype(jnp.int32)
    token = jnp.repeat(jnp.arange(n, dtype=jnp.int32), TOP_K)
    weight = top_w.reshape(-1)
    n_assign = n * TOP_K
    n_blocks = -(-n_assign // MOE_BLOCK) + N_EXPERTS
    n_rows = n_blocks * MOE_BLOCK
    counts = jnp.zeros((N_EXPERTS,), jnp.int32).at[expert].add(1)
    blocks_per = (counts + MOE_BLOCK - 1) // MOE_BLOCK
    block_end = jnp.cumsum(blocks_per)
    row_start = (block_end - blocks_per) * MOE_BLOCK
    assign_start = jnp.cumsum(counts) - counts
    order = jnp.argsort(expert)
    e_sorted = expert[order]
    dest = row_start[e_sorted] + (jnp.arange(n_assign, dtype=jnp.int32) - assign_start[e_sorted])
    row_token = jnp.zeros((n_rows,), jnp.int32).at[dest].set(token[order])
    row_weight = jnp.zeros((n_rows,), f32).at[dest].set(weight[order])
    block_expert = jnp.minimum(jnp.searchsorted(block_end, jnp.arange(n_blocks, dtype=jnp.int32), side='right'),
                               N_EXPERTS - 1)

    def run_block(args):
        tok_b, w_b, e_b = args
        xb = h[tok_b]
        yb = (jax.nn.silu(xb @ w_gate[e_b]) * (xb @ w_up[e_b])) @ w_down[e_b]
        return yb * w_b[:, None].astype(yb.dtype)

    y_rows = lax.map(run_block, (row_token.reshape(n_blocks, MOE_BLOCK),
                                 row_weight.reshape(n_blocks, MOE_BLOCK), block_expert))
    return jnp.zeros_like(h).at[row_token].add(y_rows.reshape(n_rows, d))


def setup_inputs(seed: int = 0) -> dict:
    key = jax.random.key(seed)
    ks = jax.random.split(key, 32)
    D = D_MODEL
    n_even = (DEPTH + 1) // 2
    n_odd = DEPTH // 2
    f32 = jnp.float32

    def nrm(k, shape, std):
        return jax.random.normal(k, shape, f32) * std

    return {
        "x": nrm(ks[0], (BATCH, SEQ, D), 1.0),
        "c": nrm(ks[1], (BATCH, D), 1.0),
        "ctx": nrm(ks[2], (BATCH, CTX_LEN, D), 1.0),
        "c_ctx": nrm(ks[3], (D,), 1.0),
        "norm1_w": 1.0 + nrm(ks[4], (DEPTH, D), 0.02),
        "norm2_w": 1.0 + nrm(ks[5], (DEPTH, D), 0.02),
        "w_mod": nrm(ks[6], (DEPTH, D, 6 * D), MOD_SCALE * D ** -0.5),
        "b_mod": nrm(ks[7], (DEPTH, 6 * D), 0.02),
        "even_w_in": nrm(ks[8], (n_even, D, EVEN_IN_DIM), D ** -0.5),
        "even_conv_w": nrm(ks[9], (n_even, CONV_WIDTH, CONV_DIM), CONV_WIDTH ** -0.5),
        "even_w_out": nrm(ks[10], (n_even, EVEN_OUT_DIM, D), EVEN_OUT_DIM ** -0.5),
        "odd_w_qkv": nrm(ks[11], (n_odd, D, 2 * QK_DIM + V_DIM), D ** -0.5),
        "odd_q_norm": 1.0 + nrm(ks[12], (n_odd, DIFF_HEAD_DIM), 0.02),
        "odd_k_norm": 1.0 + nrm(ks[13], (n_odd, DIFF_HEAD_DIM), 0.02),
        "odd_lambda_q1": nrm(ks[14], (n_odd, DIFF_HEAD_DIM), LAMBDA_STD),
        "odd_lambda_k1": nrm(ks[15], (n_odd, DIFF_HEAD_DIM), LAMBDA_STD),
        "odd_lambda_q2": nrm(ks[16], (n_odd, DIFF_HEAD_DIM), LAMBDA_STD),
        "odd_lambda_k2": nrm(ks[17], (n_odd, DIFF_HEAD_DIM), LAMBDA_STD),
        "odd_subln_w": 1.0 + nrm(ks[18], (n_odd, DIFF_V_DIM), 0.02),
        "odd_w_out": nrm(ks[19], (n_odd, V_DIM, D), V_DIM ** -0.5),
        "moe_w_group": nrm(ks[20], (DEPTH, D, N_GROUPS), D ** -0.5),
        "moe_b_group": nrm(ks[21], (DEPTH, N_GROUPS), 0.01),
        "moe_w_expert": nrm(ks[22], (DEPTH, D, N_EXPERTS), D ** -0.5),
        "moe_b_expert": nrm(ks[23], (DEPTH, N_EXPERTS), 0.01),
        "moe_w_gate": nrm(ks[24], (DEPTH, N_EXPERTS, D, EXPERT_FF), D ** -0.5),
        "moe_w_up": nrm(ks[25], (DEPTH, N_EXPERTS, D, EXPERT_FF), D ** -0.5),
        "moe_w_down": nrm(ks[26], (DEPTH, N_EXPERTS, EXPERT_FF, D), EXPERT_FF ** -0.5),
    }


def reference(x, c, ctx, c_ctx, norm1_w, norm2_w, w_mod, b_mod,
              even_w_in, even_conv_w, even_w_out,
              odd_w_qkv, odd_q_norm, odd_k_norm, odd_lambda_q1, odd_lambda_k1,
              odd_lambda_q2, odd_lambda_k2, odd_subln_w, odd_w_out,
              moe_w_group, moe_b_group, moe_w_expert, moe_b_expert,
              moe_w_gate, moe_w_up, moe_w_down):
    bsz, n_lat, d = x.shape
    n_ctx = ctx.shape[1]
    cos, sin = axial_rope_tables(n_lat)
    h_lat, h_ctx = x, ctx
    for l in range(DEPTH):
        last = l == DEPTH - 1
        i = l // 2
        sh1, sc1, g1, sh2, sc2, g2 = modulation(c, w_mod[l], b_mod[l])
        csh1, csc1, cg1, csh2, csc2, cg2 = modulation(c_ctx, w_mod[l], b_mod[l])
        u_lat = rmsnorm(h_lat, norm1_w[l]) * (1.0 + sc1) + sh1
        u_ctx = rmsnorm(h_ctx, norm1_w[l]) * (1.0 + csc1) + csh1
        if l % 2 == 0:
            m_lat = conv_fourier_mixer(u_lat, even_w_in[i], even_conv_w[i], even_w_out[i])
            m_ctx = None if last else conv_fourier_mixer(u_ctx, even_w_in[i], even_conv_w[i], even_w_out[i])
        else:
            m_lat, m_ctx = diff_attention_mixer(
                u_lat, u_ctx, l, cos, sin, odd_w_qkv[i], odd_q_norm[i], odd_k_norm[i],
                odd_lambda_q1[i], odd_lambda_k1[i], odd_lambda_q2[i], odd_lambda_k2[i],
                odd_subln_w[i], odd_w_out[i], not last)
        h_lat = h_lat + g1 * m_lat
        v_lat = rmsnorm(h_lat, norm2_w[l]) * (1.0 + sc2) + sh2
        moe_args = (moe_w_group[l], moe_b_group[l], moe_w_expert[l], moe_b_expert[l],
                    moe_w_gate[l], moe_w_up[l], moe_w_down[l])
        if last:
            y_lat = hier_moe(v_lat.reshape(-1, d), *moe_args)
            h_lat = h_lat + g2 * y_lat.reshape(bsz, n_lat, d)
        else:
            h_ctx = h_ctx + cg1 * m_ctx
            v_ctx = rmsnorm(h_ctx, norm2_w[l]) * (1.0 + csc2) + csh2
            tokens = jnp.concatenate([v_lat.reshape(-1, d), v_ctx.reshape(-1, d)], axis=0)
            y = hier_moe(tokens, *moe_args)
            y_lat = y[:bsz * n_lat].reshape(bsz, n_lat, d)
            y_ctx = y[bsz * n_lat:].reshape(bsz, n_ctx, d)
            h_lat = h_lat + g2 * y_lat
            h_ctx = h_ctx + cg2 * y_ctx
    return h_lat
```

```python
import numpy as np
from contextlib import ExitStack
import concourse.bass as bass
import concourse.mybir as mybir
from concourse.bass_utils import run_bass_kernel_spmd

F32 = mybir.dt.float32
BF16 = mybir.dt.bfloat16
I32 = mybir.dt.int32
AF = mybir.ActivationFunctionType
ALU = mybir.AluOpType
AX = mybir.AxisListType

NDS = 24


class Prog:
    def __init__(self, nc, es):
        self.nc = nc
        self.es = es
        self.E = {"pe": nc.tensor, "act": nc.scalar, "dve": nc.vector, "pool": nc.gpsimd, "sp": nc.sync}
        self.csem = {e: es.enter_context(nc.semaphore(f"c_{e}")) for e in ["pe", "act", "dve", "pool"]}
        self.ccnt = {e: 0 for e in self.csem}
        self.dsems = {q: [es.enter_context(nc.semaphore(f"d_{q}_{i}")) for i in range(NDS)] for q in ["sp", "pool"]}
        self.dcnt = {q: [0] * NDS for q in ["sp", "pool"]}
        self.dnext = {q: 0 for q in ["sp", "pool"]}
        self.seen = {e: {} for e in self.E}
        self.lastw = {}
        self.readers = {}
        self.n_wait = 0
        self.xsem = es.enter_context(nc.semaphore("x_cc"))
        self.xcnt = 0

    def coll(self, fn, reads=(), writes=()):
        self._deps("pool", reads, writes)
        inst = fn(self.E["pool"])
        self.xcnt += 1
        inst.then_inc(self.xsem)
        self._commit((("x", 0), self.xcnt), reads, writes)

    def _wait(self, eng, tok):
        semkey, val = tok
        if semkey == ("c", "pe") and eng == "pe":
            return
        if self.seen[eng].get(semkey, 0) >= val:
            return
        sem = self.csem[semkey[1]] if semkey[0] == "c" else (self.xsem if semkey[0] == "x" else self.dsems[semkey[1]][semkey[2]])
        self.E[eng].wait_ge(sem, val)
        self.n_wait += 1
        self.seen[eng][semkey] = val

    def _deps(self, eng, reads, writes, is_dma=False):
        for k in reads:
            for sk, v in self.lastw.get(k, {}).items():
                self._wait(eng, (sk, v))
        for k in writes:
            for sk, v in self.lastw.get(k, {}).items():
                if is_dma and sk[0] == "d":
                    continue
                self._wait(eng, (sk, v))
            for sk, v in self.readers.get(k, {}).items():
                self._wait(eng, (sk, v))

    def _commit(self, tok, reads, writes, is_dma=False):
        for k in reads:
            r = self.readers.setdefault(k, {})
            if r.get(tok[0], 0) < tok[1]:
                r[tok[0]] = tok[1]
        for k in writes:
            w = self.lastw.setdefault(k, {})
            if w.get(tok[0], 0) < tok[1]:
                w[tok[0]] = tok[1]
            if not is_dma:
                self.readers[k] = {}

    def op(self, eng, fn, reads=(), writes=()):
        self._deps(eng, reads, writes)
        inst = fn(self.E[eng])
        self.ccnt[eng] += 1
        inst.then_inc(self.csem[eng], 1)
        self._commit((("c", eng), self.ccnt[eng]), reads, writes)

    def op_if(self, eng, pred, fn, reads=(), writes=()):
        self._deps(eng, reads, writes)
        e = self.E[eng]
        with e.If(pred):
            inst = fn(e)
            inst.then_inc(self.csem[eng], 1)
        with e.Else():
            e.sem_inc(self.csem[eng], 1)
        self.ccnt[eng] += 1
        self._commit((("c", eng), self.ccnt[eng]), reads, writes)

    def dma_if(self, q, pred, fn, reads=(), writes=()):
        self._deps(q, reads, writes, is_dma=True)
        i = self.dnext[q]
        self.dnext[q] = (i + 1) % NDS
        if self.dcnt[q][i] > 0:
            self._wait(q, (("d", q, i), self.dcnt[q][i]))
        e = self.E[q]
        with e.If(pred):
            inst = fn(e)
            inst.then_inc(self.dsems[q][i], 16)
        with e.Else():
            e.sem_inc(self.dsems[q][i], 16)
        self.dcnt[q][i] += 16
        self._commit((("d", q, i), self.dcnt[q][i]), reads, writes, is_dma=True)

    def dma(self, q, fn, reads=(), writes=()):
        self._deps(q, reads, writes, is_dma=True)
        i = self.dnext[q]
        self.dnext[q] = (i + 1) % NDS
        if self.dcnt[q][i] > 0:
            self._wait(q, (("d", q, i), self.dcnt[q][i]))
        inst = fn(self.E[q])
        self.dcnt[q][i] += 16
        inst.then_inc(self.dsems[q][i], 16)
        self._commit((("d", q, i), self.dcnt[q][i]), reads, writes, is_dma=True)

    def finish(self, eng="sp"):
        for q in self.dsems:
            for i in range(NDS):
                if self.dcnt[q][i] > 0:
                    self._wait(eng, (("d", q, i), self.dcnt[q][i]))
        for e, c in self.ccnt.items():
            if c > 0:
                self._wait(eng, (("c", e), c))
        if self.xcnt > 0:
            self._wait(eng, (("x", 0), self.xcnt))


D = 2048
KD = 16
NLAT = 2048
NCTX = 256
NHALO = 128
NTOK0 = NLAT + NCTX
CAP = 1024
HCAP = 512
EPS = 1e-6
LAM_INIT1 = 0.8 - 0.6 * float(np.exp(-0.3 * 1))


class Ctx:
    pass


_UNIQ = [0]


def _T(nc, es, name, shape, dt):
    _UNIQ[0] += 1
    return es.enter_context(nc.sbuf_tensor(f"{name}_{_UNIQ[0]}", shape, dt))


def _PS(nc, es, name, shape, dt=F32):
    _UNIQ[0] += 1
    return es.enter_context(nc.psum_tensor(f"{name}_{_UNIQ[0]}", shape, dt))


def barrier(P):
    for e in ["pe", "act", "dve", "pool", "sp"]:
        P.finish(e)


def load_bcast(P, nc, es, name, src_row_ap, n=D, q="sp"):
    t = _T(nc, es, name, [128, n], F32)
    P.dma(q, lambda e: e.dma_start(out=t[:], in_=src_row_ap.partition_broadcast(128)), writes=[name])
    return t


def norm_block(P, K, src_rows, A, Bt, xf, ub, sq, st, uT=None, uT_off=0, psT=None, tag=""):
    A_t, A_k = A
    B_t, B_k = Bt
    xk, uk, sk = xf[1], ub[1], st[1]
    P.dma("sp", lambda e: e.dma_start(out=xf[0][:], in_=src_rows), writes=[xk])
    P.op("act", lambda e: e.activation(out=sq[0][:], in_=xf[0][:], func=AF.Square, accum_out=st[0][:, 0:1]),
         reads=[xk], writes=[sq[1], sk])
    P.op("act", lambda e: e.activation(out=st[0][:, 1:2], in_=st[0][:, 0:1], func=AF.Sqrt, scale=1.0 / D, bias=K.epsb[:, 0:1]),
         reads=[sk], writes=[sk])
    P.op("dve", lambda e: e.reciprocal(out=st[0][:, 2:3], in_=st[0][:, 1:2]), reads=[sk], writes=[sk])
    P.op("dve", lambda e: e.scalar_tensor_tensor(out=xf[0][:], in0=xf[0][:], scalar=st[0][:, 2:3], in1=A_t[:],
                                                  op0=ALU.mult, op1=ALU.mult), reads=[xk, sk, A_k], writes=[xk])
    P.op("pool", lambda e: e.tensor_tensor(out=ub[0][:], in0=xf[0][:], in1=B_t[:], op=ALU.add), reads=[xk, B_k], writes=[uk])
    if uT is not None:
        for half in range(2):
            pk = psT[half][1]

            def tr(e, half=half):
                for j in range(8):
                    k = half * 8 + j
                    i = e.transpose(out=psT[half][0][:, j, :], in_=ub[0][:, k * 128:(k + 1) * 128], identity=K.ident[:])
                return i
            P.op("pe", tr, reads=[uk], writes=[pk])
            eng = "act" if half == 0 else "dve"
            if eng == "act":
                P.op("act", lambda e, half=half: e.copy(out=uT[0][:, half * 8:(half + 1) * 8, uT_off:uT_off + 128], in_=psT[half][0][:]),
                     reads=[pk], writes=[uT[1]])
            else:
                P.op("dve", lambda e, half=half: e.tensor_copy(out=uT[0][:, half * 8:(half + 1) * 8, uT_off:uT_off + 128], in_=psT[half][0][:]),
                     reads=[pk], writes=[uT[1]])


def phase_mod(P, nc, K, io):
    es = ExitStack()
    K.mod_es = es
    cT = _T(nc, es, "m_cT", [128, KD, 2], F32)
    sT = _T(nc, es, "m_sT", [128, KD, 2], BF16)
    P.dma("sp", lambda e: e.dma_start(out=cT[:], in_=io.condT), writes=["m_cT"])
    P.op("act", lambda e: e.activation(out=sT[:], in_=cT[:], func=AF.Silu), reads=["m_cT"], writes=["m_sT"])
    wm = [_T(nc, es, f"m_w{i}", [128, KD, 512], BF16) for i in range(2)]
    bm = [_T(nc, es, f"m_b{i}", [2, 512], F32) for i in range(2)]
    ob = [_T(nc, es, f"m_o{i}", [2, 512], F32) for i in range(2)]
    ps = [_PS(nc, es, f"m_ps{i}", [2, 512]) for i in range(2)]
    cnt = [0]

    def work(l, cb):
        it = cnt[0]
        cnt[0] += 1
        wl = io.w_mod[l].rearrange("(k p) n -> p k n", p=128)
        w = wm[it % 2]
        wk = f"m_w{it % 2}"
        j = it % 2
        P.dma("pool", lambda e: e.dma_start(out=w[:], in_=wl[:, :, cb * 512:(cb + 1) * 512]), writes=[wk])
        P.dma("sp", lambda e: e.dma_start(out=bm[j][:], in_=io.b_mod[l:l + 1, cb * 512:(cb + 1) * 512].partition_broadcast(2)),
              writes=[f"m_b{j}"])

        def mm(e):
            for k in range(KD):
                i = e.matmul(ps[j][:], lhsT=sT[:, k, :], rhs=w[:, k, :], start=(k == 0), stop=(k == KD - 1))
            return i
        P.op("pe", mm, reads=[wk, "m_sT"], writes=[f"m_ps{j}"])
        P.op("dve", lambda e: e.tensor_tensor(out=ob[j][:], in0=ps[j][:], in1=bm[j][:], op=ALU.add),
             reads=[f"m_ps{j}", f"m_b{j}"], writes=[f"m_o{j}"])
        P.dma("sp", lambda e: e.dma_start(out=io.modv[l, :, cb * 512:(cb + 1) * 512], in_=ob[j][:]),
              reads=[f"m_o{j}"], writes=["modv"])
    for cb in range(24):
        work(0, cb)
    K.modq = [(lambda cb=cb: work(1, cb)) for cb in range(24)]
    barrier(P)


def drain_mod(P, K, n):
    q = getattr(K, "modq", None)
    while q and n > 0:
        q.pop(0)()
        n -= 1


def make_AB(P, nc, es, K, io, l, which, cond, pfx):
    base = which * 3 * D
    A = load_bcast(P, nc, es, pfx + "A", io.modv[l, cond:cond + 1, base + D:base + 2 * D])
    Bt = load_bcast(P, nc, es, pfx + "B", io.modv[l, cond:cond + 1, base:base + D])
    nw = load_bcast(P, nc, es, pfx + "nw", (io.norm1_w if which == 0 else io.norm2_w)[l:l + 1, :])
    P.op("dve", lambda e: e.scalar_tensor_tensor(out=A[:], in0=A[:], scalar=1.0, in1=nw[:], op0=ALU.add, op1=ALU.mult),
         reads=[pfx + "A", pfx + "nw"], writes=[pfx + "A"])
    return (A, pfx + "A"), (Bt, pfx + "B")


def phase_l0a(P, nc, K, io):
    NT = NLAT + NCTX + NHALO
    with ExitStack() as es:
        uT = (_T(nc, es, "a_uT", [128, KD, NT], BF16), "a_uT")
        with ExitStack() as es2:
            Al, Bl = make_AB(P, nc, es2, K, io, 0, 0, 0, "a_l")
            Ac, Bc = make_AB(P, nc, es2, K, io, 0, 0, 1, "a_c")
            xf = [(_T(nc, es2, f"a_xf{i}", [128, D], F32), f"a_xf{i}") for i in range(2)]
            ub = [(_T(nc, es2, f"a_ub{i}", [128, D], BF16), f"a_ub{i}") for i in range(2)]
            sq = (_T(nc, es2, "a_sq", [128, D], BF16), "a_sq")
            st = [(_T(nc, es2, f"a_st{i}", [128, 4], F32), f"a_st{i}") for i in range(2)]
            psT = [(_PS(nc, es2, f"a_pT{i}", [128, 8, 128], BF16), f"a_pT{i}") for i in range(2)]
            nb = NT // 128
            for b in range(nb):
                if b < 16:
                    src, A, Bt = io.x[b * 128:(b + 1) * 128, :], Al, Bl
                elif b < 18:
                    src, A, Bt = io.ctx[(b - 16) * 128:(b - 15) * 128, :], Ac, Bc
                else:
                    src, A, Bt = io.xh[:, :], Al, Bl
                norm_block(P, K, src, A, Bt, xf[b % 2], ub[b % 2], sq, st[b % 2], uT=uT, uT_off=b * 128, psT=psT)
            barrier(P)
        win = io.even_w_in.rearrange("(k p) n -> p k n", p=128)
        wt = [(_T(nc, es, f"a_w{i}", [128, KD, 128], BF16), f"a_w{i}") for i in range(4)]
        pp = [(_PS(nc, es, f"a_pp{i}", [128, 512]), f"a_pp{i}") for i in range(6)]
        cvl = _T(nc, es, "a_cvl", [128, NLAT + 2], F32)
        cvc = _T(nc, es, "a_cvc", [128, NCTX + 2], F32)
        bg = _T(nc, es, "a_bg", [128, NLAT + NCTX], F32)
        cs = [(_T(nc, es, f"a_cs{i}", [128, 512], F32), f"a_cs{i}") for i in range(2)]
        t1 = _T(nc, es, "a_t1", [128, NLAT + NCTX], F32)
        yb = _T(nc, es, "a_yb", [128, NLAT + NCTX], BF16)
        cw = _T(nc, es, "a_cw", [128, 8, 3], F32)
        P.dma("sp", lambda e: e.dma_start(out=cw[:], in_=io.conv_wT), writes=["a_cw"])
        P.op("pool", lambda e: e.memset(cvl[:], 0.0), writes=["a_cvl"])
        P.op("pool", lambda e: e.memset(cvc[:], 0.0), writes=["a_cvc"])
        tblocks = [(0, 512), (512, 512), (1024, 512), (1536, 512), (2048, 256), (2304, 128)]
        wi = 0
        ppi = 0
        fT = _T(nc, es, "a_fT", [128, 2, NLAT + NCTX], BF16)
        csd = _T(nc, es, "a_csd", [128, 2, 512], BF16)
        P.dma("sp", lambda e: e.dma_start(out=csd[:], in_=io.cs256), writes=["a_csd"])
        abt = [(_T(nc, es, f"a_ab{i}", [128, 512], BF16), f"a_ab{i}") for i in range(2)]
        for g in range(4):
            for hh in range(2):
                w, wk = wt[wi % 4]
                wi += 1
                col = 3072 + g * 256 + hh * 128
                P.dma("pool", lambda e, w=w, col=col: e.dma_start(out=w[:], in_=win[:, :, col:col + 128]), writes=[wk])
                for ti, (t0, tn) in enumerate(tblocks[:5]):
                    p_, pk = pp[ppi % 6]
                    ppi += 1

                    def mm(e, p_=p_, w=w):
                        for k in range(KD):
                            i = e.matmul(p_[:, 0:tn], lhsT=w[:, k, :], rhs=uT[0][:, k, t0:t0 + tn], start=(k == 0), stop=(k == KD - 1))
                        return i
                    P.op("pe", mm, reads=[wk, "a_uT"], writes=[pk])
                    P.op("act", lambda e: e.copy(out=fT[:, hh, t0:t0 + tn], in_=p_[:, 0:tn]), reads=[pk], writes=["a_fT"])
            for b in range(18):
                p_, pk = pp[ppi % 6]
                ppi += 1

                def mm2(e, p_=p_):
                    for hh in range(2):
                        i = e.matmul(p_[:], lhsT=fT[:, hh, b * 128:(b + 1) * 128], rhs=csd[:, hh, :], start=(hh == 0), stop=(hh == 1))
                    return i
                P.op("pe", mm2, reads=["a_fT", "a_csd"], writes=[pk])
                a_, ak = abt[b % 2]
                if b % 2 == 0:
                    P.op("act", lambda e: e.copy(out=a_[:], in_=p_[:]), reads=[pk], writes=[ak])
                else:
                    P.op("dve", lambda e: e.tensor_copy(out=a_[:], in_=p_[:]), reads=[pk], writes=[ak])
                if b < 16:
                    P.dma("sp", lambda e: e.dma_start(out=io.ab_own[g // 2, b * 128:(b + 1) * 128, (g % 2) * 512:(g % 2 + 1) * 512], in_=a_[:]),
                          reads=[ak], writes=["ab_own"])
                else:
                    P.dma("sp", lambda e: e.dma_start(out=io.ab_ctx[(b - 16) * 128:(b - 15) * 128, g * 512:(g + 1) * 512], in_=a_[:]),
                          reads=[ak], writes=["ab_ctx"])
        pend = _gather_thunks(P, io.ab_own.rearrange("g t n -> (g t) n"), io.ab4, 4096, 512, "ab_own", "ab4") if io.mode == "ALL" else []
        for c in range(8):
            ws = []
            for part in range(3):
                w, wk = wt[wi % 4]
                wi += 1
                col = part * 1024 + c * 128
                P.dma("pool", lambda e, w=w, col=col: e.dma_start(out=w[:], in_=win[:, :, col:col + 128]), writes=[wk])
                ws.append((w, wk))
            if pend:
                pend.pop(0)()
            drain_mod(P, K, 3)
            for ti, (t0, tn) in enumerate(tblocks):
                pb = []
                for part in range(3):
                    p_, pk = pp[ppi % 6]
                    ppi += 1
                    w, wk = ws[part]

                    def mm(e, p_=p_, w=w):
                        for k in range(KD):
                            i = e.matmul(p_[:, 0:tn], lhsT=w[:, k, :], rhs=uT[0][:, k, t0:t0 + tn], start=(k == 0), stop=(k == KD - 1))
                        return i
                    P.op("pe", mm, reads=[wk, "a_uT"], writes=[pk])
                    pb.append((p_, pk))
                c_, ck = cs[ti % 2]
                P.op("act", lambda e: e.copy(out=c_[:, 0:tn], in_=pb[1][0][:, 0:tn]), reads=[pb[1][1]], writes=[ck])
                if ti < 4:
                    dst = cvl[:, 1 + t0:1 + t0 + tn]
                    dk = "a_cvl"
                elif ti == 4:
                    dst = cvc[:, 1:1 + tn]
                    dk = "a_cvc"
                else:
                    dst = None
                if dst is not None:
                    P.op("dve", lambda e: e.tensor_tensor(out=dst, in0=c_[:, 0:tn], in1=pb[2][0][:, 0:tn], op=ALU.mult),
                         reads=[ck, pb[2][1]], writes=[dk])
                    P.op("act", lambda e: e.copy(out=bg[:, t0:t0 + tn], in_=pb[0][0][:, 0:tn]), reads=[pb[0][1]], writes=["a_bg"])
                else:
                    P.op("dve", lambda e: e.tensor_tensor(out=c_[:, 0:1], in0=c_[:, 0:1], in1=pb[2][0][:, 0:1], op=ALU.mult),
                         reads=[ck, pb[2][1]], writes=[ck])
                    P.op("dve", lambda e: e.tensor_scalar(out=cvl[:, 0:1], in0=c_[:, 0:1], scalar1=K.flags[:, 0:1], scalar2=None, op0=ALU.mult),
                         reads=[ck], writes=["a_cvl"])
                    P.op("dve", lambda e: e.tensor_scalar(out=cvl[:, NLAT + 1:NLAT + 2], in0=c_[:, 0:1], scalar1=K.flags[:, 1:2], scalar2=None, op0=ALU.mult),
                         reads=[ck], writes=["a_cvl"])
            for (cv, cvk, o0, n) in [(cvl, "a_cvl", 0, NLAT), (cvc, "a_cvc", NLAT, NCTX)]:
                tt = t1[:, o0:o0 + n]
                P.op("dve", lambda e: e.tensor_scalar(out=tt, in0=cv[:, 1:n + 1], scalar1=cw[:, c, 1:2], scalar2=None, op0=ALU.mult),
                     reads=[cvk, "a_cw"], writes=["a_t1"])
                P.op("dve", lambda e: e.scalar_tensor_tensor(out=tt, in0=cv[:, 0:n], scalar=cw[:, c, 0:1], in1=tt, op0=ALU.mult, op1=ALU.add),
                     reads=[cvk, "a_t1"], writes=["a_t1"])
                P.op("dve", lambda e: e.scalar_tensor_tensor(out=tt, in0=cv[:, 2:n + 2], scalar=cw[:, c, 2:3], in1=tt, op0=ALU.mult, op1=ALU.add),
                     reads=[cvk, "a_t1"], writes=["a_t1"])
            P.op("pool", lambda e: e.tensor_tensor(out=yb[:], in0=t1[:], in1=bg[:], op=ALU.mult), reads=["a_t1", "a_bg"], writes=["a_yb"])
            P.dma("sp", lambda e: e.dma_start(out=io.ycT[c], in_=yb[:]), reads=["a_yb"], writes=["ycT"])
    drain_mod(P, K, 100)
    barrier(P)
    if getattr(K, "mod_es", None) is not None:
        K.mod_es.close()
        K.mod_es = None


PHASES_OF = {"A": ["mod", "l0a"], "B": ["l0b", "op0", "moe0", "l1a"], "C": ["attn", "op1", "moe1"],
             "ALL": ["mod", "l0a", "xab", "l0b", "op0", "moe0", "l1a", "xkv", "attn", "op1", "moe1"]}
LAUNCH_OF = {"mod": "A", "l0a": "A", "l0b": "B", "op0": "B", "moe0": "B", "l1a": "B", "attn": "C", "op1": "C", "moe1": "C"}


def declare(nc, mode):
    io = Ctx()
    io.ext_in, io.ext_out = [], []

    def inp(name, shape, dt, launches):
        if mode != "ALL" and mode not in launches:
            return None
        io.ext_in.append(name)
        return nc.dram_tensor(name, shape, dt, kind="ExternalInput").ap()

    def mid(name, shape, dt, prod, cons, scratch=""):
        if mode == "ALL" or mode in scratch:
            return nc.dram_tensor(name, shape, dt, kind="Internal").ap()
        if mode == prod:
            io.ext_out.append(name)
            return nc.dram_tensor(name, shape, dt, kind="ExternalOutput").ap()
        if mode in cons:
            io.ext_in.append(name)
            return nc.dram_tensor(name, shape, dt, kind="ExternalInput").ap()
        return None

    io.x = inp("x", [NLAT, D], F32, "AB")
    io.ctx = inp("ctx", [NCTX, D], F32, "AB")
    io.xh = inp("xh", [128, D], F32, "A")
    io.condT = inp("condT", [128, KD, 2], F32, "A")
    io.flags = inp("flags", [128, 2], F32, "A")
    io.w_mod = inp("w_mod", [2, D, 6 * D], F32, "A")
    io.b_mod = inp("b_mod", [2, 6 * D], F32, "A")
    io.norm1_w = inp("norm1_w", [2, D], F32, "AB")
    io.norm2_w = inp("norm2_w", [2, D], F32, "BC")
    io.even_w_in = inp("even_w_in", [D, 4096], F32, "A")
    io.conv_wT = inp("conv_wT", [128, 8, 3], F32, "A")
    io.even_w_out = inp("even_w_out", [D, D], F32, "B")
    io.odd_w_qkv = inp("odd_w_qkv", [D, 6144], F32, "B")
    io.qk_norm = inp("qk_norm", [128, 2], F32, "B")
    io.lam_vecs = inp("lam_vecs", [1, 4, 128], F32, "C")
    io.subln_wT = inp("subln_wT", [128, 2], F32, "C")
    io.odd_w_out = inp("odd_w_out", [D, D], F32, "C")
    io.moe_wr = [inp(f"moe_wr{l}", [D, 20], F32, "BC"[l]) for l in range(2)]
    io.moe_br = inp("moe_br", [2, 20], F32, "BC")
    io.moe_w_gate = [inp(f"moe_w_gate{l}", [16, D, 512], F32, "BC"[l]) for l in range(2)]
    io.moe_w_up = [inp(f"moe_w_up{l}", [16, D, 512], F32, "BC"[l]) for l in range(2)]
    io.moe_w_down = [inp(f"moe_w_down{l}", [16, 512, D], F32, "BC"[l]) for l in range(2)]
    io.ident = inp("ident", [128, 128], BF16, "ABC")
    io.cs256 = inp("cs256", [128, 2, 512], BF16, "A")
    io.dft_c = inp("dft_c", [4096, NLAT], BF16, "B")
    io.dft_s = inp("dft_s", [4096, NLAT], BF16, "B")
    io.dft256 = inp("dft256", [128, 2, 2, 256], BF16, "B")
    io.rope_cs = inp("rope_cs", [2, 128, NLAT], F32, "B")
    io.rotT = inp("rotT", [128, 128], BF16, "B")
    io.tri = inp("tri", [128, 128], BF16, "BC")
    io.modv = mid("modv", [2, 2, 6 * D], F32, "A", "BC")
    io.ycT = mid("ycT", [8, 128, NLAT + NCTX], BF16, "A", "B")
    io.ab_own = mid("ab_own", [2, NLAT, 1024], BF16, "A", "")
    io.ab_ctx = mid("ab_ctx", [NCTX, D], BF16, "A", "B")
    io.ab_full = mid("ab_full", [2, 2 * NLAT, 1024], BF16, "X", "B") if mode != "ALL" else None
    io.mode = mode
    if mode == "ALL":
        io.ab4 = nc.dram_tensor("ab4", [4 * 2 * NLAT, 1024], BF16, kind="Internal").ap()
        io.k4 = nc.dram_tensor("k4", [4 * 16 * 128, NLAT], BF16, kind="Internal").ap()
        io.v4 = nc.dram_tensor("v4", [4 * 4 * NLAT, 512], BF16, kind="Internal").ap()
    io.idx_ab = inp("idx_ab", [128, 64], I32, "")
    io.idx_k = inp("idx_k", [128, 32], I32, "")
    io.idx_v = inp("idx_v", [128, 128], I32, "")
    io.yfT = mid("yfT", [8, 128, NLAT + NCTX], BF16, "-", "", "B")
    io.h_lat = mid("h_lat", [NLAT, D], F32, "B", "C")
    io.h_ctx = mid("h_ctx", [NCTX, D], F32, "-", "", "B")
    io.h_mid = mid("h_mid", [NLAT, D], F32, "-", "", "BC")
    io.xg = mid("xg", [16 * CAP, D], BF16, "-", "", "BC")
    io.yg = mid("yg", [16 * CAP, D], BF16, "-", "", "BC")
    io.qT = mid("qT", [16, 128, NLAT], BF16, "B", "C")
    io.kT_own = mid("kT_own", [16, 128, NLAT], BF16, "B", "")
    io.kT_ctx = mid("kT_ctx", [16, 128, NCTX], BF16, "B", "C")
    io.v_own = mid("v_own", [4, NLAT, 512], BF16, "B", "")
    io.v_ctx = mid("v_ctx", [NCTX, D], BF16, "B", "C")
    io.kT_full = mid("kT_full", [16, 128, 2 * NLAT], BF16, "X", "C") if mode != "ALL" else None
    io.v_full = mid("v_full", [4, 2 * NLAT, 512], BF16, "X", "C") if mode != "ALL" else None
    io.oT = mid("oT", [16, 128, NLAT], BF16, "-", "", "C")
    io.out = mid("out", [NLAT, D], F32, "C", "") if mode != "ALL" else None
    io.dbg = None
    if mode == "ALL":
        io.ext_out.append("dbg")
        io.dbg = nc.dram_tensor("dbg", [2, 128, 16], F32, kind="ExternalOutput").ap()
        io.ext_out.append("out")
        io.out = nc.dram_tensor("out", [NLAT, D], F32, kind="ExternalOutput").ap()
    return io


def build(mode, phases=None):
    nc = bass.Bass("TRN2", target_bir_lowering=False)
    io = declare(nc, mode)
    for nm in ["xg", "yg", "yfT", "oT", "h_ctx", "ab_own", "kT_own", "v_own"]:
        if getattr(io, nm) is None and mode in "BC":
            pass
    es = ExitStack()
    with es:
        P = Prog(nc, es)
        K = Ctx()
        K.ident = _T(nc, es, "k_ident", [128, 128], BF16)
        K.epsb = _T(nc, es, "k_eps", [128, 1], F32)
        K.ones = _T(nc, es, "k_ones", [128, 128], BF16)
        K.flags = _T(nc, es, "k_flags", [128, 2], F32)
        P.dma("sp", lambda e: e.dma_start(out=K.ident[:], in_=io.ident), writes=["k_ident"])
        P.op("pool", lambda e: e.memset(K.epsb[:], EPS), writes=["k_eps"])
        P.op("pool", lambda e: e.memset(K.ones[:], 1.0), writes=["k_ones"])
        if io.flags is not None:
            P.dma("sp", lambda e: e.dma_start(out=K.flags[:], in_=io.flags), writes=["k_flags"])
        K.breg = nc.gpsimd.to_reg(16 * CAP - 1)
        K.breg_big = nc.gpsimd.to_reg(4 * 16 * 128 * 16 - 1)
        barrier(P)
        for ph in (phases or PHASES_OF[mode]):
            PHASE_FN[ph](P, nc, K, io)
            barrier(P)

        print(f"[build {mode}] waits={P.n_wait} counts={P.ccnt}")
    return nc, io


PHASE_FN = {"mod": phase_mod, "l0a": phase_l0a}


def phase_l0b(P, nc, K, io):
    with ExitStack() as es:
        abps = [_T(nc, es, f"b_abp{i}", [128, 32, 1024], BF16) for i in range(2)]
        dc = [(_T(nc, es, f"b_dc{i}", [128, 8, 512], BF16), f"b_dc{i}") for i in range(2)]
        ds = [(_T(nc, es, f"b_ds{i}", [128, 8, 512], BF16), f"b_ds{i}") for i in range(2)]
        acc = [(_PS(nc, es, f"b_acc{i}", [128, 512]), f"b_acc{i}") for i in range(8)]
        yo = [(_T(nc, es, f"b_yo{i}", [128, 512], BF16), f"b_yo{i}") for i in range(2)]
        if io.mode == "ALL":
            ixab = _T(nc, es, "b_ixab", [128, 64], I32)
            P.dma("sp", lambda e: e.dma_start(out=ixab[:], in_=io.idx_ab), writes=["b_ixab"])
        dcv = io.dft_c.rearrange("(c p) n -> p c n", p=128)
        dsv = io.dft_s.rearrange("(c p) n -> p c n", p=128)
        di = 0
        yi = 0
        ai = 0
        for gp in range(2):
            abp = abps[gp]
            if io.mode == "ALL" and gp == 0:
              for gq in range(2):
                for c in range(32):
                    P.dma("pool", lambda e: e.indirect_dma_start(out=abps[gq][:, c, :], out_offset=None, in_=io.ab4,
                                                                  in_offset=bass.IndirectOffsetOnAxis(ap=ixab[:, gq * 32 + c:gq * 32 + c + 1], axis=0),
                                                                  bounds_check=K.breg_big, oob_is_err=False), reads=[f"ab4_{q_}" for q_ in range(8)] + ["b_ixab"], writes=[f"b_abp{gq}"])
            elif io.mode != "ALL":
                abv = io.ab_full[gp].rearrange("(c p) n -> p c n", p=128)
                for q4 in range(4):
                    P.dma("sp", lambda e: e.dma_start(out=abp[:, q4 * 8:(q4 + 1) * 8, :], in_=abv[:, q4 * 8:(q4 + 1) * 8, :]), writes=[f"b_abp{gp}"])
            for nb in range(4):
                banks = [acc[(ai + a) % 8] for a in range(4)]
                ai += 4
                for tq in range(4):
                    c_, ck = dc[di % 2]
                    s_, sk = ds[di % 2]
                    di += 1
                    P.dma("sp", lambda e: e.dma_start(out=c_[:], in_=dcv[:, tq * 8:(tq + 1) * 8, nb * 512:(nb + 1) * 512]), writes=[ck])
                    P.dma("sp", lambda e: e.dma_start(out=s_[:], in_=dsv[:, tq * 8:(tq + 1) * 8, nb * 512:(nb + 1) * 512]), writes=[sk])

                    def mm(e):
                        for tc in range(8):
                            t = tq * 8 + tc
                            for a in range(4):
                                gl, hh = a // 2, a % 2
                                first = (tq == 0 and tc == 0)
                                last = (tq == 3 and tc == 7)
                                e.matmul(banks[a][0][:], lhsT=abp[:, t, gl * 512 + hh * 128:gl * 512 + hh * 128 + 128], rhs=c_[:, tc, :],
                                         start=first, stop=False)
                                i = e.matmul(banks[a][0][:], lhsT=abp[:, t, gl * 512 + 256 + hh * 128:gl * 512 + 256 + hh * 128 + 128], rhs=s_[:, tc, :],
                                             start=False, stop=last)
                        return i
                    P.op("pe", mm, reads=[f"b_abp{gp}", ck, sk], writes=[b[1] for b in banks])
                for a in range(4):
                    y_, yk = yo[yi % 2]
                    yi += 1
                    if a % 2 == 0:
                        P.op("act", lambda e: e.copy(out=y_[:], in_=banks[a][0][:]), reads=[banks[a][1]], writes=[yk])
                    else:
                        P.op("dve", lambda e: e.tensor_copy(out=y_[:], in_=banks[a][0][:]), reads=[banks[a][1]], writes=[yk])
                    ch = (gp * 2 + a // 2) * 2 + a % 2
                    P.dma("sp", lambda e: e.dma_start(out=io.yfT[ch, :, nb * 512:(nb + 1) * 512], in_=y_[:]), reads=[yk], writes=["yfT"])
        barrier(P)
        abc = _T(nc, es, "b_abc", [128, 2, D], BF16)
        d2 = _T(nc, es, "b_d2", [128, 2, 2, 256], BF16)
        P.dma("sp", lambda e: e.dma_start(out=abc[:], in_=io.ab_ctx.rearrange("(c p) n -> p c n", p=128)), writes=["b_abc"])
        P.dma("sp", lambda e: e.dma_start(out=d2[:], in_=io.dft256), writes=["b_d2"])
        for ch in range(8):
            g, hh = ch // 2, ch % 2
            bk, bkk = acc[ch % 8]

            def mmc(e):
                n = 0
                for t in range(2):
                    for part in range(2):
                        i = e.matmul(bk[:, 0:256], lhsT=abc[:, t, g * 512 + part * 256 + hh * 128:g * 512 + part * 256 + hh * 128 + 128],
                                     rhs=d2[:, t, part, :], start=(n == 0), stop=(n == 3))
                        n += 1
                return i
            P.op("pe", mmc, reads=["b_abc", "b_d2"], writes=[bkk])
            y_, yk = yo[ch % 2]
            P.op("act", lambda e: e.copy(out=y_[:, 0:256], in_=bk[:, 0:256]), reads=[bkk], writes=[yk])
            P.dma("sp", lambda e: e.dma_start(out=io.yfT[ch, :, NLAT:NLAT + NCTX], in_=y_[:, 0:256]), reads=[yk], writes=["yfT"])
    barrier(P)


def phase_op(P, nc, K, io, l, srcs, w_dram, segs):
    with ExitStack() as es:
        wo = _T(nc, es, "o_w", [128, KD, D], BF16)
        wv = w_dram.rearrange("(k p) n -> p k n", p=128)
        for q4 in range(4):
            P.dma("pool", lambda e: e.dma_start(out=wo[:, q4 * 4:(q4 + 1) * 4, :], in_=wv[:, q4 * 4:(q4 + 1) * 4, :]), writes=["o_w"])
        Gs = {}
        for cond in sorted(set(s[2] for s in segs)):
            Gs[cond] = load_bcast(P, nc, es, f"o_G{cond}", io.modv[l, cond:cond + 1, 2 * D:3 * D])
        yt = [(_T(nc, es, f"o_yt{i}", [128, KD, 512], BF16), f"o_yt{i}") for i in range(2)]
        hx = [(_T(nc, es, f"o_hx{i}", [128, D], F32), f"o_hx{i}") for i in range(2)]
        mg = [(_T(nc, es, f"o_mg{i}", [128, D], F32), f"o_mg{i}") for i in range(2)]
        ps = [(_PS(nc, es, f"o_ps{i}", [128, 512]), f"o_ps{i}") for i in range(8)]
        si = 0
        bi = 0
        pi = 0
        for (tok0, ntok, cond, h_src, h_dst) in segs:
            for s0 in range(0, ntok, 512):
                sn = min(512, ntok - s0)
                y_, yk = yt[si % 2]
                si += 1
                for half, src in enumerate(srcs):
                    P.dma("sp", lambda e: e.dma_start(out=y_[:, half * 8:(half + 1) * 8, 0:sn],
                                                      in_=src[:, :, tok0 + s0:tok0 + s0 + sn].rearrange("c p t -> p c t")), writes=[yk])
                for b in range(sn // 128):
                    r0 = s0 + b * 128
                    h_, hk = hx[bi % 2]
                    m_, mk = mg[bi % 2]
                    bi += 1
                    P.dma("sp", lambda e: e.dma_start(out=h_[:], in_=h_src[r0:r0 + 128, :]), writes=[hk])
                    for cb in range(4):
                        p_, pk = ps[pi % 8]
                        pi += 1

                        def mm(e):
                            for c in range(KD):
                                i = e.matmul(p_[:], lhsT=y_[:, c, b * 128:(b + 1) * 128], rhs=wo[:, c, cb * 512:(cb + 1) * 512],
                                             start=(c == 0), stop=(c == KD - 1))
                            return i
                        P.op("pe", mm, reads=[yk, "o_w"], writes=[pk])
                        P.op("dve", lambda e: e.tensor_tensor(out=m_[:, cb * 512:(cb + 1) * 512], in0=p_[:], in1=Gs[cond][:, cb * 512:(cb + 1) * 512], op=ALU.mult),
                             reads=[pk, f"o_G{cond}"], writes=[mk])
                    P.op("pool", lambda e: e.tensor_tensor(out=m_[:], in0=m_[:], in1=h_[:], op=ALU.add), reads=[mk, hk], writes=[mk])
                    P.dma("sp", lambda e: e.dma_start(out=h_dst[r0:r0 + 128, :], in_=m_[:]), reads=[mk], writes=["hdst"])
    barrier(P)


def phase_op0(P, nc, K, io):
    phase_op(P, nc, K, io, 0, [io.ycT, io.yfT], io.even_w_out,
             [(0, NLAT, 0, io.x, io.h_mid), (NLAT, NCTX, 1, io.ctx, io.h_ctx)])


def phase_op1(P, nc, K, io):
    oT = io.oT
    phase_op(P, nc, K, io, 1, [oT[0:8], oT[8:16]], io.odd_w_out, [(0, NLAT, 0, io.h_lat, io.h_mid)])


def phase_moe(P, nc, K, io, l, segs):
    blocks = []
    for (ntok, cond, h_src, h_dst) in segs:
        for b in range(ntok // 128):
            blocks.append((cond, h_src[b * 128:(b + 1) * 128, :], h_dst[b * 128:(b + 1) * 128, :]))
    nb = len(blocks)
    NJ = HCAP // 128
    with ExitStack() as es:
        idx1 = _T(nc, es, "e_idx1", [128, nb], I32)
        idx2 = _T(nc, es, "e_idx2", [128, nb], I32)
        wts = _T(nc, es, "e_wts", [128, 2, nb], F32)
        cnti = _T(nc, es, "e_cnti", [128, 16], I32)
        Gs = {}
        with ExitStack() as es2:
            ABs = {}
            for cond in sorted(set(b[0] for b in blocks)):
                ABs[cond] = make_AB(P, nc, es2, K, io, l, 1, cond, f"e_c{cond}")
            xf = [(_T(nc, es2, f"e_xf{i}", [128, D], F32), f"e_xf{i}") for i in range(2)]
            ub = [(_T(nc, es2, f"e_ub{i}", [128, D], BF16), f"e_ub{i}") for i in range(3)]
            sq = (_T(nc, es2, "e_sq", [128, D], BF16), "e_sq")
            st = [(_T(nc, es2, f"e_st{i}", [128, 4], F32), f"e_st{i}") for i in range(2)]
            vT = [(_T(nc, es2, f"e_vT{i}", [128, KD, 128], BF16), f"e_vT{i}") for i in range(2)]
            psT = [(_PS(nc, es2, f"e_pT{i}", [128, 8, 128], BF16), f"e_pT{i}") for i in range(2)]
            pl = [(_PS(nc, es2, f"e_pl{i}", [128, 64]), f"e_pl{i}") for i in range(2)]
            wr = _T(nc, es2, "e_wr", [128, KD, 20], BF16)
            br = _T(nc, es2, "e_br", [128, 20], F32)
            tri = _T(nc, es2, "e_tri", [128, 128], BF16)
            ecap = _T(nc, es2, "e_ecap", [128, 16], F32)
            base = _T(nc, es2, "e_base", [128, 16], F32)
            P.dma("pool", lambda e: e.dma_start(out=wr[:], in_=io.moe_wr[l].rearrange("(k p) n -> p k n", p=128)), writes=["e_wr"])
            P.dma("sp", lambda e: e.dma_start(out=br[:], in_=io.moe_br[l:l + 1, :].partition_broadcast(128)), writes=["e_br"])
            P.dma("sp", lambda e: e.dma_start(out=tri[:], in_=io.tri), writes=["e_tri"])
            P.op("pool", lambda e: e.iota(ecap[:], pattern=[[CAP, 16]], base=0, channel_multiplier=0, allow_small_or_imprecise_dtypes=True), writes=["e_ecap"])
            P.op("pool", lambda e: e.memset(base[:], 0.0), writes=["e_base"])
            sm = [(_T(nc, es2, f"e_sm{i}", [128, 128], F32), f"e_sm{i}") for i in range(2)]
            ac = [(_T(nc, es2, f"e_ac{i}", [128, 16], BF16), f"e_ac{i}") for i in range(2)]
            def route(bi, cond, src, dst):
                    A, Bt = ABs[cond]
                    u_, uk = ub[bi % 3]
                    v_, vk = vT[bi % 2]
                    norm_block(P, K, src, A, Bt, xf[bi % 2], (u_, uk), sq, st[bi % 2], uT=(v_, vk), uT_off=0, psT=psT)
                    p_, pk = pl[bi % 2]
                    s_, smk = sm[bi % 2]
                    a_, ak = ac[bi % 2]

                    def mm(e):
                        for k in range(KD):
                            i = e.matmul(p_[:, 0:20], lhsT=v_[:, k, :], rhs=wr[:, k, :], start=(k == 0), stop=(k == KD - 1))
                        return i
                    P.op("pe", mm, reads=[vk, "e_wr"], writes=[pk])
                    R = [smk]

                    def dv(fn, extra_r=(), extra_w=()):
                        P.op("dve", fn, reads=R + list(extra_r), writes=R + list(extra_w))
                    lg = s_[:, 0:20]
                    gm, ngm, gs, gw = s_[:, 20:21], s_[:, 21:22], s_[:, 22:23], s_[:, 23:24]
                    mgk = s_[:, 24:28]
                    esel = s_[:, 28:32]
                    m1, m2 = s_[:, 32:33], s_[:, 33:34]
                    o1, o2 = s_[:, 34:38], s_[:, 38:42]
                    es2_ = s_[:, 42:46]
                    dd, ed = s_[:, 46:47], s_[:, 47:48]
                    E1, E2 = s_[:, 48:64], s_[:, 64:80]
                    pos, posc, tmp16 = s_[:, 80:96], s_[:, 96:112], s_[:, 112:128]
                    dv(lambda e: e.tensor_tensor(out=lg, in0=p_[:, 0:20], in1=br[:], op=ALU.add), extra_r=[pk, "e_br"])
                    yield
                    dv(lambda e: e.tensor_reduce(out=gm, in_=s_[:, 0:4], axis=AX.X, op=ALU.max))
                    yield
                    dv(lambda e: e.tensor_scalar(out=ngm, in0=gm, scalar1=-1.0, scalar2=None, op0=ALU.mult))
                    yield
                    P.op("act", lambda e: e.activation(out=tmp16[:, 0:4], in_=s_[:, 0:4], func=AF.Exp, bias=ngm, scale=1.0, accum_out=gs), reads=R, writes=R)
                    yield
                    dv(lambda e: e.reciprocal(out=gw, in_=gs))
                    yield
                    dv(lambda e: e.tensor_scalar(out=mgk, in0=s_[:, 0:4], scalar1=gm, scalar2=None, op0=ALU.is_equal))
                    yield
                    dv(lambda e: e.tensor_scalar(out=esel, in0=s_[:, 4:8], scalar1=s_[:, 24:25], scalar2=None, op0=ALU.mult))
                    yield
                    for g in range(1, 4):
                        dv(lambda e: e.scalar_tensor_tensor(out=esel, in0=s_[:, 4 + 4 * g:8 + 4 * g], scalar=s_[:, 24 + g:25 + g], in1=esel, op0=ALU.mult, op1=ALU.add))
                        yield
                    dv(lambda e: e.tensor_reduce(out=m1, in_=esel, axis=AX.X, op=ALU.max))
                    yield
                    dv(lambda e: e.tensor_scalar(out=o1, in0=esel, scalar1=m1, scalar2=None, op0=ALU.is_equal))
                    yield
                    dv(lambda e: e.scalar_tensor_tensor(out=es2_, in0=o1, scalar=-1.0e30, in1=esel, op0=ALU.mult, op1=ALU.add))
                    yield
                    dv(lambda e: e.tensor_reduce(out=m2, in_=es2_, axis=AX.X, op=ALU.max))
                    yield
                    dv(lambda e: e.tensor_scalar(out=o2, in0=es2_, scalar1=m2, scalar2=None, op0=ALU.is_equal))
                    yield
                    dv(lambda e: e.tensor_tensor(out=dd, in0=m2, in1=m1, op=ALU.subtract))
                    yield
                    P.op("act", lambda e: e.activation(out=ed, in_=dd, func=AF.Exp), reads=R, writes=R)
                    yield
                    dv(lambda e: e.tensor_scalar(out=ed, in0=ed, scalar1=1.0, scalar2=None, op0=ALU.add))
                    yield
                    dv(lambda e: e.reciprocal(out=ed, in_=ed))
                    yield
                    dv(lambda e: e.tensor_tensor(out=wts[:, 0, bi:bi + 1], in0=gw, in1=ed, op=ALU.mult), extra_w=["e_wts"])
                    yield
                    dv(lambda e: e.tensor_tensor(out=wts[:, 1, bi:bi + 1], in0=gw, in1=wts[:, 0, bi:bi + 1], op=ALU.subtract), extra_r=["e_wts"], extra_w=["e_wts"])
                    yield
                    for g in range(4):
                        dv(lambda e: e.tensor_scalar(out=s_[:, 48 + 4 * g:52 + 4 * g], in0=o1, scalar1=s_[:, 24 + g:25 + g], scalar2=None, op0=ALU.mult))
                        yield
                        dv(lambda e: e.tensor_scalar(out=s_[:, 64 + 4 * g:68 + 4 * g], in0=o2, scalar1=s_[:, 24 + g:25 + g], scalar2=None, op0=ALU.mult))
                        yield
                    dv(lambda e: e.tensor_tensor(out=a_[:], in0=E1, in1=E2, op=ALU.add), extra_w=[ak])
                    yield

                    def mm2(e):
                        e.matmul(p_[:, 32:48], lhsT=tri[:], rhs=a_[:], start=True, stop=True)
                        return e.matmul(p_[:, 48:64], lhsT=K.ones[:], rhs=a_[:], start=True, stop=True)
                    P.op("pe", mm2, reads=[ak, "e_tri", "k_ones"], writes=[pk])
                    dv(lambda e: e.tensor_tensor(out=pos, in0=p_[:, 32:48], in1=base[:], op=ALU.add), extra_r=[pk, "e_base"])
                    yield
                    dv(lambda e: e.tensor_tensor(out=base[:], in0=p_[:, 48:64], in1=base[:], op=ALU.add), extra_r=[pk, "e_base"], extra_w=["e_base"])
                    yield
                    dv(lambda e: e.tensor_scalar(out=tmp16, in0=pos, scalar1=float(CAP), scalar2=1.0e6, op0=ALU.is_ge, op1=ALU.mult))
                    yield
                    dv(lambda e: e.tensor_tensor(out=posc, in0=pos, in1=ecap[:], op=ALU.add), extra_r=["e_ecap"])
                    yield
                    dv(lambda e: e.tensor_tensor(out=posc, in0=posc, in1=tmp16, op=ALU.add))
                    yield
                    dv(lambda e: e.tensor_tensor(out=tmp16, in0=posc, in1=E1, op=ALU.mult))
                    yield
                    dv(lambda e: e.tensor_reduce(out=dd, in_=tmp16, axis=AX.X, op=ALU.add))
                    yield
                    dv(lambda e: e.tensor_copy(out=idx1[:, bi:bi + 1], in_=dd), extra_w=[f"e_i1_{bi}"])
                    yield
                    dv(lambda e: e.tensor_tensor(out=tmp16, in0=posc, in1=E2, op=ALU.mult))
                    yield
                    dv(lambda e: e.tensor_reduce(out=dd, in_=tmp16, axis=AX.X, op=ALU.add))
                    yield
                    dv(lambda e: e.tensor_copy(out=idx2[:, bi:bi + 1], in_=dd), extra_w=[f"e_i2_{bi}"])
                    yield
                    for (ix, ik) in [(idx1, f"e_i1_{bi}"), (idx2, f"e_i2_{bi}")]:
                        P.dma("pool", lambda e: e.indirect_dma_start(out=io.xg, out_offset=bass.IndirectOffsetOnAxis(ap=ix[:, bi:bi + 1], axis=0),
                                                                      in_=u_[:], in_offset=None, bounds_check=K.breg, oob_is_err=False),
                              reads=[uk, ik], writes=["xg"])
            gens = [route(bi, cond, src, dst) for bi, (cond, src, dst) in enumerate(blocks)]
            active = []

            def step(g_):
                try:
                    next(g_)
                    return True
                except StopIteration:
                    if g_ in active:
                        active.remove(g_)
                    return False
            while gens or active:
                if gens and len(active) < 2:
                    if active:
                        for _ in range(4):
                            if not step(active[0]):
                                break
                    active.append(gens.pop(0))
                for g_ in list(active):
                    step(g_)
            if io.dbg is not None:
                P.dma("sp", lambda e: e.dma_start(out=io.dbg[l], in_=base[:]), reads=["e_base"], writes=["dbg"])
            P.op("dve", lambda e: e.tensor_copy(out=cnti[:], in_=base[:]), reads=["e_base"], writes=["e_cnti"])
            barrier(P)
        with ExitStack() as es3:
            wg = [(_T(nc, es3, f"e_wg{i}", [128, KD, 512], BF16), f"e_wg{i}") for i in range(2)]
            wu = [(_T(nc, es3, f"e_wu{i}", [128, KD, 512], BF16), f"e_wu{i}") for i in range(2)]
            wd = [(_T(nc, es3, f"e_wd{i}", [128, 4, D], BF16), f"e_wd{i}") for i in range(2)]
            xr = [(_T(nc, es3, f"e_xr{i}", [128, NJ, D], BF16), f"e_xr{i}") for i in range(2)]
            xT = _T(nc, es3, "e_xT", [128, KD, HCAP], BF16)
            aT = _T(nc, es3, "e_aT", [128, 4, HCAP], BF16)
            sg = [(_T(nc, es3, f"e_sg{i}", [128, HCAP], F32), f"e_sg{i}") for i in range(2)]
            yb = [(_T(nc, es3, f"e_yb{i}", [128, D], BF16), f"e_yb{i}") for i in range(2)]
            psT = [(_PS(nc, es3, f"e_qT{i}", [128, 8, 128], BF16), f"e_qT{i}") for i in range(2)]
            pg = [(_PS(nc, es3, f"e_pg{i}", [128, 512]), f"e_pg{i}") for i in range(6)]
            cregs = {"pe": es3.enter_context(nc.tensor.register(f"e_cntp{l}")),
                     "act": es3.enter_context(nc.scalar.register(f"e_cnta{l}")),
                     "dve": es3.enter_context(nc.vector.register(f"e_cntv{l}")),
                     "sp": es3.enter_context(nc.sync.register(f"e_cnts{l}"))}
            pgi = 0
            ybi = 0
            xri = 0
            for ex in range(16):
                g_, gk = wg[ex % 2]
                u_, uk = wu[ex % 2]
                d_, dk = wd[ex % 2]
                gv = io.moe_w_gate[l][ex].rearrange("(k p) n -> p k n", p=128)
                uv = io.moe_w_up[l][ex].rearrange("(k p) n -> p k n", p=128)
                dvw = io.moe_w_down[l][ex].rearrange("(k p) n -> p k n", p=128)
                for hq in range(2):
                    P.dma("pool", lambda e: e.dma_start(out=g_[:, hq * 8:(hq + 1) * 8, :], in_=gv[:, hq * 8:(hq + 1) * 8, :]), writes=[gk])
                    P.dma("pool", lambda e: e.dma_start(out=u_[:, hq * 8:(hq + 1) * 8, :], in_=uv[:, hq * 8:(hq + 1) * 8, :]), writes=[uk])
                    P.dma("pool", lambda e: e.dma_start(out=d_[:, hq * 2:(hq + 1) * 2, :], in_=dvw[:, hq * 2:(hq + 1) * 2, :]), writes=[dk])
                preds = {}
                for en in ["pe", "act", "dve", "sp"]:
                    P._deps(en, ["e_cnti"], [])
                    P.E[en].reg_load(cregs[en], cnti[0:1, ex:ex + 1])
                    preds[en] = P.E[en].snap(cregs[en]) > HCAP
                for hh in range(2):
                    x_, xk = xr[xri % 2]
                    xri += 1
                    r0 = ex * CAP + hh * HCAP

                    def peop(fn, reads, writes):
                        cop("pe", fn, reads, writes)

                    def cop(en, fn, reads, writes):
                        if hh == 0:
                            P.op(en, fn, reads=reads, writes=writes)
                        else:
                            P.op_if(en, preds[en], fn, reads=reads, writes=writes)
                    def cdma(fn, reads, writes):
                        if hh == 0:
                            P.dma("sp", fn, reads=reads, writes=writes)
                        else:
                            P.dma_if("sp", preds["sp"], fn, reads=reads, writes=writes)
                    cdma(lambda e: e.dma_start(out=x_[:], in_=io.xg[r0:r0 + HCAP, :].rearrange("(j p) n -> p j n", p=128)), ["xg"], [xk])
                    for j in range(NJ):
                        for half in range(2):
                            pt, ptk = psT[half]

                            def tr(e):
                                for jj in range(8):
                                    k = half * 8 + jj
                                    i = e.transpose(out=pt[:, jj, :], in_=x_[:, j, k * 128:(k + 1) * 128], identity=K.ident[:])
                                return i
                            peop(tr, [xk], [ptk])
                            if half == 0:
                                cop("act", lambda e: e.copy(out=xT[:, 0:8, j * 128:(j + 1) * 128], in_=pt[:]), [ptk], ["e_xT"])
                            else:
                                cop("dve", lambda e: e.tensor_copy(out=xT[:, 8:16, j * 128:(j + 1) * 128], in_=pt[:]), [ptk], ["e_xT"])
                    for fc in range(4):
                        pG, pGk = pg[pgi % 6]
                        pU, pUk = pg[(pgi + 1) % 6]
                        pgi += 2

                        def mmg(e):
                            for k in range(KD):
                                i = e.matmul(pG[:, 0:HCAP], lhsT=g_[:, k, fc * 128:(fc + 1) * 128], rhs=xT[:, k, :], start=(k == 0), stop=(k == KD - 1))
                            return i

                        def mmu(e):
                            for k in range(KD):
                                i = e.matmul(pU[:, 0:HCAP], lhsT=u_[:, k, fc * 128:(fc + 1) * 128], rhs=xT[:, k, :], start=(k == 0), stop=(k == KD - 1))
                            return i
                        peop(mmg, [gk, "e_xT"], [pGk])
                        peop(mmu, [uk, "e_xT"], [pUk])
                        s_, sk = sg[fc % 2]
                        cop("act", lambda e: e.activation(out=s_[:], in_=pG[:, 0:HCAP], func=AF.Silu), [pGk], [sk])
                        cop("dve", lambda e: e.tensor_tensor(out=aT[:, fc, :], in0=s_[:], in1=pU[:, 0:HCAP], op=ALU.mult), [sk, pUk], ["e_aT"])
                    for j in range(NJ):
                        y_, yk = yb[ybi % 2]
                        ybi += 1
                        for cb in range(4):
                            pY, pYk = pg[pgi % 6]
                            pgi += 1

                            def mmd(e):
                                for fc in range(4):
                                    i = e.matmul(pY[:], lhsT=aT[:, fc, j * 128:(j + 1) * 128], rhs=d_[:, fc, cb * 512:(cb + 1) * 512], start=(fc == 0), stop=(fc == 3))
                                return i
                            peop(mmd, ["e_aT", dk], [pYk])
                            if cb % 2 == 0:
                                cop("act", lambda e: e.copy(out=y_[:, cb * 512:(cb + 1) * 512], in_=pY[:]), [pYk], [yk])
                            else:
                                cop("dve", lambda e: e.tensor_copy(out=y_[:, cb * 512:(cb + 1) * 512], in_=pY[:]), [pYk], [yk])
                        cdma(lambda e: e.dma_start(out=io.yg[r0 + j * 128:r0 + (j + 1) * 128, :], in_=y_[:]), [yk], ["yg"])
            barrier(P)
        with ExitStack() as es4:
            for cond in sorted(set(b[0] for b in blocks)):
                Gs[cond] = load_bcast(P, nc, es4, f"e_G{cond}", io.modv[l, cond:cond + 1, 5 * D:6 * D])
            y1 = [(_T(nc, es4, f"e_y1{i}", [128, D], BF16), f"e_y1{i}") for i in range(2)]
            y2 = [(_T(nc, es4, f"e_y2{i}", [128, D], BF16), f"e_y2{i}") for i in range(2)]
            hx = [(_T(nc, es4, f"e_hx{i}", [128, D], F32), f"e_hx{i}") for i in range(2)]
            tt = [(_T(nc, es4, f"e_tt{i}", [128, D], F32), f"e_tt{i}") for i in range(2)]
            t1 = [(_T(nc, es4, f"e_tq{i}", [128, D], F32), f"e_tq{i}") for i in range(2)]
            for i in range(2):
                P.op("pool", lambda e: e.memset(y1[i][0][:], 0.0), writes=[y1[i][1]])
                P.op("pool", lambda e: e.memset(y2[i][0][:], 0.0), writes=[y2[i][1]])
            for bi, (cond, src, dst) in enumerate(blocks):
                a_, ak = y1[bi % 2]
                b_, bk = y2[bi % 2]
                h_, hk = hx[bi % 2]
                t_, tk = tt[bi % 2]
                q_, qk = t1[bi % 2]
                P.dma("pool", lambda e: e.indirect_dma_start(out=a_[:], out_offset=None, in_=io.yg, in_offset=bass.IndirectOffsetOnAxis(ap=idx1[:, bi:bi + 1], axis=0),
                                                              bounds_check=K.breg, oob_is_err=False), reads=["yg", f"e_i1_{bi}"], writes=[ak])
                P.dma("pool", lambda e: e.indirect_dma_start(out=b_[:], out_offset=None, in_=io.yg, in_offset=bass.IndirectOffsetOnAxis(ap=idx2[:, bi:bi + 1], axis=0),
                                                              bounds_check=K.breg, oob_is_err=False), reads=["yg", f"e_i2_{bi}"], writes=[bk])
                P.dma("sp", lambda e: e.dma_start(out=h_[:], in_=src), writes=[hk])
                P.op("act", lambda e: e.activation(out=q_[:], in_=a_[:], func=AF.Identity, scale=wts[:, 0, bi:bi + 1]), reads=[ak, "e_wts"], writes=[qk])
                P.op("dve", lambda e: e.scalar_tensor_tensor(out=t_[:], in0=b_[:], scalar=wts[:, 1, bi:bi + 1], in1=q_[:], op0=ALU.mult, op1=ALU.add),
                     reads=[bk, "e_wts", qk], writes=[tk])
                P.op("dve", lambda e: e.tensor_tensor(out=t_[:], in0=t_[:], in1=Gs[cond][:], op=ALU.mult), reads=[tk, f"e_G{cond}"], writes=[tk])
                P.op("dve", lambda e: e.tensor_tensor(out=t_[:], in0=t_[:], in1=h_[:], op=ALU.add), reads=[tk, hk], writes=[tk])
                P.dma("sp", lambda e: e.dma_start(out=dst, in_=t_[:]), reads=[tk], writes=["hdst"])
    barrier(P)


def phase_moe0(P, nc, K, io):
    phase_moe(P, nc, K, io, 0, [(NLAT, 0, io.h_mid, io.h_lat), (NCTX, 1, io.h_ctx, io.h_ctx)])


def phase_moe1(P, nc, K, io):
    phase_moe(P, nc, K, io, 1, [(NLAT, 0, io.h_mid, io.out)])


PHASE_FN.update({"l0b": phase_l0b, "op0": phase_op0, "op1": phase_op1, "moe0": phase_moe0, "moe1": phase_moe1})


def phase_l1a(P, nc, K, io):
    NT = NLAT + NCTX
    with ExitStack() as es:
        uT = (_T(nc, es, "q_uT", [128, KD, NT], BF16), "q_uT")
        with ExitStack() as es2:
            Al, Bl = make_AB(P, nc, es2, K, io, 1, 0, 0, "q_l")
            Ac, Bc = make_AB(P, nc, es2, K, io, 1, 0, 1, "q_c")
            xf = [(_T(nc, es2, f"q_xf{i}", [128, D], F32), f"q_xf{i}") for i in range(2)]
            ub = [(_T(nc, es2, f"q_ub{i}", [128, D], BF16), f"q_ub{i}") for i in range(2)]
            sq = (_T(nc, es2, "q_sq", [128, D], BF16), "q_sq")
            st = [(_T(nc, es2, f"q_st{i}", [128, 4], F32), f"q_st{i}") for i in range(2)]
            psT = [(_PS(nc, es2, f"q_pT{i}", [128, 8, 128], BF16), f"q_pT{i}") for i in range(2)]
            for b in range(NT // 128):
                if b < 16:
                    src, A, Bt = io.h_lat[b * 128:(b + 1) * 128, :], Al, Bl
                else:
                    src, A, Bt = io.h_ctx[(b - 16) * 128:(b - 15) * 128, :], Ac, Bc
                norm_block(P, K, src, A, Bt, xf[b % 2], ub[b % 2], sq, st[b % 2], uT=uT, uT_off=b * 128, psT=psT)
            barrier(P)
        wv = io.odd_w_qkv.rearrange("(k p) n -> p k n", p=128)
        wt = [(_T(nc, es, f"q_w{i}", [128, KD, 128], BF16), f"q_w{i}") for i in range(3)]
        cosT = _T(nc, es, "q_cos", [128, NLAT], F32)
        sinT = _T(nc, es, "q_sin", [128, NLAT], F32)
        rotT = _T(nc, es, "q_rot", [128, 128], BF16)
        gn = _T(nc, es, "q_gn", [128, 2], F32)
        P.dma("sp", lambda e: e.dma_start(out=cosT[:], in_=io.rope_cs[0]), writes=["q_cos"])
        P.dma("sp", lambda e: e.dma_start(out=sinT[:], in_=io.rope_cs[1]), writes=["q_sin"])
        P.dma("sp", lambda e: e.dma_start(out=rotT[:], in_=io.rotT), writes=["q_rot"])
        P.dma("sp", lambda e: e.dma_start(out=gn[:], in_=io.qk_norm), writes=["q_gn"])
        px = [(_PS(nc, es, f"q_px{i}", [128, 512]), f"q_px{i}") for i in range(3)]
        pss = [(_PS(nc, es, f"q_pss{i}", [128, 512]), f"q_pss{i}") for i in range(2)]
        pr = [(_PS(nc, es, f"q_pr{i}", [128, 512]), f"q_pr{i}") for i in range(2)]
        sqb = [(_T(nc, es, f"q_sqb{i}", [128, 512], BF16), f"q_sqb{i}") for i in range(3)]
        rs = [(_T(nc, es, f"q_rs{i}", [128, 512], F32), f"q_rs{i}") for i in range(2)]
        yb = [(_T(nc, es, f"q_yb{i}", [128, 512], BF16), f"q_yb{i}") for i in range(3)]
        t1 = [(_T(nc, es, f"q_t1{i}", [128, 512], F32), f"q_t1{i}") for i in range(2)]
        t2 = [(_T(nc, es, f"q_t2{i}", [128, 512], F32), f"q_t2{i}") for i in range(2)]
        ob = [(_T(nc, es, f"q_ob{i}", [128, 512], BF16), f"q_ob{i}") for i in range(2)]
        it = 0

        def run_slabs(slabs, pend=()):
            items = []
            for s in slabs:
                isq = s < 16
                tbl = [(0, 512), (512, 512), (1024, 512), (1536, 512)] + ([] if isq else [(2048, 256)])
                for (t0, tn) in tbl:
                    items.append((s, isq, t0, tn))
            n = len(items)
            wcur = {}

            def s1(i):
                s, isq, t0, tn = items[i]
                def ensure(sx):
                    if sx not in wcur:
                        w, wk = wt[sx % 3]
                        col = sx * 128
                        P.dma("pool", lambda e: e.dma_start(out=w[:], in_=wv[:, :, col:col + 128]), writes=[wk])
                        wcur[sx] = (w, wk)
                ensure(s)
                if s + 1 in slabs and (s + 1) not in wcur:
                    ensure(s + 1)
                    if pend:
                        pend.pop(0)()
                w, wk = wcur[s]
                p_, pk = px[i % 3]
                sq_, sqk = sqb[i % 3]

                def mm(e):
                    for k in range(KD):
                        ii = e.matmul(p_[:, 0:tn], lhsT=w[:, k, :], rhs=uT[0][:, k, t0:t0 + tn], start=(k == 0), stop=(k == KD - 1))
                    return ii
                P.op("pe", mm, reads=[wk, "q_uT"], writes=[pk])
                P.op("act", lambda e: e.activation(out=sq_[:, 0:tn], in_=p_[:, 0:tn], func=AF.Square), reads=[pk], writes=[sqk])

            def s2(i):
                s, isq, t0, tn = items[i]
                p_, pk = px[i % 3]
                sq_, sqk = sqb[i % 3]
                ps_, psk = pss[i % 2]
                r_, rk = rs[i % 2]
                y_, yk = yb[i % 3]
                P.op("pe", lambda e: e.matmul(ps_[:, 0:tn], lhsT=K.ones[:], rhs=sq_[:, 0:tn], start=True, stop=True), reads=[sqk, "k_ones"], writes=[psk])
                P.op("act", lambda e: e.activation(out=r_[:, 0:tn], in_=ps_[:, 0:tn], func=AF.Sqrt, scale=1.0 / 128, bias=K.epsb[:, 0:1]), reads=[psk], writes=[rk])
                P.op("dve", lambda e: e.reciprocal(out=r_[:, 0:tn], in_=r_[:, 0:tn]), reads=[rk], writes=[rk])
                gcol = gn[:, 0:1] if isq else gn[:, 1:2]
                P.op("dve", lambda e: e.scalar_tensor_tensor(out=y_[:, 0:tn], in0=p_[:, 0:tn], scalar=gcol, in1=r_[:, 0:tn], op0=ALU.mult, op1=ALU.mult),
                     reads=[pk, rk, "q_gn"], writes=[yk])

            def s3(i):
                s, isq, t0, tn = items[i]
                y_, yk = yb[i % 3]
                pr_, prk = pr[i % 2]
                a_, ak = t1[i % 2]
                b_, bk = t2[i % 2]
                o_, ok = ob[i % 2]
                if t0 < NLAT:
                    P.op("pe", lambda e: e.matmul(pr_[:, 0:tn], lhsT=rotT[:], rhs=y_[:, 0:tn], start=True, stop=True), reads=[yk, "q_rot"], writes=[prk])
                    P.op("pool", lambda e: e.tensor_tensor(out=a_[:, 0:tn], in0=y_[:, 0:tn], in1=cosT[:, t0:t0 + tn], op=ALU.mult), reads=[yk, "q_cos"], writes=[ak])
                    P.op("dve", lambda e: e.tensor_tensor(out=b_[:, 0:tn], in0=pr_[:, 0:tn], in1=sinT[:, t0:t0 + tn], op=ALU.mult), reads=[prk, "q_sin"], writes=[bk])
                    P.op("pool", lambda e: e.tensor_tensor(out=o_[:, 0:tn], in0=a_[:, 0:tn], in1=b_[:, 0:tn], op=ALU.add), reads=[ak, bk], writes=[ok])
                    dst = io.qT[s, :, t0:t0 + tn] if isq else io.kT_own[s - 16, :, t0:t0 + tn]
                    P.dma("sp", lambda e: e.dma_start(out=dst, in_=o_[:, 0:tn]), reads=[ok], writes=["q_out" if isq else "k_out"])
                else:
                    P.dma("sp", lambda e: e.dma_start(out=io.kT_ctx[s - 16, :, 0:tn], in_=y_[:, 0:tn]), reads=[yk], writes=["k_out"])
            for t in range(n + 2):
                if t < n:
                    s1(t)
                if 0 <= t - 1 < n:
                    s2(t - 1)
                if 0 <= t - 2 < n:
                    s3(t - 2)

        run_slabs(list(range(16, 32)))
        wvt = [(_T(nc, es, f"q_wv{i}", [128, KD, 512], BF16), f"q_wv{i}") for i in range(4)]
        for cb in range(4):
            w, wk = wvt[cb]
            for hq in range(2):
                P.dma("pool", lambda e: e.dma_start(out=w[:, hq * 8:(hq + 1) * 8, :], in_=wv[:, hq * 8:(hq + 1) * 8, 4096 + cb * 512:4096 + (cb + 1) * 512]), writes=[wk])
        pend = []
        for cb in range(4):
            w, wk = wvt[cb]
            for _ in range(2):
                if pend:
                    pend.pop(0)()
            for b in range(NT // 128):
                i2 = it % 2
                it += 1
                p_, pk = px[i2]
                o_, ok = ob[i2]

                def mmv(e):
                    for k in range(KD):
                        i = e.matmul(p_[:], lhsT=uT[0][:, k, b * 128:(b + 1) * 128], rhs=w[:, k, :], start=(k == 0), stop=(k == KD - 1))
                    return i
                P.op("pe", mmv, reads=[wk, "q_uT"], writes=[pk])
                if b % 2 == 0:
                    P.op("act", lambda e: e.copy(out=o_[:], in_=p_[:]), reads=[pk], writes=[ok])
                else:
                    P.op("dve", lambda e: e.tensor_copy(out=o_[:], in_=p_[:]), reads=[pk], writes=[ok])
                if b < 16:
                    dst = io.v_own[cb, b * 128:(b + 1) * 128, :]
                else:
                    dst = io.v_ctx[(b - 16) * 128:(b - 15) * 128, cb * 512:(cb + 1) * 512]
                P.dma("sp", lambda e: e.dma_start(out=dst, in_=o_[:]), reads=[ok], writes=["v_out"])
        run_slabs(list(range(16)), pend)
        while pend:
            pend.pop(0)()
    barrier(P)


def phase_attn(P, nc, K, io):
    NK = 2 * NLAT + NCTX
    NKC = NK // 128
    with ExitStack() as es:
        lv = _T(nc, es, "t_lv", [1, 4, 128], F32)
        l1 = _T(nc, es, "t_l1", [1, 8], F32)
        onesf = _T(nc, es, "t_onesf", [1, 128], F32)
        nlam = _T(nc, es, "t_nlam", [128, 1], F32)
        sw = _T(nc, es, "t_sw", [128, 2], F32)
        P.dma("sp", lambda e: e.dma_start(out=lv[:], in_=io.lam_vecs), writes=["t_lv"])
        P.dma("sp", lambda e: e.dma_start(out=sw[:], in_=io.subln_wT), writes=["t_sw"])
        P.op("pool", lambda e: e.memset(onesf[:], 1.0), writes=["t_onesf"])
        P.op("dve", lambda e: e.tensor_tensor(out=lv[:, 0, :], in0=lv[:, 0, :], in1=lv[:, 1, :], op=ALU.mult), reads=["t_lv"], writes=["t_lv"])
        P.op("dve", lambda e: e.tensor_tensor(out=lv[:, 2, :], in0=lv[:, 2, :], in1=lv[:, 3, :], op=ALU.mult), reads=["t_lv"], writes=["t_lv"])
        P.op("dve", lambda e: e.tensor_reduce(out=l1[:, 0:1], in_=lv[:, 0, :], axis=AX.X, op=ALU.add), reads=["t_lv"], writes=["t_l1"])
        P.op("dve", lambda e: e.tensor_reduce(out=l1[:, 1:2], in_=lv[:, 2, :], axis=AX.X, op=ALU.add), reads=["t_lv"], writes=["t_l1"])
        P.op("act", lambda e: e.activation(out=l1[:, 2:4], in_=l1[:, 0:2], func=AF.Exp), reads=["t_l1"], writes=["t_l1"])
        P.op("dve", lambda e: e.tensor_tensor(out=l1[:, 4:5], in0=l1[:, 3:4], in1=l1[:, 2:3], op=ALU.subtract), reads=["t_l1"], writes=["t_l1"])
        P.op("dve", lambda e: e.tensor_scalar(out=l1[:, 4:5], in0=l1[:, 4:5], scalar1=-LAM_INIT1, scalar2=None, op0=ALU.add), reads=["t_l1"], writes=["t_l1"])
        P.op("dve", lambda e: e.tensor_scalar(out=sw[:], in0=sw[:], scalar1=1.0 - LAM_INIT1, scalar2=None, op0=ALU.mult), reads=["t_sw"], writes=["t_sw"])
        kTb = [[_T(nc, es, f"t_kT{b}{c}", [128, NK], BF16) for c in range(2)] for b in range(2)]
        vtb = [_T(nc, es, f"t_v{b}", [128, NKC, 512], BF16) for b in range(2)]
        if io.mode == "ALL":
            ixk = _T(nc, es, "t_ixk", [128, 32], I32)
            ixv = _T(nc, es, "t_ixv", [128, 128], I32)
            P.dma("sp", lambda e: e.dma_start(out=ixk[:], in_=io.idx_k), writes=["t_ixk"])
            P.dma("sp", lambda e: e.dma_start(out=ixv[:], in_=io.idx_v), writes=["t_ixv"])
        qt = [(_T(nc, es, f"t_q{i}", [128, 2, 512], BF16), f"t_q{i}") for i in range(2)]
        pT = [(_T(nc, es, f"t_pT{i}", [128, 512], BF16), f"t_pT{i}") for i in range(3)]
        pS = [(_PS(nc, es, f"t_pS{i}", [128, 512]), f"t_pS{i}") for i in range(2)]
        pO = [[(_PS(nc, es, f"t_pO{c}{j}", [128, 512]), f"t_pO{c}{j}") for j in range(3)] for c in range(2)]
        rc = [_T(nc, es, f"t_rc{c}", [128, 512], F32) for c in range(2)]
        oh = [(_T(nc, es, f"t_oh{i}", [128, 512], F32), f"t_oh{i}") for i in range(2)]
        o2 = [(_T(nc, es, f"t_o2{i}", [128, 512], F32), f"t_o2{i}") for i in range(2)]
        osq = [(_T(nc, es, f"t_osq{i}", [128, 512], BF16), f"t_osq{i}") for i in range(2)]
        rsd = _T(nc, es, "t_rsd", [128, 512], F32)
        oo = [(_T(nc, es, f"t_oo{i}", [128, 512], BF16), f"t_oo{i}") for i in range(2)]
        P.op("pe", lambda e: e.matmul(pS[0][0][:, 0:1], lhsT=onesf[:], rhs=l1[:, 4:5], start=True, stop=True), reads=["t_l1", "t_onesf"], writes=["t_pS0"])
        P.op("dve", lambda e: e.tensor_copy(out=nlam[:], in_=pS[0][0][:, 0:1]), reads=["t_pS0"], writes=["t_nlam"])
        scale = 128 ** -0.5
        qi = 0
        pi = 0
        si = 0
        if io.mode == "ALL":
            kth = _gather_thunks(P, io.kT_own.rearrange("s p t -> (s p) t"), io.k4, 2048, 256, "k_out", "k4")
            vth = _gather_thunks(P, io.v_own.rearrange("g t n -> (g t) n"), io.v4, 8192, 1024, "v_out", "v4")
        else:
            kth, vth = [], []

        def issue_pair_colls(hp):
            if kth and hp < 4:
                for q_ in (2 * hp, 2 * hp + 1):
                    kth[q_]()
                    vth[q_]()

        def load_head(h):
            hp = h // 2
            vt = vtb[hp % 2]
            vk = f"t_v{hp % 2}"
            kT = kTb[h % 2]
            if h % 2 == 0:
                P.dma("sp", lambda e: e.dma_start(out=vt[:, 0:2, :], in_=io.v_ctx[:, hp * 512:(hp + 1) * 512].rearrange("(c p) n -> p c n", p=128)), writes=[vk])
                if io.mode == "ALL":
                    for c32 in range(32):
                        P.dma("pool", lambda e: e.indirect_dma_start(out=vt[:, 2 + c32, :], out_offset=None, in_=io.v4,
                                                                      in_offset=bass.IndirectOffsetOnAxis(ap=ixv[:, hp * 32 + c32:hp * 32 + c32 + 1], axis=0),
                                                                      bounds_check=K.breg_big, oob_is_err=False), reads=[f"v4_{2 * hp + (c32 % 16) // 8}", "t_ixv"], writes=[vk])
                else:
                    for q4 in range(4):
                        P.dma("sp", lambda e: e.dma_start(out=vt[:, 2 + q4 * 8:2 + (q4 + 1) * 8, :],
                                                          in_=io.v_full[hp, q4 * 1024:(q4 + 1) * 1024, :].rearrange("(c p) n -> p c n", p=128)), writes=[vk])
            for c in range(2):
                s = 2 * h + c
                kk = f"t_kT{h % 2}{c}"
                P.dma("sp", lambda e: e.dma_start(out=kT[c][:, 0:NCTX], in_=io.kT_ctx[s]), writes=[kk])
                if io.mode == "ALL":
                    for hf in range(2):
                        P.dma("pool", lambda e: e.indirect_dma_start(out=kT[c][:, NCTX + hf * NLAT:NCTX + (hf + 1) * NLAT], out_offset=None, in_=io.k4,
                                                                      in_offset=bass.IndirectOffsetOnAxis(ap=ixk[:, s * 2 + hf:s * 2 + hf + 1], axis=0),
                                                                      bounds_check=K.breg_big, oob_is_err=False), reads=[f"k4_{h}", "t_ixk"], writes=[kk])
                else:
                    P.dma("sp", lambda e: e.dma_start(out=kT[c][:, NCTX:NK], in_=io.kT_full[s]), writes=[kk])

        issue_pair_colls(0)
        load_head(0)
        for h in range(8):
            hp = h // 2
            vo = (h % 2) * 256
            vt = vtb[hp % 2]
            vk = f"t_v{hp % 2}"
            kT = kTb[h % 2]
            kks = [f"t_kT{h % 2}{c}" for c in range(2)]
            for qb in range(4):
                q_, qk = qt[qi % 2]
                qi += 1
                P.dma("sp", lambda e: e.dma_start(out=q_[:], in_=io.qT[2 * h:2 * h + 2, :, qb * 512:(qb + 1) * 512].rearrange("c p t -> p c t")), writes=[qk])
                steps = [(c, kc) for c in range(2) for kc in range(NKC)]
                bufs = []
                for _ in steps:
                    bufs.append((pS[si % 2], pT[pi % 3]))
                    si += 1
                    pi += 1

                def emit_S(i):
                    c, kc = steps[i]
                    (s_, sk), (p_, pk) = bufs[i]
                    P.op("pe", lambda e: e.matmul(s_[:], lhsT=kT[c][:, kc * 128:(kc + 1) * 128], rhs=q_[:, c, :], start=True, stop=True),
                         reads=[kks[c], qk], writes=[sk])
                    P.op("act", lambda e: e.activation(out=p_[:], in_=s_[:], func=AF.Exp, scale=scale), reads=[sk], writes=[pk])

                def emit_PV(i):
                    c, kc = steps[i]
                    (s_, sk), (p_, pk) = bufs[i]

                    def mmo(e):
                        e.matmul(pO[c][0][0][:], lhsT=vt[:, kc, vo:vo + 128], rhs=p_[:], start=(kc == 0), stop=(kc == NKC - 1))
                        e.matmul(pO[c][1][0][:], lhsT=vt[:, kc, vo + 128:vo + 256], rhs=p_[:], start=(kc == 0), stop=(kc == NKC - 1))
                        return e.matmul(pO[c][2][0][:], lhsT=K.ones[:], rhs=p_[:], start=(kc == 0), stop=(kc == NKC - 1))
                    P.op("pe", mmo, reads=[pk, vk, "k_ones"], writes=[pO[c][0][1], pO[c][1][1], pO[c][2][1]])
                if qb == 1 and h + 1 < 8:
                    load_head(h + 1)
                    if (h + 1) % 2 == 1:
                        issue_pair_colls((h + 1) // 2 + 1)
                emit_S(0)
                for i in range(len(steps)):
                    if i + 1 < len(steps):
                        emit_S(i + 1)
                    emit_PV(i)
                P.op("dve", lambda e: e.reciprocal(out=rc[0][:], in_=pO[0][2][0][:]), reads=[pO[0][2][1]], writes=["t_rc0"])
                P.op("dve", lambda e: e.reciprocal(out=rc[1][:], in_=pO[1][2][0][:]), reads=[pO[1][2][1]], writes=["t_rc1"])
                P.op("dve", lambda e: e.tensor_scalar(out=rc[1][:], in0=rc[1][:], scalar1=nlam[:, 0:1], scalar2=None, op0=ALU.mult), reads=["t_rc1", "t_nlam"], writes=["t_rc1"])
                for hf in range(2):
                    a_, ak = oh[hf]
                    b_, bk = o2[hf]
                    q2_, q2k = osq[hf]
                    P.op("dve", lambda e: e.tensor_tensor(out=a_[:], in0=pO[0][hf][0][:], in1=rc[0][:], op=ALU.mult), reads=[pO[0][hf][1], "t_rc0"], writes=[ak])
                    P.op("dve", lambda e: e.tensor_tensor(out=b_[:], in0=pO[1][hf][0][:], in1=rc[1][:], op=ALU.mult), reads=[pO[1][hf][1], "t_rc1"], writes=[bk])
                    P.op("dve", lambda e: e.tensor_tensor(out=a_[:], in0=a_[:], in1=b_[:], op=ALU.add), reads=[ak, bk], writes=[ak])
                    P.op("act", lambda e: e.activation(out=q2_[:], in_=a_[:], func=AF.Square), reads=[ak], writes=[q2k])
                s_, sk = pS[si % 2]
                si += 1

                def mms(e):
                    e.matmul(s_[:], lhsT=K.ones[:], rhs=osq[0][0][:], start=True, stop=False)
                    return e.matmul(s_[:], lhsT=K.ones[:], rhs=osq[1][0][:], start=False, stop=True)
                P.op("pe", mms, reads=[osq[0][1], osq[1][1], "k_ones"], writes=[sk])
                P.op("act", lambda e: e.activation(out=rsd[:], in_=s_[:], func=AF.Sqrt, scale=1.0 / 256, bias=K.epsb[:, 0:1]), reads=[sk], writes=["t_rsd"])
                P.op("dve", lambda e: e.reciprocal(out=rsd[:], in_=rsd[:]), reads=["t_rsd"], writes=["t_rsd"])
                for hf in range(2):
                    o_, ok = oo[hf]
                    P.op("dve", lambda e: e.scalar_tensor_tensor(out=o_[:], in0=oh[hf][0][:], scalar=sw[:, hf:hf + 1], in1=rsd[:], op0=ALU.mult, op1=ALU.mult),
                         reads=[oh[hf][1], "t_sw", "t_rsd"], writes=[ok])
                    P.dma("sp", lambda e: e.dma_start(out=io.oT[2 * h + hf, :, qb * 512:(qb + 1) * 512], in_=o_[:]), reads=[ok], writes=["oT"])
    barrier(P)


RG4 = [[0, 1, 2, 3], [4, 5, 6, 7]]


def _gather_thunks(P, src2d, dst2d, rows, chunk, rkey, wkey):
    th = []
    for q in range(rows // chunk):
        def f(q=q):
            P.coll(lambda e: e.collective_compute("AllGather", ALU.bypass, replica_groups=RG4,
                                                  ins=[src2d[q * chunk:(q + 1) * chunk, :].opt()],
                                                  outs=[dst2d[q * 4 * chunk:(q + 1) * 4 * chunk, :].opt()]),
                   reads=[rkey], writes=[f"{wkey}_{q}"])
        th.append(f)
    return th


def _gather_chunks(P, src2d, dst2d, rows, chunk, rkey, wkey):
    for f in _gather_thunks(P, src2d, dst2d, rows, chunk, rkey, wkey):
        f()


def phase_xab(P, nc, K, io):
    pass


def phase_xkv(P, nc, K, io):
    pass


PHASE_FN.update({"l1a": phase_l1a, "attn": phase_attn, "xab": phase_xab, "xkv": phase_xkv})


_BF = None
_CACHE = {}
FUSED = True
LAST_DBG = None


def _bf():
    global _BF
    if _BF is None:
        import ml_dtypes
        _BF = ml_dtypes.bfloat16
    return _BF


def _consts(hf):
    BF = _bf()
    c = {}
    c["ident"] = np.eye(128, dtype=np.float32).astype(BF)
    k = np.arange(256)
    ang = 2 * np.pi * np.outer(k, k) / 256
    cs = np.concatenate([np.cos(ang), np.sin(ang)], 1) / 16.0
    c["cs256"] = np.ascontiguousarray(cs.reshape(2, 128, 512).transpose(1, 0, 2)).astype(BF)
    t = np.arange(4096, dtype=np.int64)[:, None]
    n = (hf * 2048 + np.arange(2048, dtype=np.int64))[None, :]
    a2 = 2 * np.pi * ((t * n) % 4096).astype(np.float64) / 4096
    c["dft_c"] = (np.cos(a2) / 64.0).astype(np.float32).astype(BF)
    c["dft_s"] = (-np.sin(a2) / 64.0).astype(np.float32).astype(BF)
    d2 = np.stack([np.cos(ang), -np.sin(ang)], 1) / 16.0
    c["dft256"] = np.ascontiguousarray(d2.reshape(2, 128, 2, 256).transpose(1, 0, 2, 3)).astype(np.float32).astype(BF)
    tok = hf * 2048 + np.arange(2048)
    row, col = (tok // 64).astype(np.float32), (tok % 64).astype(np.float32)
    invf = (10000.0 ** (-np.arange(32, dtype=np.float32) / 32)).astype(np.float32)
    i = np.arange(128)
    pos = np.where((i // 64)[:, None] == 0, row[None, :], col[None, :]).astype(np.float32)
    angr = pos * invf[i % 32][:, None]
    c["rope_cs"] = np.stack([np.cos(angr), np.sin(angr)], 0).astype(np.float32)
    R = np.zeros((128, 128), np.float32)
    for ii in range(128):
        if ii % 64 < 32:
            R[ii, ii + 32] = -1.0
        else:
            R[ii, ii - 32] = 1.0
    c["rotT"] = np.ascontiguousarray(R.T).astype(BF)
    c["tri"] = np.triu(np.ones((128, 128), np.float32), k=1).astype(BF)
    return c


def _core_inputs(r, inp):
    b, hf = r // 2, r % 2
    m = {}
    m["x"] = np.ascontiguousarray(inp["x"][b, hf * 2048:(hf + 1) * 2048])
    m["ctx"] = np.ascontiguousarray(inp["ctx"][b])
    ht = 2048 if hf == 0 else 2047
    xh = np.zeros((128, 2048), np.float32)
    xh[0] = inp["x"][b, ht]
    m["xh"] = xh
    cond = np.stack([inp["c"][b], inp["c_ctx"]], 0)
    m["condT"] = np.ascontiguousarray(cond.reshape(2, 16, 128).transpose(2, 1, 0))
    fl = np.zeros((128, 2), np.float32)
    fl[:, 0] = 1.0 if hf == 1 else 0.0
    fl[:, 1] = 1.0 if hf == 0 else 0.0
    m["flags"] = fl
    for k in ["w_mod", "b_mod", "norm1_w", "norm2_w"]:
        m[k] = inp[k]
    m["even_w_in"] = inp["even_w_in"][0]
    m["conv_wT"] = np.ascontiguousarray(inp["even_conv_w"][0].reshape(3, 8, 128).transpose(2, 1, 0))
    m["even_w_out"] = inp["even_w_out"][0]
    m["odd_w_qkv"] = inp["odd_w_qkv"][0]
    m["qk_norm"] = np.ascontiguousarray(np.stack([inp["odd_q_norm"][0], inp["odd_k_norm"][0]], 1))
    m["lam_vecs"] = np.ascontiguousarray(np.stack([inp["odd_lambda_q1"][0], inp["odd_lambda_k1"][0], inp["odd_lambda_q2"][0], inp["odd_lambda_k2"][0]], 0)[None])
    m["subln_wT"] = np.ascontiguousarray(inp["odd_subln_w"][0].reshape(2, 128).T)
    m["odd_w_out"] = inp["odd_w_out"][0]
    for l in range(2):
        m[f"moe_wr{l}"] = np.ascontiguousarray(np.concatenate([inp["moe_w_group"][l], inp["moe_w_expert"][l]], 1))
        m[f"moe_w_gate{l}"] = inp["moe_w_gate"][l]
        m[f"moe_w_up{l}"] = inp["moe_w_up"][l]
        m[f"moe_w_down{l}"] = inp["moe_w_down"][l]
    m["moe_br"] = np.ascontiguousarray(np.concatenate([inp["moe_b_group"], inp["moe_b_expert"]], 1))
    m.update(_consts(hf))
    pb = (r % 4) // 2
    p = np.arange(128, dtype=np.int64)[:, None]
    def grow(f, rank, chunk):
        return (f // chunk) * (4 * chunk) + rank * chunk + (f % chunk)
    cols = []
    for gp in range(2):
        for c in range(32):
            cols.append(grow(gp * 2048 + (c % 16) * 128 + p, 2 * pb + c // 16, 512))
    m["idx_ab"] = np.concatenate(cols, 1).astype(np.int32)
    cols = []
    for s_ in range(16):
        for hs in range(2):
            cols.append(grow(s_ * 128 + p, 2 * pb + hs, 256))
    m["idx_k"] = np.concatenate(cols, 1).astype(np.int32)
    cols = []
    for hp in range(4):
        for c in range(32):
            cols.append(grow(hp * 2048 + (c % 16) * 128 + p, 2 * pb + c // 16, 1024))
    m["idx_v"] = np.concatenate(cols, 1).astype(np.int32)
    return m


def _get(mode):
    if mode not in _CACHE:
        _CACHE[mode] = build(mode)
    return _CACHE[mode]


def _launch(mode, maps):
    nc, io = _get(mode)
    res = run_bass_kernel_spmd(nc, [{k: m[k] for k in io.ext_in} for m in maps], core_ids=list(range(len(maps))))
    return [{k: np.asarray(r[k]) for k in io.ext_out} for r in res.results]


def kernel(**inputs):
    inp = {k: np.asarray(v) for k, v in inputs.items()}
    maps = [_core_inputs(r, inp) for r in range(8)]
    if FUSED:
        rr = _launch("ALL", maps)
        global LAST_DBG
        LAST_DBG = [r["dbg"] for r in rr]
        out = np.zeros((4, 4096, 2048), np.float32)
        for r in range(8):
            out[r // 2, (r % 2) * 2048:(r % 2 + 1) * 2048] = rr[r]["out"]
        return out
    ra = _launch("A", maps)
    for r in range(8):
        p0, p1 = (r // 2) * 2, (r // 2) * 2 + 1
        maps[r]["modv"] = ra[r]["modv"]
        maps[r]["ycT"] = ra[r]["ycT"]
        maps[r]["ab_ctx"] = ra[r]["ab_ctx"]
        maps[r]["ab_full"] = np.concatenate([ra[p0]["ab_own"], ra[p1]["ab_own"]], 1)
    rb = _launch("B", maps)
    for r in range(8):
        p0, p1 = (r // 2) * 2, (r // 2) * 2 + 1
        for k in ["h_lat", "qT", "kT_ctx", "v_ctx"]:
            maps[r][k] = rb[r][k]
        maps[r]["kT_full"] = np.concatenate([rb[p0]["kT_own"], rb[p1]["kT_own"]], 2)
        maps[r]["v_full"] = np.concatenate([rb[p0]["v_own"], rb[p1]["v_own"]], 1)
    rc = _launch("C", maps)
    out = np.zeros((4, 4096, 2048), np.float32)
    for r in range(8):
        out[r // 2, (r % 2) * 2048:(r % 2 + 1) * 2048] = rc[r]["out"]
    return out
```

```python
import numpy as np
from contextlib import ExitStack
import concourse.bass as bass
import concourse.mybir as mybir
from concourse.bass_utils import run_bass_kernel_spmd

F32 = mybir.dt.float32
BF16 = mybir.dt.bfloat16
I32 = mybir.dt.int32
AF = mybir.ActivationFunctionType
ALU = mybir.AluOpType
AX = mybir.AxisListType

NDS = 24


class Prog:
    def __init__(self, nc, es):
        self.nc = nc
        self.es = es
        self.E = {"pe": nc.tensor, "act": nc.scalar, "dve": nc.vector, "pool": nc.gpsimd, "sp": nc.sync}
        self.csem = {e: es.enter_context(nc.semaphore(f"c_{e}")) for e in ["pe", "act", "dve", "pool"]}
        self.ccnt = {e: 0 for e in self.csem}
        self.dsems = [es.enter_context(nc.semaphore(f"d_{i}")) for i in range(NDS)]
        self.dcnt = [0] * NDS
        self.dnext = 0
        self.seen = {e: {} for e in self.E}
        self.lastw = {}
        self.readers = {}
        self.n_wait = 0
        self.xsem = es.enter_context(nc.semaphore("x_cc"))
        self.xcnt = 0

    def coll(self, fn, reads=(), writes=()):
        self._deps("pool", reads, writes)
        inst = fn(self.E["pool"])
        self.xcnt += 1
        inst.then_inc(self.xsem)
        self._commit((("x", 0), self.xcnt), reads, writes)

    def _wait(self, eng, tok):
        semkey, val = tok
        if semkey == ("c", "pe") and eng == "pe":
            return
        if self.seen[eng].get(semkey, 0) >= val:
            return
        sem = self.csem[semkey[1]] if semkey[0] == "c" else (self.xsem if semkey[0] == "x" else self.dsems[semkey[1]])
        self.E[eng].wait_ge(sem, val)
        self.n_wait += 1
        self.seen[eng][semkey] = val

    def _deps(self, eng, reads, writes, is_dma=False):
        for k in reads:
            for sk, v in self.lastw.get(k, {}).items():
                self._wait(eng, (sk, v))
        for k in writes:
            for sk, v in self.lastw.get(k, {}).items():
                if is_dma and sk[0] == "d":
                    continue
                self._wait(eng, (sk, v))
            for sk, v in self.readers.get(k, {}).items():
                self._wait(eng, (sk, v))

    def _commit(self, tok, reads, writes, is_dma=False):
        for k in reads:
            r = self.readers.setdefault(k, {})
            if r.get(tok[0], 0) < tok[1]:
                r[tok[0]] = tok[1]
        for k in writes:
            w = self.lastw.setdefault(k, {})
            if w.get(tok[0], 0) < tok[1]:
                w[tok[0]] = tok[1]
            if not is_dma:
                self.readers[k] = {}

    def op(self, eng, fn, reads=(), writes=()):
        self._deps(eng, reads, writes)
        inst = fn(self.E[eng])
        self.ccnt[eng] += 1
        inst.then_inc(self.csem[eng], 1)
        self._commit((("c", eng), self.ccnt[eng]), reads, writes)

    def op_if(self, eng, pred, fn, reads=(), writes=()):
        self._deps(eng, reads, writes)
        e = self.E[eng]
        with e.If(pred):
            inst = fn(e)
            inst.then_inc(self.csem[eng], 1)
        with e.Else():
            e.sem_inc(self.csem[eng], 1)
        self.ccnt[eng] += 1
        self._commit((("c", eng), self.ccnt[eng]), reads, writes)

    def dma_if(self, q, pred, fn, reads=(), writes=()):
        self._deps(q, reads, writes, is_dma=True)
        i = self.dnext
        self.dnext = (i + 1) % NDS
        if self.dcnt[i] > 0:
            self._wait(q, (("d", i), self.dcnt[i]))
        e = self.E[q]
        with e.If(pred):
            inst = fn(e)
            inst.then_inc(self.dsems[i], 16)
        with e.Else():
            e.sem_inc(self.dsems[i], 16)
        self.dcnt[i] += 16
        self._commit((("d", i), self.dcnt[i]), reads, writes, is_dma=True)

    def dma(self, q, fn, reads=(), writes=()):
        self._deps(q, reads, writes, is_dma=True)
        i = self.dnext
        self.dnext = (i + 1) % NDS
        if self.dcnt[i] > 0:
            self._wait(q, (("d", i), self.dcnt[i]))
        inst = fn(self.E[q])
        self.dcnt[i] += 16
        inst.then_inc(self.dsems[i], 16)
        self._commit((("d", i), self.dcnt[i]), reads, writes, is_dma=True)

    def finish(self, eng="sp"):
        for i in range(NDS):
            if self.dcnt[i] > 0:
                self._wait(eng, (("d", i), self.dcnt[i]))
        for e, c in self.ccnt.items():
            if c > 0:
                self._wait(eng, (("c", e), c))
        if self.xcnt > 0:
            self._wait(eng, (("x", 0), self.xcnt))


D = 2048
KD = 16
NLAT = 2048
NCTX = 256
NHALO = 128
NTOK0 = NLAT + NCTX
CAP = 1024
HCAP = 512
EPS = 1e-6
LAM_INIT1 = 0.8 - 0.6 * float(np.exp(-0.3 * 1))


class Ctx:
    pass


_UNIQ = [0]


def _T(nc, es, name, shape, dt):
    _UNIQ[0] += 1
    return es.enter_context(nc.sbuf_tensor(f"{name}_{_UNIQ[0]}", shape, dt))


def _PS(nc, es, name, shape, dt=F32):
    _UNIQ[0] += 1
    return es.enter_context(nc.psum_tensor(f"{name}_{_UNIQ[0]}", shape, dt))


def barrier(P):
    for e in ["pe", "act", "dve", "pool", "sp"]:
        P.finish(e)


def load_bcast(P, nc, es, name, src_row_ap, n=D, q="sp"):
    t = _T(nc, es, name, [128, n], F32)
    P.dma(q, lambda e: e.dma_start(out=t[:], in_=src_row_ap.partition_broadcast(128)), writes=[name])
    return t


def norm_block(P, K, src_rows, A, Bt, xf, ub, sq, st, uT=None, uT_off=0, psT=None, tag=""):
    A_t, A_k = A
    B_t, B_k = Bt
    xk, uk, sk = xf[1], ub[1], st[1]
    P.dma("sp", lambda e: e.dma_start(out=xf[0][:], in_=src_rows), writes=[xk])
    P.op("act", lambda e: e.activation(out=sq[0][:], in_=xf[0][:], func=AF.Square, accum_out=st[0][:, 0:1]),
         reads=[xk], writes=[sq[1], sk])
    P.op("act", lambda e: e.activation(out=st[0][:, 1:2], in_=st[0][:, 0:1], func=AF.Sqrt, scale=1.0 / D, bias=K.epsb[:, 0:1]),
         reads=[sk], writes=[sk])
    P.op("dve", lambda e: e.reciprocal(out=st[0][:, 2:3], in_=st[0][:, 1:2]), reads=[sk], writes=[sk])
    P.op("dve", lambda e: e.scalar_tensor_tensor(out=xf[0][:], in0=xf[0][:], scalar=st[0][:, 2:3], in1=A_t[:],
                                                  op0=ALU.mult, op1=ALU.mult), reads=[xk, sk, A_k], writes=[xk])
    P.op("pool", lambda e: e.tensor_tensor(out=ub[0][:], in0=xf[0][:], in1=B_t[:], op=ALU.add), reads=[xk, B_k], writes=[uk])
    if uT is not None:
        for half in range(2):
            pk = psT[half][1]

            def tr(e, half=half):
                for j in range(8):
                    k = half * 8 + j
                    i = e.transpose(out=psT[half][0][:, j, :], in_=ub[0][:, k * 128:(k + 1) * 128], identity=K.ident[:])
                return i
            P.op("pe", tr, reads=[uk], writes=[pk])
            eng = "act" if half == 0 else "dve"
            if eng == "act":
                P.op("act", lambda e, half=half: e.copy(out=uT[0][:, half * 8:(half + 1) * 8, uT_off:uT_off + 128], in_=psT[half][0][:]),
                     reads=[pk], writes=[uT[1]])
            else:
                P.op("dve", lambda e, half=half: e.tensor_copy(out=uT[0][:, half * 8:(half + 1) * 8, uT_off:uT_off + 128], in_=psT[half][0][:]),
                     reads=[pk], writes=[uT[1]])


def phase_mod(P, nc, K, io):
    es = ExitStack()
    K.mod_es = es
    cT = _T(nc, es, "m_cT", [128, KD, 2], F32)
    sT = _T(nc, es, "m_sT", [128, KD, 2], BF16)
    P.dma("sp", lambda e: e.dma_start(out=cT[:], in_=io.condT), writes=["m_cT"])
    P.op("act", lambda e: e.activation(out=sT[:], in_=cT[:], func=AF.Silu), reads=["m_cT"], writes=["m_sT"])
    wm = [_T(nc, es, f"m_w{i}", [128, KD, 512], BF16) for i in range(2)]
    bm = [_T(nc, es, f"m_b{i}", [2, 512], F32) for i in range(2)]
    ob = [_T(nc, es, f"m_o{i}", [2, 512], F32) for i in range(2)]
    ps = [_PS(nc, es, f"m_ps{i}", [2, 512]) for i in range(2)]
    cnt = [0]

    def work(l, cb):
        it = cnt[0]
        cnt[0] += 1
        wl = io.w_mod[l].rearrange("(k p) n -> p k n", p=128)
        w = wm[it % 2]
        wk = f"m_w{it % 2}"
        j = it % 2
        P.dma("pool", lambda e: e.dma_start(out=w[:], in_=wl[:, :, cb * 512:(cb + 1) * 512]), writes=[wk])
        P.dma("sp", lambda e: e.dma_start(out=bm[j][:], in_=io.b_mod[l:l + 1, cb * 512:(cb + 1) * 512].partition_broadcast(2)),
              writes=[f"m_b{j}"])

        def mm(e):
            for k in range(KD):
                i = e.matmul(ps[j][:], lhsT=sT[:, k, :], rhs=w[:, k, :], start=(k == 0), stop=(k == KD - 1))
            return i
        P.op("pe", mm, reads=[wk, "m_sT"], writes=[f"m_ps{j}"])
        P.op("dve", lambda e: e.tensor_tensor(out=ob[j][:], in0=ps[j][:], in1=bm[j][:], op=ALU.add),
             reads=[f"m_ps{j}", f"m_b{j}"], writes=[f"m_o{j}"])
        P.dma("sp", lambda e: e.dma_start(out=io.modv[l, :, cb * 512:(cb + 1) * 512], in_=ob[j][:]),
              reads=[f"m_o{j}"], writes=["modv"])
    for cb in range(24):
        work(0, cb)
    K.modq = [(lambda cb=cb: work(1, cb)) for cb in range(24)]
    barrier(P)


def drain_mod(P, K, n):
    q = getattr(K, "modq", None)
    while q and n > 0:
        q.pop(0)()
        n -= 1


def make_AB(P, nc, es, K, io, l, which, cond, pfx):
    base = which * 3 * D
    A = load_bcast(P, nc, es, pfx + "A", io.modv[l, cond:cond + 1, base + D:base + 2 * D])
    Bt = load_bcast(P, nc, es, pfx + "B", io.modv[l, cond:cond + 1, base:base + D])
    nw = load_bcast(P, nc, es, pfx + "nw", (io.norm1_w if which == 0 else io.norm2_w)[l:l + 1, :])
    P.op("dve", lambda e: e.scalar_tensor_tensor(out=A[:], in0=A[:], scalar=1.0, in1=nw[:], op0=ALU.add, op1=ALU.mult),
         reads=[pfx + "A", pfx + "nw"], writes=[pfx + "A"])
    return (A, pfx + "A"), (Bt, pfx + "B")


def phase_l0a(P, nc, K, io):
    NT = NLAT + NCTX + NHALO
    with ExitStack() as es:
        uT = (_T(nc, es, "a_uT", [128, KD, NT], BF16), "a_uT")
        with ExitStack() as es2:
            Al, Bl = make_AB(P, nc, es2, K, io, 0, 0, 0, "a_l")
            Ac, Bc = make_AB(P, nc, es2, K, io, 0, 0, 1, "a_c")
            xf = [(_T(nc, es2, f"a_xf{i}", [128, D], F32), f"a_xf{i}") for i in range(2)]
            ub = [(_T(nc, es2, f"a_ub{i}", [128, D], BF16), f"a_ub{i}") for i in range(2)]
            sq = (_T(nc, es2, "a_sq", [128, D], BF16), "a_sq")
            st = [(_T(nc, es2, f"a_st{i}", [128, 4], F32), f"a_st{i}") for i in range(2)]
            psT = [(_PS(nc, es2, f"a_pT{i}", [128, 8, 128], BF16), f"a_pT{i}") for i in range(2)]
            nb = NT // 128
            for b in range(nb):
                if b < 16:
                    src, A, Bt = io.x[b * 128:(b + 1) * 128, :], Al, Bl
                elif b < 18:
                    src, A, Bt = io.ctx[(b - 16) * 128:(b - 15) * 128, :], Ac, Bc
                else:
                    src, A, Bt = io.xh[:, :], Al, Bl
                norm_block(P, K, src, A, Bt, xf[b % 2], ub[b % 2], sq, st[b % 2], uT=uT, uT_off=b * 128, psT=psT)
            barrier(P)
        win = io.even_w_in.rearrange("(k p) n -> p k n", p=128)
        wt = [(_T(nc, es, f"a_w{i}", [128, KD, 128], BF16), f"a_w{i}") for i in range(4)]
        pp = [(_PS(nc, es, f"a_pp{i}", [128, 512]), f"a_pp{i}") for i in range(6)]
        cvl = _T(nc, es, "a_cvl", [128, NLAT + 2], F32)
        cvc = _T(nc, es, "a_cvc", [128, NCTX + 2], F32)
        bg = _T(nc, es, "a_bg", [128, NLAT + NCTX], F32)
        cs = [(_T(nc, es, f"a_cs{i}", [128, 512], F32), f"a_cs{i}") for i in range(2)]
        t1 = _T(nc, es, "a_t1", [128, NLAT + NCTX], F32)
        yb = _T(nc, es, "a_yb", [128, NLAT + NCTX], BF16)
        cw = _T(nc, es, "a_cw", [128, 8, 3], F32)
        P.dma("sp", lambda e: e.dma_start(out=cw[:], in_=io.conv_wT), writes=["a_cw"])
        P.op("pool", lambda e: e.memset(cvl[:], 0.0), writes=["a_cvl"])
        P.op("pool", lambda e: e.memset(cvc[:], 0.0), writes=["a_cvc"])
        tblocks = [(0, 512), (512, 512), (1024, 512), (1536, 512), (2048, 256), (2304, 128)]
        wi = 0
        ppi = 0
        fT = _T(nc, es, "a_fT", [128, 2, NLAT + NCTX], BF16)
        csd = _T(nc, es, "a_csd", [128, 2, 512], BF16)
        P.dma("sp", lambda e: e.dma_start(out=csd[:], in_=io.cs256), writes=["a_csd"])
        abt = [(_T(nc, es, f"a_ab{i}", [128, 512], BF16), f"a_ab{i}") for i in range(2)]
        for g in range(4):
            for hh in range(2):
                w, wk = wt[wi % 4]
                wi += 1
                col = 3072 + g * 256 + hh * 128
                P.dma("pool", lambda e, w=w, col=col: e.dma_start(out=w[:], in_=win[:, :, col:col + 128]), writes=[wk])
                for ti, (t0, tn) in enumerate(tblocks[:5]):
                    p_, pk = pp[ppi % 6]
                    ppi += 1

                    def mm(e, p_=p_, w=w):
                        for k in range(KD):
                            i = e.matmul(p_[:, 0:tn], lhsT=w[:, k, :], rhs=uT[0][:, k, t0:t0 + tn], start=(k == 0), stop=(k == KD - 1))
                        return i
                    P.op("pe", mm, reads=[wk, "a_uT"], writes=[pk])
                    P.op("act", lambda e: e.copy(out=fT[:, hh, t0:t0 + tn], in_=p_[:, 0:tn]), reads=[pk], writes=["a_fT"])
            for b in range(18):
                p_, pk = pp[ppi % 6]
                ppi += 1

                def mm2(e, p_=p_):
                    for hh in range(2):
                        i = e.matmul(p_[:], lhsT=fT[:, hh, b * 128:(b + 1) * 128], rhs=csd[:, hh, :], start=(hh == 0), stop=(hh == 1))
                    return i
                P.op("pe", mm2, reads=["a_fT", "a_csd"], writes=[pk])
                a_, ak = abt[b % 2]
                if b % 2 == 0:
                    P.op("act", lambda e: e.copy(out=a_[:], in_=p_[:]), reads=[pk], writes=[ak])
                else:
                    P.op("dve", lambda e: e.tensor_copy(out=a_[:], in_=p_[:]), reads=[pk], writes=[ak])
                if b < 16:
                    P.dma("sp", lambda e: e.dma_start(out=io.ab_own[g // 2, b * 128:(b + 1) * 128, (g % 2) * 512:(g % 2 + 1) * 512], in_=a_[:]),
                          reads=[ak], writes=["ab_own"])
                else:
                    P.dma("sp", lambda e: e.dma_start(out=io.ab_ctx[(b - 16) * 128:(b - 15) * 128, g * 512:(g + 1) * 512], in_=a_[:]),
                          reads=[ak], writes=["ab_ctx"])
        pend = _gather_thunks(P, io.ab_own.rearrange("g t n -> (g t) n"), io.ab4, 4096, 512, "ab_own", "ab4") if io.mode == "ALL" else []
        for c in range(8):
            ws = []
            for part in range(3):
                w, wk = wt[wi % 4]
                wi += 1
                col = part * 1024 + c * 128
                P.dma("pool", lambda e, w=w, col=col: e.dma_start(out=w[:], in_=win[:, :, col:col + 128]), writes=[wk])
                ws.append((w, wk))
            if pend:
                pend.pop(0)()
            drain_mod(P, K, 3)
            for ti, (t0, tn) in enumerate(tblocks):
                pb = []
                for part in range(3):
                    p_, pk = pp[ppi % 6]
                    ppi += 1
                    w, wk = ws[part]

                    def mm(e, p_=p_, w=w):
                        for k in range(KD):
                            i = e.matmul(p_[:, 0:tn], lhsT=w[:, k, :], rhs=uT[0][:, k, t0:t0 + tn], start=(k == 0), stop=(k == KD - 1))
                        return i
                    P.op("pe", mm, reads=[wk, "a_uT"], writes=[pk])
                    pb.append((p_, pk))
                c_, ck = cs[ti % 2]
                P.op("act", lambda e: e.copy(out=c_[:, 0:tn], in_=pb[1][0][:, 0:tn]), reads=[pb[1][1]], writes=[ck])
                if ti < 4:
                    dst = cvl[:, 1 + t0:1 + t0 + tn]
                    dk = "a_cvl"
                elif ti == 4:
                    dst = cvc[:, 1:1 + tn]
                    dk = "a_cvc"
                else:
                    dst = None
                if dst is not None:
                    P.op("dve", lambda e: e.tensor_tensor(out=dst, in0=c_[:, 0:tn], in1=pb[2][0][:, 0:tn], op=ALU.mult),
                         reads=[ck, pb[2][1]], writes=[dk])
                    P.op("act", lambda e: e.copy(out=bg[:, t0:t0 + tn], in_=pb[0][0][:, 0:tn]), reads=[pb[0][1]], writes=["a_bg"])
                else:
                    P.op("dve", lambda e: e.tensor_tensor(out=c_[:, 0:1], in0=c_[:, 0:1], in1=pb[2][0][:, 0:1], op=ALU.mult),
                         reads=[ck, pb[2][1]], writes=[ck])
                    P.op("dve", lambda e: e.tensor_scalar(out=cvl[:, 0:1], in0=c_[:, 0:1], scalar1=K.flags[:, 0:1], scalar2=None, op0=ALU.mult),
                         reads=[ck], writes=["a_cvl"])
                    P.op("dve", lambda e: e.tensor_scalar(out=cvl[:, NLAT + 1:NLAT + 2], in0=c_[:, 0:1], scalar1=K.flags[:, 1:2], scalar2=None, op0=ALU.mult),
                         reads=[ck], writes=["a_cvl"])
            for (cv, cvk, o0, n) in [(cvl, "a_cvl", 0, NLAT), (cvc, "a_cvc", NLAT, NCTX)]:
                tt = t1[:, o0:o0 + n]
                P.op("dve", lambda e: e.tensor_scalar(out=tt, in0=cv[:, 1:n + 1], scalar1=cw[:, c, 1:2], scalar2=None, op0=ALU.mult),
                     reads=[cvk, "a_cw"], writes=["a_t1"])
                P.op("dve", lambda e: e.scalar_tensor_tensor(out=tt, in0=cv[:, 0:n], scalar=cw[:, c, 0:1], in1=tt, op0=ALU.mult, op1=ALU.add),
                     reads=[cvk, "a_t1"], writes=["a_t1"])
                P.op("dve", lambda e: e.scalar_tensor_tensor(out=tt, in0=cv[:, 2:n + 2], scalar=cw[:, c, 2:3], in1=tt, op0=ALU.mult, op1=ALU.add),
                     reads=[cvk, "a_t1"], writes=["a_t1"])
            P.op("pool", lambda e: e.tensor_tensor(out=yb[:], in0=t1[:], in1=bg[:], op=ALU.mult), reads=["a_t1", "a_bg"], writes=["a_yb"])
            P.dma("sp", lambda e: e.dma_start(out=io.ycT[c], in_=yb[:]), reads=["a_yb"], writes=["ycT"])
    drain_mod(P, K, 100)
    barrier(P)
    if getattr(K, "mod_es", None) is not None:
        K.mod_es.close()
        K.mod_es = None


PHASES_OF = {"A": ["mod", "l0a"], "B": ["l0b", "op0", "moe0", "l1a"], "C": ["attn", "op1", "moe1"],
             "ALL": ["mod", "l0a", "xab", "l0b", "op0", "moe0", "l1a", "xkv", "attn", "op1", "moe1"]}
LAUNCH_OF = {"mod": "A", "l0a": "A", "l0b": "B", "op0": "B", "moe0": "B", "l1a": "B", "attn": "C", "op1": "C", "moe1": "C"}


def declare(nc, mode):
    io = Ctx()
    io.ext_in, io.ext_out = [], []

    def inp(name, shape, dt, launches):
        if mode != "ALL" and mode not in launches:
            return None
        io.ext_in.append(name)
        return nc.dram_tensor(name, shape, dt, kind="ExternalInput").ap()

    def mid(name, shape, dt, prod, cons, scratch=""):
        if mode == "ALL" or mode in scratch:
            return nc.dram_tensor(name, shape, dt, kind="Internal").ap()
        if mode == prod:
            io.ext_out.append(name)
            return nc.dram_tensor(name, shape, dt, kind="ExternalOutput").ap()
        if mode in cons:
            io.ext_in.append(name)
            return nc.dram_tensor(name, shape, dt, kind="ExternalInput").ap()
        return None

    io.x = inp("x", [NLAT, D], F32, "AB")
    io.ctx = inp("ctx", [NCTX, D], F32, "AB")
    io.xh = inp("xh", [128, D], F32, "A")
    io.condT = inp("condT", [128, KD, 2], F32, "A")
    io.flags = inp("flags", [128, 2], F32, "A")
    io.w_mod = inp("w_mod", [2, D, 6 * D], F32, "A")
    io.b_mod = inp("b_mod", [2, 6 * D], F32, "A")
    io.norm1_w = inp("norm1_w", [2, D], F32, "AB")
    io.norm2_w = inp("norm2_w", [2, D], F32, "BC")
    io.even_w_in = inp("even_w_in", [D, 4096], F32, "A")
    io.conv_wT = inp("conv_wT", [128, 8, 3], F32, "A")
    io.even_w_out = inp("even_w_out", [D, D], F32, "B")
    io.odd_w_qkv = inp("odd_w_qkv", [D, 6144], F32, "B")
    io.qk_norm = inp("qk_norm", [128, 2], F32, "B")
    io.lam_vecs = inp("lam_vecs", [1, 4, 128], F32, "C")
    io.subln_wT = inp("subln_wT", [128, 2], F32, "C")
    io.odd_w_out = inp("odd_w_out", [D, D], F32, "C")
    io.moe_wr = [inp(f"moe_wr{l}", [D, 20], F32, "BC"[l]) for l in range(2)]
    io.moe_br = inp("moe_br", [2, 20], F32, "BC")
    io.moe_w_gate = [inp(f"moe_w_gate{l}", [16, D, 512], F32, "BC"[l]) for l in range(2)]
    io.moe_w_up = [inp(f"moe_w_up{l}", [16, D, 512], F32, "BC"[l]) for l in range(2)]
    io.moe_w_down = [inp(f"moe_w_down{l}", [16, 512, D], F32, "BC"[l]) for l in range(2)]
    io.ident = inp("ident", [128, 128], BF16, "ABC")
    io.cs256 = inp("cs256", [128, 2, 512], BF16, "A")
    io.dft_c = inp("dft_c", [4096, NLAT], BF16, "B")
    io.dft_s = inp("dft_s", [4096, NLAT], BF16, "B")
    io.dft256 = inp("dft256", [128, 2, 2, 256], BF16, "B")
    io.rope_cs = inp("rope_cs", [2, 128, NLAT], F32, "B")
    io.rotT = inp("rotT", [128, 128], BF16, "B")
    io.tri = inp("tri", [128, 128], BF16, "BC")
    io.modv = mid("modv", [2, 2, 6 * D], F32, "A", "BC")
    io.ycT = mid("ycT", [8, 128, NLAT + NCTX], BF16, "A", "B")
    io.ab_own = mid("ab_own", [2, NLAT, 1024], BF16, "A", "")
    io.ab_ctx = mid("ab_ctx", [NCTX, D], BF16, "A", "B")
    io.ab_full = mid("ab_full", [2, 2 * NLAT, 1024], BF16, "X", "B") if mode != "ALL" else None
    io.mode = mode
    if mode == "ALL":
        io.ab4 = nc.dram_tensor("ab4", [4 * 2 * NLAT, 1024], BF16, kind="Internal").ap()
        io.k4 = nc.dram_tensor("k4", [4 * 16 * 128, NLAT], BF16, kind="Internal").ap()
        io.v4 = nc.dram_tensor("v4", [4 * 4 * NLAT, 512], BF16, kind="Internal").ap()
    io.idx_ab = inp("idx_ab", [128, 64], I32, "")
    io.idx_k = inp("idx_k", [128, 32], I32, "")
    io.idx_v = inp("idx_v", [128, 128], I32, "")
    io.yfT = mid("yfT", [8, 128, NLAT + NCTX], BF16, "-", "", "B")
    io.h_lat = mid("h_lat", [NLAT, D], F32, "B", "C")
    io.h_ctx = mid("h_ctx", [NCTX, D], F32, "-", "", "B")
    io.h_mid = mid("h_mid", [NLAT, D], F32, "-", "", "BC")
    io.xg = mid("xg", [16 * CAP, D], BF16, "-", "", "BC")
    io.yg = mid("yg", [16 * CAP, D], BF16, "-", "", "BC")
    io.qT = mid("qT", [16, 128, NLAT], BF16, "B", "C")
    io.kT_own = mid("kT_own", [16, 128, NLAT], BF16, "B", "")
    io.kT_ctx = mid("kT_ctx", [16, 128, NCTX], BF16, "B", "C")
    io.v_own = mid("v_own", [4, NLAT, 512], BF16, "B", "")
    io.v_ctx = mid("v_ctx", [NCTX, D], BF16, "B", "C")
    io.kT_full = mid("kT_full", [16, 128, 2 * NLAT], BF16, "X", "C") if mode != "ALL" else None
    io.v_full = mid("v_full", [4, 2 * NLAT, 512], BF16, "X", "C") if mode != "ALL" else None
    io.oT = mid("oT", [16, 128, NLAT], BF16, "-", "", "C")
    io.out = mid("out", [NLAT, D], F32, "C", "") if mode != "ALL" else None
    io.dbg = None
    if mode == "ALL":
        io.ext_out.append("dbg")
        io.dbg = nc.dram_tensor("dbg", [2, 128, 16], F32, kind="ExternalOutput").ap()
        io.ext_out.append("out")
        io.out = nc.dram_tensor("out", [NLAT, D], F32, kind="ExternalOutput").ap()
    return io


def build(mode, phases=None):
    nc = bass.Bass("TRN2", target_bir_lowering=False)
    io = declare(nc, mode)
    for nm in ["xg", "yg", "yfT", "oT", "h_ctx", "ab_own", "kT_own", "v_own"]:
        if getattr(io, nm) is None and mode in "BC":
            pass
    es = ExitStack()
    with es:
        P = Prog(nc, es)
        K = Ctx()
        K.ident = _T(nc, es, "k_ident", [128, 128], BF16)
        K.epsb = _T(nc, es, "k_eps", [128, 1], F32)
        K.ones = _T(nc, es, "k_ones", [128, 128], BF16)
        K.flags = _T(nc, es, "k_flags", [128, 2], F32)
        P.dma("sp", lambda e: e.dma_start(out=K.ident[:], in_=io.ident), writes=["k_ident"])
        P.op("pool", lambda e: e.memset(K.epsb[:], EPS), writes=["k_eps"])
        P.op("pool", lambda e: e.memset(K.ones[:], 1.0), writes=["k_ones"])
        if io.flags is not None:
            P.dma("sp", lambda e: e.dma_start(out=K.flags[:], in_=io.flags), writes=["k_flags"])
        K.breg = nc.gpsimd.to_reg(16 * CAP - 1)
        K.breg_big = nc.gpsimd.to_reg(4 * 16 * 128 * 16 - 1)
        barrier(P)
        for ph in (phases or PHASES_OF[mode]):
            PHASE_FN[ph](P, nc, K, io)
            barrier(P)

        print(f"[build {mode}] waits={P.n_wait} counts={P.ccnt}")
    return nc, io


PHASE_FN = {"mod": phase_mod, "l0a": phase_l0a}


def phase_l0b(P, nc, K, io):
    with ExitStack() as es:
        abps = [_T(nc, es, f"b_abp{i}", [128, 32, 1024], BF16) for i in range(2)]
        dc = [(_T(nc, es, f"b_dc{i}", [128, 8, 512], BF16), f"b_dc{i}") for i in range(2)]
        ds = [(_T(nc, es, f"b_ds{i}", [128, 8, 512], BF16), f"b_ds{i}") for i in range(2)]
        acc = [(_PS(nc, es, f"b_acc{i}", [128, 512]), f"b_acc{i}") for i in range(8)]
        yo = [(_T(nc, es, f"b_yo{i}", [128, 512], BF16), f"b_yo{i}") for i in range(2)]
        if io.mode == "ALL":
            ixab = _T(nc, es, "b_ixab", [128, 64], I32)
            P.dma("sp", lambda e: e.dma_start(out=ixab[:], in_=io.idx_ab), writes=["b_ixab"])
        dcv = io.dft_c.rearrange("(c p) n -> p c n", p=128)
        dsv = io.dft_s.rearrange("(c p) n -> p c n", p=128)
        di = 0
        yi = 0
        ai = 0
        for gp in range(2):
            abp = abps[gp]
            if io.mode == "ALL" and gp == 0:
              for gq in range(2):
                for c in range(32):
                    P.dma("pool", lambda e: e.indirect_dma_start(out=abps[gq][:, c, :], out_offset=None, in_=io.ab4,
                                                                  in_offset=bass.IndirectOffsetOnAxis(ap=ixab[:, gq * 32 + c:gq * 32 + c + 1], axis=0),
                                                                  bounds_check=K.breg_big, oob_is_err=False), reads=[f"ab4_{q_}" for q_ in range(8)] + ["b_ixab"], writes=[f"b_abp{gq}"])
            elif io.mode != "ALL":
                abv = io.ab_full[gp].rearrange("(c p) n -> p c n", p=128)
                for q4 in range(4):
                    P.dma("sp", lambda e: e.dma_start(out=abp[:, q4 * 8:(q4 + 1) * 8, :], in_=abv[:, q4 * 8:(q4 + 1) * 8, :]), writes=[f"b_abp{gp}"])
            for nb in range(4):
                banks = [acc[(ai + a) % 8] for a in range(4)]
                ai += 4
                for tq in range(4):
                    c_, ck = dc[di % 2]
                    s_, sk = ds[di % 2]
                    di += 1
                    P.dma("sp", lambda e: e.dma_start(out=c_[:], in_=dcv[:, tq * 8:(tq + 1) * 8, nb * 512:(nb + 1) * 512]), writes=[ck])
                    P.dma("sp", lambda e: e.dma_start(out=s_[:], in_=dsv[:, tq * 8:(tq + 1) * 8, nb * 512:(nb + 1) * 512]), writes=[sk])

                    def mm(e):
                        for tc in range(8):
                            t = tq * 8 + tc
                            for a in range(4):
                                gl, hh = a // 2, a % 2
                                first = (tq == 0 and tc == 0)
                                last = (tq == 3 and tc == 7)
                                e.matmul(banks[a][0][:], lhsT=abp[:, t, gl * 512 + hh * 128:gl * 512 + hh * 128 + 128], rhs=c_[:, tc, :],
                                         start=first, stop=False)
                                i = e.matmul(banks[a][0][:], lhsT=abp[:, t, gl * 512 + 256 + hh * 128:gl * 512 + 256 + hh * 128 + 128], rhs=s_[:, tc, :],
                                             start=False, stop=last)
                        return i
                    P.op("pe", mm, reads=[f"b_abp{gp}", ck, sk], writes=[b[1] for b in banks])
                for a in range(4):
                    y_, yk = yo[yi % 2]
                    yi += 1
                    if a % 2 == 0:
                        P.op("act", lambda e: e.copy(out=y_[:], in_=banks[a][0][:]), reads=[banks[a][1]], writes=[yk])
                    else:
                        P.op("dve", lambda e: e.tensor_copy(out=y_[:], in_=banks[a][0][:]), reads=[banks[a][1]], writes=[yk])
                    ch = (gp * 2 + a // 2) * 2 + a % 2
                    P.dma("sp", lambda e: e.dma_start(out=io.yfT[ch, :, nb * 512:(nb + 1) * 512], in_=y_[:]), reads=[yk], writes=["yfT"])
        barrier(P)
        abc = _T(nc, es, "b_abc", [128, 2, D], BF16)
        d2 = _T(nc, es, "b_d2", [128, 2, 2, 256], BF16)
        P.dma("sp", lambda e: e.dma_start(out=abc[:], in_=io.ab_ctx.rearrange("(c p) n -> p c n", p=128)), writes=["b_abc"])
        P.dma("sp", lambda e: e.dma_start(out=d2[:], in_=io.dft256), writes=["b_d2"])
        for ch in range(8):
            g, hh = ch // 2, ch % 2
            bk, bkk = acc[ch % 8]

            def mmc(e):
                n = 0
                for t in range(2):
                    for part in range(2):
                        i = e.matmul(bk[:, 0:256], lhsT=abc[:, t, g * 512 + part * 256 + hh * 128:g * 512 + part * 256 + hh * 128 + 128],
                                     rhs=d2[:, t, part, :], start=(n == 0), stop=(n == 3))
                        n += 1
                return i
            P.op("pe", mmc, reads=["b_abc", "b_d2"], writes=[bkk])
            y_, yk = yo[ch % 2]
            P.op("act", lambda e: e.copy(out=y_[:, 0:256], in_=bk[:, 0:256]), reads=[bkk], writes=[yk])
            P.dma("sp", lambda e: e.dma_start(out=io.yfT[ch, :, NLAT:NLAT + NCTX], in_=y_[:, 0:256]), reads=[yk], writes=["yfT"])
    barrier(P)


def phase_op(P, nc, K, io, l, srcs, w_dram, segs):
    with ExitStack() as es:
        wo = _T(nc, es, "o_w", [128, KD, D], BF16)
        wv = w_dram.rearrange("(k p) n -> p k n", p=128)
        for q4 in range(4):
            P.dma("pool", lambda e: e.dma_start(out=wo[:, q4 * 4:(q4 + 1) * 4, :], in_=wv[:, q4 * 4:(q4 + 1) * 4, :]), writes=["o_w"])
        Gs = {}
        for cond in sorted(set(s[2] for s in segs)):
            Gs[cond] = load_bcast(P, nc, es, f"o_G{cond}", io.modv[l, cond:cond + 1, 2 * D:3 * D])
        yt = [(_T(nc, es, f"o_yt{i}", [128, KD, 512], BF16), f"o_yt{i}") for i in range(2)]
        hx = [(_T(nc, es, f"o_hx{i}", [128, D], F32), f"o_hx{i}") for i in range(2)]
        mg = [(_T(nc, es, f"o_mg{i}", [128, D], F32), f"o_mg{i}") for i in range(2)]
        ps = [(_PS(nc, es, f"o_ps{i}", [128, 512]), f"o_ps{i}") for i in range(8)]
        si = 0
        bi = 0
        pi = 0
        for (tok0, ntok, cond, h_src, h_dst) in segs:
            for s0 in range(0, ntok, 512):
                sn = min(512, ntok - s0)
                y_, yk = yt[si % 2]
                si += 1
                for half, src in enumerate(srcs):
                    P.dma("sp", lambda e: e.dma_start(out=y_[:, half * 8:(half + 1) * 8, 0:sn],
                                                      in_=src[:, :, tok0 + s0:tok0 + s0 + sn].rearrange("c p t -> p c t")), writes=[yk])
                for b in range(sn // 128):
                    r0 = s0 + b * 128
                    h_, hk = hx[bi % 2]
                    m_, mk = mg[bi % 2]
                    bi += 1
                    P.dma("sp", lambda e: e.dma_start(out=h_[:], in_=h_src[r0:r0 + 128, :]), writes=[hk])
                    for cb in range(4):
                        p_, pk = ps[pi % 8]
                        pi += 1

                        def mm(e):
                            for c in range(KD):
                                i = e.matmul(p_[:], lhsT=y_[:, c, b * 128:(b + 1) * 128], rhs=wo[:, c, cb * 512:(cb + 1) * 512],
                                             start=(c == 0), stop=(c == KD - 1))
                            return i
                        P.op("pe", mm, reads=[yk, "o_w"], writes=[pk])
                        P.op("dve", lambda e: e.tensor_tensor(out=m_[:, cb * 512:(cb + 1) * 512], in0=p_[:], in1=Gs[cond][:, cb * 512:(cb + 1) * 512], op=ALU.mult),
                             reads=[pk, f"o_G{cond}"], writes=[mk])
                    P.op("pool", lambda e: e.tensor_tensor(out=m_[:], in0=m_[:], in1=h_[:], op=ALU.add), reads=[mk, hk], writes=[mk])
                    P.dma("sp", lambda e: e.dma_start(out=h_dst[r0:r0 + 128, :], in_=m_[:]), reads=[mk], writes=["hdst"])
    barrier(P)


def phase_op0(P, nc, K, io):
    phase_op(P, nc, K, io, 0, [io.ycT, io.yfT], io.even_w_out,
             [(0, NLAT, 0, io.x, io.h_mid), (NLAT, NCTX, 1, io.ctx, io.h_ctx)])


def phase_op1(P, nc, K, io):
    oT = io.oT
    phase_op(P, nc, K, io, 1, [oT[0:8], oT[8:16]], io.odd_w_out, [(0, NLAT, 0, io.h_lat, io.h_mid)])


def phase_moe(P, nc, K, io, l, segs):
    blocks = []
    for (ntok, cond, h_src, h_dst) in segs:
        for b in range(ntok // 128):
            blocks.append((cond, h_src[b * 128:(b + 1) * 128, :], h_dst[b * 128:(b + 1) * 128, :]))
    nb = len(blocks)
    NJ = HCAP // 128
    with ExitStack() as es:
        idx1 = _T(nc, es, "e_idx1", [128, nb], I32)
        idx2 = _T(nc, es, "e_idx2", [128, nb], I32)
        wts = _T(nc, es, "e_wts", [128, 2, nb], F32)
        cnti = _T(nc, es, "e_cnti", [128, 16], I32)
        Gs = {}
        with ExitStack() as es2:
            ABs = {}
            for cond in sorted(set(b[0] for b in blocks)):
                ABs[cond] = make_AB(P, nc, es2, K, io, l, 1, cond, f"e_c{cond}")
            xf = [(_T(nc, es2, f"e_xf{i}", [128, D], F32), f"e_xf{i}") for i in range(2)]
            ub = [(_T(nc, es2, f"e_ub{i}", [128, D], BF16), f"e_ub{i}") for i in range(3)]
            sq = (_T(nc, es2, "e_sq", [128, D], BF16), "e_sq")
            st = [(_T(nc, es2, f"e_st{i}", [128, 4], F32), f"e_st{i}") for i in range(2)]
            vT = [(_T(nc, es2, f"e_vT{i}", [128, KD, 128], BF16), f"e_vT{i}") for i in range(2)]
            psT = [(_PS(nc, es2, f"e_pT{i}", [128, 8, 128], BF16), f"e_pT{i}") for i in range(2)]
            pl = [(_PS(nc, es2, f"e_pl{i}", [128, 64]), f"e_pl{i}") for i in range(2)]
            wr = _T(nc, es2, "e_wr", [128, KD, 20], BF16)
            br = _T(nc, es2, "e_br", [128, 20], F32)
            tri = _T(nc, es2, "e_tri", [128, 128], BF16)
            ecap = _T(nc, es2, "e_ecap", [128, 16], F32)
            base = _T(nc, es2, "e_base", [128, 16], F32)
            P.dma("pool", lambda e: e.dma_start(out=wr[:], in_=io.moe_wr[l].rearrange("(k p) n -> p k n", p=128)), writes=["e_wr"])
            P.dma("sp", lambda e: e.dma_start(out=br[:], in_=io.moe_br[l:l + 1, :].partition_broadcast(128)), writes=["e_br"])
            P.dma("sp", lambda e: e.dma_start(out=tri[:], in_=io.tri), writes=["e_tri"])
            P.op("pool", lambda e: e.iota(ecap[:], pattern=[[CAP, 16]], base=0, channel_multiplier=0, allow_small_or_imprecise_dtypes=True), writes=["e_ecap"])
            P.op("pool", lambda e: e.memset(base[:], 0.0), writes=["e_base"])
            sm = [(_T(nc, es2, f"e_sm{i}", [128, 128], F32), f"e_sm{i}") for i in range(2)]
            ac = [(_T(nc, es2, f"e_ac{i}", [128, 16], BF16), f"e_ac{i}") for i in range(2)]
            def route(bi, cond, src, dst):
                    A, Bt = ABs[cond]
                    u_, uk = ub[bi % 3]
                    v_, vk = vT[bi % 2]
                    norm_block(P, K, src, A, Bt, xf[bi % 2], (u_, uk), sq, st[bi % 2], uT=(v_, vk), uT_off=0, psT=psT)
                    p_, pk = pl[bi % 2]
                    s_, smk = sm[bi % 2]
                    a_, ak = ac[bi % 2]

                    def mm(e):
                        for k in range(KD):
                            i = e.matmul(p_[:, 0:20], lhsT=v_[:, k, :], rhs=wr[:, k, :], start=(k == 0), stop=(k == KD - 1))
                        return i
                    P.op("pe", mm, reads=[vk, "e_wr"], writes=[pk])
                    R = [smk]

                    def dv(fn, extra_r=(), extra_w=()):
                        P.op("dve", fn, reads=R + list(extra_r), writes=R + list(extra_w))
                    lg = s_[:, 0:20]
                    gm, ngm, gs, gw = s_[:, 20:21], s_[:, 21:22], s_[:, 22:23], s_[:, 23:24]
                    mgk = s_[:, 24:28]
                    esel = s_[:, 28:32]
                    m1, m2 = s_[:, 32:33], s_[:, 33:34]
                    o1, o2 = s_[:, 34:38], s_[:, 38:42]
                    es2_ = s_[:, 42:46]
                    dd, ed = s_[:, 46:47], s_[:, 47:48]
                    E1, E2 = s_[:, 48:64], s_[:, 64:80]
                    pos, posc, tmp16 = s_[:, 80:96], s_[:, 96:112], s_[:, 112:128]
                    dv(lambda e: e.tensor_tensor(out=lg, in0=p_[:, 0:20], in1=br[:], op=ALU.add), extra_r=[pk, "e_br"])
                    yield
                    dv(lambda e: e.tensor_reduce(out=gm, in_=s_[:, 0:4], axis=AX.X, op=ALU.max))
                    yield
                    dv(lambda e: e.tensor_scalar(out=ngm, in0=gm, scalar1=-1.0, scalar2=None, op0=ALU.mult))
                    yield
                    P.op("act", lambda e: e.activation(out=tmp16[:, 0:4], in_=s_[:, 0:4], func=AF.Exp, bias=ngm, scale=1.0, accum_out=gs), reads=R, writes=R)
                    yield
                    dv(lambda e: e.reciprocal(out=gw, in_=gs))
                    yield
                    dv(lambda e: e.tensor_scalar(out=mgk, in0=s_[:, 0:4], scalar1=gm, scalar2=None, op0=ALU.is_equal))
                    yield
                    dv(lambda e: e.tensor_scalar(out=esel, in0=s_[:, 4:8], scalar1=s_[:, 24:25], scalar2=None, op0=ALU.mult))
                    yield
                    for g in range(1, 4):
                        dv(lambda e: e.scalar_tensor_tensor(out=esel, in0=s_[:, 4 + 4 * g:8 + 4 * g], scalar=s_[:, 24 + g:25 + g], in1=esel, op0=ALU.mult, op1=ALU.add))
                        yield
                    dv(lambda e: e.tensor_reduce(out=m1, in_=esel, axis=AX.X, op=ALU.max))
                    yield
                    dv(lambda e: e.tensor_scalar(out=o1, in0=esel, scalar1=m1, scalar2=None, op0=ALU.is_equal))
                    yield
                    dv(lambda e: e.scalar_tensor_tensor(out=es2_, in0=o1, scalar=-1.0e30, in1=esel, op0=ALU.mult, op1=ALU.add))
                    yield
                    dv(lambda e: e.tensor_reduce(out=m2, in_=es2_, axis=AX.X, op=ALU.max))
                    yield
                    dv(lambda e: e.tensor_scalar(out=o2, in0=es2_, scalar1=m2, scalar2=None, op0=ALU.is_equal))
                    yield
                    dv(lambda e: e.tensor_tensor(out=dd, in0=m2, in1=m1, op=ALU.subtract))
                    yield
                    P.op("act", lambda e: e.activation(out=ed, in_=dd, func=AF.Exp), reads=R, writes=R)
                    yield
                    dv(lambda e: e.tensor_scalar(out=ed, in0=ed, scalar1=1.0, scalar2=None, op0=ALU.add))
                    yield
                    dv(lambda e: e.reciprocal(out=ed, in_=ed))
                    yield
                    dv(lambda e: e.tensor_tensor(out=wts[:, 0, bi:bi + 1], in0=gw, in1=ed, op=ALU.mult), extra_w=["e_wts"])
                    yield
                    dv(lambda e: e.tensor_tensor(out=wts[:, 1, bi:bi + 1], in0=gw, in1=wts[:, 0, bi:bi + 1], op=ALU.subtract), extra_r=["e_wts"], extra_w=["e_wts"])
                    yield
                    for g in range(4):
                        dv(lambda e: e.tensor_scalar(out=s_[:, 48 + 4 * g:52 + 4 * g], in0=o1, scalar1=s_[:, 24 + g:25 + g], scalar2=None, op0=ALU.mult))
                        yield
                        dv(lambda e: e.tensor_scalar(out=s_[:, 64 + 4 * g:68 + 4 * g], in0=o2, scalar1=s_[:, 24 + g:25 + g], scalar2=None, op0=ALU.mult))
                        yield
                    dv(lambda e: e.tensor_tensor(out=a_[:], in0=E1, in1=E2, op=ALU.add), extra_w=[ak])
                    yield

                    def mm2(e):
                        e.matmul(p_[:, 32:48], lhsT=tri[:], rhs=a_[:], start=True, stop=True)
                        return e.matmul(p_[:, 48:64], lhsT=K.ones[:], rhs=a_[:], start=True, stop=True)
                    P.op("pe", mm2, reads=[ak, "e_tri", "k_ones"], writes=[pk])
                    dv(lambda e: e.tensor_tensor(out=pos, in0=p_[:, 32:48], in1=base[:], op=ALU.add), extra_r=[pk, "e_base"])
                    yield
                    dv(lambda e: e.tensor_tensor(out=base[:], in0=p_[:, 48:64], in1=base[:], op=ALU.add), extra_r=[pk, "e_base"], extra_w=["e_base"])
                    yield
                    dv(lambda e: e.tensor_scalar(out=tmp16, in0=pos, scalar1=float(CAP), scalar2=1.0e6, op0=ALU.is_ge, op1=ALU.mult))
                    yield
                    dv(lambda e: e.tensor_tensor(out=posc, in0=pos, in1=ecap[:], op=ALU.add), extra_r=["e_ecap"])
                    yield
                    dv(lambda e: e.tensor_tensor(out=posc, in0=posc, in1=tmp16, op=ALU.add))
                    yield
                    dv(lambda e: e.tensor_tensor(out=tmp16, in0=posc, in1=E1, op=ALU.mult))
                    yield
                    dv(lambda e: e.tensor_reduce(out=dd, in_=tmp16, axis=AX.X, op=ALU.add))
                    yield
                    dv(lambda e: e.tensor_copy(out=idx1[:, bi:bi + 1], in_=dd), extra_w=[f"e_i1_{bi}"])
                    yield
                    dv(lambda e: e.tensor_tensor(out=tmp16, in0=posc, in1=E2, op=ALU.mult))
                    yield
                    dv(lambda e: e.tensor_reduce(out=dd, in_=tmp16, axis=AX.X, op=ALU.add))
                    yield
                    dv(lambda e: e.tensor_copy(out=idx2[:, bi:bi + 1], in_=dd), extra_w=[f"e_i2_{bi}"])
                    yield
                    for (ix, ik) in [(idx1, f"e_i1_{bi}"), (idx2, f"e_i2_{bi}")]:
                        P.dma("pool", lambda e: e.indirect_dma_start(out=io.xg, out_offset=bass.IndirectOffsetOnAxis(ap=ix[:, bi:bi + 1], axis=0),
                                                                      in_=u_[:], in_offset=None, bounds_check=K.breg, oob_is_err=False),
                              reads=[uk, ik], writes=["xg"])
            gens = [route(bi, cond, src, dst) for bi, (cond, src, dst) in enumerate(blocks)]
            active = []

            def step(g_):
                try:
                    next(g_)
                    return True
                except StopIteration:
                    if g_ in active:
                        active.remove(g_)
                    return False
            while gens or active:
                if gens and len(active) < 2:
                    if active:
                        for _ in range(4):
                            if not step(active[0]):
                                break
                    active.append(gens.pop(0))
                for g_ in list(active):
                    step(g_)
            if io.dbg is not None:
                P.dma("sp", lambda e: e.dma_start(out=io.dbg[l], in_=base[:]), reads=["e_base"], writes=["dbg"])
            P.op("dve", lambda e: e.tensor_copy(out=cnti[:], in_=base[:]), reads=["e_base"], writes=["e_cnti"])
            barrier(P)
        with ExitStack() as es3:
            wg = [(_T(nc, es3, f"e_wg{i}", [128, KD, 512], BF16), f"e_wg{i}") for i in range(2)]
            wu = [(_T(nc, es3, f"e_wu{i}", [128, KD, 512], BF16), f"e_wu{i}") for i in range(2)]
            wd = [(_T(nc, es3, f"e_wd{i}", [128, 4, D], BF16), f"e_wd{i}") for i in range(2)]
            xr = [(_T(nc, es3, f"e_xr{i}", [128, NJ, D], BF16), f"e_xr{i}") for i in range(2)]
            xT = _T(nc, es3, "e_xT", [128, KD, HCAP], BF16)
            aT = _T(nc, es3, "e_aT", [128, 4, HCAP], BF16)
            sg = [(_T(nc, es3, f"e_sg{i}", [128, HCAP], F32), f"e_sg{i}") for i in range(2)]
            yb = [(_T(nc, es3, f"e_yb{i}", [128, D], BF16), f"e_yb{i}") for i in range(2)]
            psT = [(_PS(nc, es3, f"e_qT{i}", [128, 8, 128], BF16), f"e_qT{i}") for i in range(2)]
            pg = [(_PS(nc, es3, f"e_pg{i}", [128, 512]), f"e_pg{i}") for i in range(6)]
            cregs = {"pe": es3.enter_context(nc.tensor.register(f"e_cntp{l}")),
                     "act": es3.enter_context(nc.scalar.register(f"e_cnta{l}")),
                     "dve": es3.enter_context(nc.vector.register(f"e_cntv{l}")),
                     "sp": es3.enter_context(nc.sync.register(f"e_cnts{l}"))}
            pgi = 0
            ybi = 0
            xri = 0
            for ex in range(16):
                g_, gk = wg[ex % 2]
                u_, uk = wu[ex % 2]
                d_, dk = wd[ex % 2]
                gv = io.moe_w_gate[l][ex].rearrange("(k p) n -> p k n", p=128)
                uv = io.moe_w_up[l][ex].rearrange("(k p) n -> p k n", p=128)
                dvw = io.moe_w_down[l][ex].rearrange("(k p) n -> p k n", p=128)
                for hq in range(2):
                    P.dma("pool", lambda e: e.dma_start(out=g_[:, hq * 8:(hq + 1) * 8, :], in_=gv[:, hq * 8:(hq + 1) * 8, :]), writes=[gk])
                    P.dma("pool", lambda e: e.dma_start(out=u_[:, hq * 8:(hq + 1) * 8, :], in_=uv[:, hq * 8:(hq + 1) * 8, :]), writes=[uk])
                    P.dma("pool", lambda e: e.dma_start(out=d_[:, hq * 2:(hq + 1) * 2, :], in_=dvw[:, hq * 2:(hq + 1) * 2, :]), writes=[dk])
                preds = {}
                for en in ["pe", "act", "dve", "sp"]:
                    P._deps(en, ["e_cnti"], [])
                    P.E[en].reg_load(cregs[en], cnti[0:1, ex:ex + 1])
                    preds[en] = P.E[en].snap(cregs[en]) > HCAP
                for hh in range(2):
                    x_, xk = xr[xri % 2]
                    xri += 1
                    r0 = ex * CAP + hh * HCAP

                    def peop(fn, reads, writes):
                        cop("pe", fn, reads, writes)

                    def cop(en, fn, reads, writes):
                        if hh == 0:
                            P.op(en, fn, reads=reads, writes=writes)
                        else:
                            P.op_if(en, preds[en], fn, reads=reads, writes=writes)
                    def cdma(fn, reads, writes):
                        if hh == 0:
                            P.dma("sp", fn, reads=reads, writes=writes)
                        else:
                            P.dma_if("sp", preds["sp"], fn, reads=reads, writes=writes)
                    cdma(lambda e: e.dma_start(out=x_[:], in_=io.xg[r0:r0 + HCAP, :].rearrange("(j p) n -> p j n", p=128)), ["xg"], [xk])
                    for j in range(NJ):
                        if hh == 1:
                            def tr2(e):
                                for k in range(16):
                                    i = e.transpose(out=psT[k // 8][0][:, k % 8, :], in_=x_[:, j, k * 128:(k + 1) * 128], identity=K.ident[:])
                                return i
                            peop(tr2, [xk], [psT[0][1], psT[1][1]])
                        for half in range(2):
                            pt, ptk = psT[half]

                            def tr(e):
                                for jj in range(8):
                                    k = half * 8 + jj
                                    i = e.transpose(out=pt[:, jj, :], in_=x_[:, j, k * 128:(k + 1) * 128], identity=K.ident[:])
                                return i
                            if hh == 0:
                                peop(tr, [xk], [ptk])
                            if half == 0:
                                cop("act", lambda e: e.copy(out=xT[:, 0:8, j * 128:(j + 1) * 128], in_=pt[:]), [ptk], ["e_xT"])
                            else:
                                cop("dve", lambda e: e.tensor_copy(out=xT[:, 8:16, j * 128:(j + 1) * 128], in_=pt[:]), [ptk], ["e_xT"])
                    for fc in range(4):
                        pG, pGk = pg[pgi % 6]
                        pU, pUk = pg[(pgi + 1) % 6]
                        pgi += 2

                        def mmg(e):
                            for k in range(KD):
                                i = e.matmul(pG[:, 0:HCAP], lhsT=g_[:, k, fc * 128:(fc + 1) * 128], rhs=xT[:, k, :], start=(k == 0), stop=(k == KD - 1))
                            return i

                        def mmu(e):
                            for k in range(KD):
                                i = e.matmul(pU[:, 0:HCAP], lhsT=u_[:, k, fc * 128:(fc + 1) * 128], rhs=xT[:, k, :], start=(k == 0), stop=(k == KD - 1))
                            return i
                        if hh == 0:
                            peop(mmg, [gk, "e_xT"], [pGk])
                            peop(mmu, [uk, "e_xT"], [pUk])
                        else:
                            peop(lambda e: (mmg(e), mmu(e))[1], [gk, uk, "e_xT"], [pGk, pUk])
                        s_, sk = sg[fc % 2]
                        cop("act", lambda e: e.activation(out=s_[:], in_=pG[:, 0:HCAP], func=AF.Silu), [pGk], [sk])
                        cop("dve", lambda e: e.tensor_tensor(out=aT[:, fc, :], in0=s_[:], in1=pU[:, 0:HCAP], op=ALU.mult), [sk, pUk], ["e_aT"])
                    for j in range(NJ):
                        y_, yk = yb[ybi % 2]
                        ybi += 1
                        if hh == 1:
                            bks = [pg[(pgi + cb_) % 6] for cb_ in range(4)]

                            def mmd4(e):
                                for cb_ in range(4):
                                    for fc in range(4):
                                        i = e.matmul(bks[cb_][0][:], lhsT=aT[:, fc, j * 128:(j + 1) * 128], rhs=d_[:, fc, cb_ * 512:(cb_ + 1) * 512],
                                                     start=(fc == 0), stop=(fc == 3))
                                return i
                            peop(mmd4, ["e_aT", dk], [b_[1] for b_ in bks])
                        for cb in range(4):
                            pY, pYk = pg[pgi % 6]
                            pgi += 1

                            def mmd(e):
                                for fc in range(4):
                                    i = e.matmul(pY[:], lhsT=aT[:, fc, j * 128:(j + 1) * 128], rhs=d_[:, fc, cb * 512:(cb + 1) * 512], start=(fc == 0), stop=(fc == 3))
                                return i
                            if hh == 0:
                                peop(mmd, ["e_aT", dk], [pYk])
                            if cb % 2 == 0:
                                cop("act", lambda e: e.copy(out=y_[:, cb * 512:(cb + 1) * 512], in_=pY[:]), [pYk], [yk])
                            else:
                                cop("dve", lambda e: e.tensor_copy(out=y_[:, cb * 512:(cb + 1) * 512], in_=pY[:]), [pYk], [yk])
                        cdma(lambda e: e.dma_start(out=io.yg[r0 + j * 128:r0 + (j + 1) * 128, :], in_=y_[:]), [yk], ["yg"])
            barrier(P)
        with ExitStack() as es4:
            for cond in sorted(set(b[0] for b in blocks)):
                Gs[cond] = load_bcast(P, nc, es4, f"e_G{cond}", io.modv[l, cond:cond + 1, 5 * D:6 * D])
            y1 = [(_T(nc, es4, f"e_y1{i}", [128, D], BF16), f"e_y1{i}") for i in range(2)]
            y2 = [(_T(nc, es4, f"e_y2{i}", [128, D], BF16), f"e_y2{i}") for i in range(2)]
            hx = [(_T(nc, es4, f"e_hx{i}", [128, D], F32), f"e_hx{i}") for i in range(2)]
            tt = [(_T(nc, es4, f"e_tt{i}", [128, D], F32), f"e_tt{i}") for i in range(2)]
            t1 = [(_T(nc, es4, f"e_tq{i}", [128, D], F32), f"e_tq{i}") for i in range(2)]
            for i in range(2):
                P.op("pool", lambda e: e.memset(y1[i][0][:], 0.0), writes=[y1[i][1]])
                P.op("pool", lambda e: e.memset(y2[i][0][:], 0.0), writes=[y2[i][1]])
            for bi, (cond, src, dst) in enumerate(blocks):
                a_, ak = y1[bi % 2]
                b_, bk = y2[bi % 2]
                h_, hk = hx[bi % 2]
                t_, tk = tt[bi % 2]
                q_, qk = t1[bi % 2]
                P.dma("pool", lambda e: e.indirect_dma_start(out=a_[:], out_offset=None, in_=io.yg, in_offset=bass.IndirectOffsetOnAxis(ap=idx1[:, bi:bi + 1], axis=0),
                                                              bounds_check=K.breg, oob_is_err=False), reads=["yg", f"e_i1_{bi}"], writes=[ak])
                P.dma("pool", lambda e: e.indirect_dma_start(out=b_[:], out_offset=None, in_=io.yg, in_offset=bass.IndirectOffsetOnAxis(ap=idx2[:, bi:bi + 1], axis=0),
                                                              bounds_check=K.breg, oob_is_err=False), reads=["yg", f"e_i2_{bi}"], writes=[bk])
                P.dma("sp", lambda e: e.dma_start(out=h_[:], in_=src), writes=[hk])
                P.op("act", lambda e: e.activation(out=q_[:], in_=a_[:], func=AF.Identity, scale=wts[:, 0, bi:bi + 1]), reads=[ak, "e_wts"], writes=[qk])
                P.op("dve", lambda e: e.scalar_tensor_tensor(out=t_[:], in0=b_[:], scalar=wts[:, 1, bi:bi + 1], in1=q_[:], op0=ALU.mult, op1=ALU.add),
                     reads=[bk, "e_wts", qk], writes=[tk])
                P.op("dve", lambda e: e.tensor_tensor(out=t_[:], in0=t_[:], in1=Gs[cond][:], op=ALU.mult), reads=[tk, f"e_G{cond}"], writes=[tk])
                P.op("dve", lambda e: e.tensor_tensor(out=t_[:], in0=t_[:], in1=h_[:], op=ALU.add), reads=[tk, hk], writes=[tk])
                P.dma("sp", lambda e: e.dma_start(out=dst, in_=t_[:]), reads=[tk], writes=["hdst"])
    barrier(P)


def phase_moe0(P, nc, K, io):
    phase_moe(P, nc, K, io, 0, [(NLAT, 0, io.h_mid, io.h_lat), (NCTX, 1, io.h_ctx, io.h_ctx)])


def phase_moe1(P, nc, K, io):
    phase_moe(P, nc, K, io, 1, [(NLAT, 0, io.h_mid, io.out)])


PHASE_FN.update({"l0b": phase_l0b, "op0": phase_op0, "op1": phase_op1, "moe0": phase_moe0, "moe1": phase_moe1})


def phase_l1a(P, nc, K, io):
    NT = NLAT + NCTX
    with ExitStack() as es:
        uT = (_T(nc, es, "q_uT", [128, KD, NT], BF16), "q_uT")
        with ExitStack() as es2:
            Al, Bl = make_AB(P, nc, es2, K, io, 1, 0, 0, "q_l")
            Ac, Bc = make_AB(P, nc, es2, K, io, 1, 0, 1, "q_c")
            xf = [(_T(nc, es2, f"q_xf{i}", [128, D], F32), f"q_xf{i}") for i in range(2)]
            ub = [(_T(nc, es2, f"q_ub{i}", [128, D], BF16), f"q_ub{i}") for i in range(2)]
            sq = (_T(nc, es2, "q_sq", [128, D], BF16), "q_sq")
            st = [(_T(nc, es2, f"q_st{i}", [128, 4], F32), f"q_st{i}") for i in range(2)]
            psT = [(_PS(nc, es2, f"q_pT{i}", [128, 8, 128], BF16), f"q_pT{i}") for i in range(2)]
            for b in range(NT // 128):
                if b < 16:
                    src, A, Bt = io.h_lat[b * 128:(b + 1) * 128, :], Al, Bl
                else:
                    src, A, Bt = io.h_ctx[(b - 16) * 128:(b - 15) * 128, :], Ac, Bc
                norm_block(P, K, src, A, Bt, xf[b % 2], ub[b % 2], sq, st[b % 2], uT=uT, uT_off=b * 128, psT=psT)
            barrier(P)
        wv = io.odd_w_qkv.rearrange("(k p) n -> p k n", p=128)
        wt = [(_T(nc, es, f"q_w{i}", [128, KD, 128], BF16), f"q_w{i}") for i in range(3)]
        cosT = _T(nc, es, "q_cos", [128, NLAT], F32)
        sinT = _T(nc, es, "q_sin", [128, NLAT], F32)
        rotT = _T(nc, es, "q_rot", [128, 128], BF16)
        gn = _T(nc, es, "q_gn", [128, 2], F32)
        P.dma("sp", lambda e: e.dma_start(out=cosT[:], in_=io.rope_cs[0]), writes=["q_cos"])
        P.dma("sp", lambda e: e.dma_start(out=sinT[:], in_=io.rope_cs[1]), writes=["q_sin"])
        P.dma("sp", lambda e: e.dma_start(out=rotT[:], in_=io.rotT), writes=["q_rot"])
        P.dma("sp", lambda e: e.dma_start(out=gn[:], in_=io.qk_norm), writes=["q_gn"])
        px = [(_PS(nc, es, f"q_px{i}", [128, 512]), f"q_px{i}") for i in range(3)]
        pss = [(_PS(nc, es, f"q_pss{i}", [128, 512]), f"q_pss{i}") for i in range(2)]
        pr = [(_PS(nc, es, f"q_pr{i}", [128, 512]), f"q_pr{i}") for i in range(2)]
        sqb = [(_T(nc, es, f"q_sqb{i}", [128, 512], BF16), f"q_sqb{i}") for i in range(3)]
        rs = [(_T(nc, es, f"q_rs{i}", [128, 512], F32), f"q_rs{i}") for i in range(2)]
        yb = [(_T(nc, es, f"q_yb{i}", [128, 512], BF16), f"q_yb{i}") for i in range(3)]
        t1 = [(_T(nc, es, f"q_t1{i}", [128, 512], F32), f"q_t1{i}") for i in range(2)]
        t2 = [(_T(nc, es, f"q_t2{i}", [128, 512], F32), f"q_t2{i}") for i in range(2)]
        ob = [(_T(nc, es, f"q_ob{i}", [128, 512], BF16), f"q_ob{i}") for i in range(2)]
        it = 0

        def run_slabs(slabs, pend=()):
            items = []
            for s in slabs:
                isq = s < 16
                tbl = [(0, 512), (512, 512), (1024, 512), (1536, 512)] + ([] if isq else [(2048, 256)])
                for (t0, tn) in tbl:
                    items.append((s, isq, t0, tn))
            n = len(items)
            wcur = {}

            def s1(i):
                s, isq, t0, tn = items[i]
                def ensure(sx):
                    if sx not in wcur:
                        w, wk = wt[sx % 3]
                        col = sx * 128
                        P.dma("pool", lambda e: e.dma_start(out=w[:], in_=wv[:, :, col:col + 128]), writes=[wk])
                        wcur[sx] = (w, wk)
                ensure(s)
                if s + 1 in slabs and (s + 1) not in wcur:
                    ensure(s + 1)
                    if pend:
                        pend.pop(0)()
                w, wk = wcur[s]
                p_, pk = px[i % 3]
                sq_, sqk = sqb[i % 3]

                def mm(e):
                    for k in range(KD):
                        ii = e.matmul(p_[:, 0:tn], lhsT=w[:, k, :], rhs=uT[0][:, k, t0:t0 + tn], start=(k == 0), stop=(k == KD - 1))
                    return ii
                P.op("pe", mm, reads=[wk, "q_uT"], writes=[pk])
                P.op("act", lambda e: e.activation(out=sq_[:, 0:tn], in_=p_[:, 0:tn], func=AF.Square), reads=[pk], writes=[sqk])

            def s2(i):
                s, isq, t0, tn = items[i]
                p_, pk = px[i % 3]
                sq_, sqk = sqb[i % 3]
                ps_, psk = pss[i % 2]
                r_, rk = rs[i % 2]
                y_, yk = yb[i % 3]
                P.op("pe", lambda e: e.matmul(ps_[:, 0:tn], lhsT=K.ones[:], rhs=sq_[:, 0:tn], start=True, stop=True), reads=[sqk, "k_ones"], writes=[psk])
                P.op("act", lambda e: e.activation(out=r_[:, 0:tn], in_=ps_[:, 0:tn], func=AF.Sqrt, scale=1.0 / 128, bias=K.epsb[:, 0:1]), reads=[psk], writes=[rk])
                P.op("dve", lambda e: e.reciprocal(out=r_[:, 0:tn], in_=r_[:, 0:tn]), reads=[rk], writes=[rk])
                gcol = gn[:, 0:1] if isq else gn[:, 1:2]
                P.op("dve", lambda e: e.scalar_tensor_tensor(out=y_[:, 0:tn], in0=p_[:, 0:tn], scalar=gcol, in1=r_[:, 0:tn], op0=ALU.mult, op1=ALU.mult),
                     reads=[pk, rk, "q_gn"], writes=[yk])

            def s3(i):
                s, isq, t0, tn = items[i]
                y_, yk = yb[i % 3]
                pr_, prk = pr[i % 2]
                a_, ak = t1[i % 2]
                b_, bk = t2[i % 2]
                o_, ok = ob[i % 2]
                if t0 < NLAT:
                    P.op("pe", lambda e: e.matmul(pr_[:, 0:tn], lhsT=rotT[:], rhs=y_[:, 0:tn], start=True, stop=True), reads=[yk, "q_rot"], writes=[prk])
                    P.op("pool", lambda e: e.tensor_tensor(out=a_[:, 0:tn], in0=y_[:, 0:tn], in1=cosT[:, t0:t0 + tn], op=ALU.mult), reads=[yk, "q_cos"], writes=[ak])
                    P.op("dve", lambda e: e.tensor_tensor(out=b_[:, 0:tn], in0=pr_[:, 0:tn], in1=sinT[:, t0:t0 + tn], op=ALU.mult), reads=[prk, "q_sin"], writes=[bk])
                    P.op("pool", lambda e: e.tensor_tensor(out=o_[:, 0:tn], in0=a_[:, 0:tn], in1=b_[:, 0:tn], op=ALU.add), reads=[ak, bk], writes=[ok])
                    dst = io.qT[s, :, t0:t0 + tn] if isq else io.kT_own[s - 16, :, t0:t0 + tn]
                    P.dma("sp", lambda e: e.dma_start(out=dst, in_=o_[:, 0:tn]), reads=[ok], writes=["q_out" if isq else "k_out"])
                else:
                    P.dma("sp", lambda e: e.dma_start(out=io.kT_ctx[s - 16, :, 0:tn], in_=y_[:, 0:tn]), reads=[yk], writes=["k_out"])
            for t in range(n + 2):
                if t < n:
                    s1(t)
                if 0 <= t - 1 < n:
                    s2(t - 1)
                if 0 <= t - 2 < n:
                    s3(t - 2)

        run_slabs(list(range(16, 32)))
        wvt = [(_T(nc, es, f"q_wv{i}", [128, KD, 512], BF16), f"q_wv{i}") for i in range(4)]
        for cb in range(4):
            w, wk = wvt[cb]
            for hq in range(2):
                P.dma("pool", lambda e: e.dma_start(out=w[:, hq * 8:(hq + 1) * 8, :], in_=wv[:, hq * 8:(hq + 1) * 8, 4096 + cb * 512:4096 + (cb + 1) * 512]), writes=[wk])
        pend = []
        for cb in range(4):
            w, wk = wvt[cb]
            for _ in range(2):
                if pend:
                    pend.pop(0)()
            for b in range(NT // 128):
                i2 = it % 2
                it += 1
                p_, pk = px[i2]
                o_, ok = ob[i2]

                def mmv(e):
                    for k in range(KD):
                        i = e.matmul(p_[:], lhsT=uT[0][:, k, b * 128:(b + 1) * 128], rhs=w[:, k, :], start=(k == 0), stop=(k == KD - 1))
                    return i
                P.op("pe", mmv, reads=[wk, "q_uT"], writes=[pk])
                if b % 2 == 0:
                    P.op("act", lambda e: e.copy(out=o_[:], in_=p_[:]), reads=[pk], writes=[ok])
                else:
                    P.op("dve", lambda e: e.tensor_copy(out=o_[:], in_=p_[:]), reads=[pk], writes=[ok])
                if b < 16:
                    dst = io.v_own[cb, b * 128:(b + 1) * 128, :]
                else:
                    dst = io.v_ctx[(b - 16) * 128:(b - 15) * 128, cb * 512:(cb + 1) * 512]
                P.dma("sp", lambda e: e.dma_start(out=dst, in_=o_[:]), reads=[ok], writes=["v_out"])
        run_slabs(list(range(16)), pend)
        while pend:
            pend.pop(0)()
    barrier(P)


def phase_attn(P, nc, K, io):
    NK = 2 * NLAT + NCTX
    NKC = NK // 128
    with ExitStack() as es:
        lv = _T(nc, es, "t_lv", [1, 4, 128], F32)
        l1 = _T(nc, es, "t_l1", [1, 8], F32)
        onesf = _T(nc, es, "t_onesf", [1, 128], F32)
        nlam = _T(nc, es, "t_nlam", [128, 1], F32)
        sw = _T(nc, es, "t_sw", [128, 2], F32)
        P.dma("sp", lambda e: e.dma_start(out=lv[:], in_=io.lam_vecs), writes=["t_lv"])
        P.dma("sp", lambda e: e.dma_start(out=sw[:], in_=io.subln_wT), writes=["t_sw"])
        P.op("pool", lambda e: e.memset(onesf[:], 1.0), writes=["t_onesf"])
        P.op("dve", lambda e: e.tensor_tensor(out=lv[:, 0, :], in0=lv[:, 0, :], in1=lv[:, 1, :], op=ALU.mult), reads=["t_lv"], writes=["t_lv"])
        P.op("dve", lambda e: e.tensor_tensor(out=lv[:, 2, :], in0=lv[:, 2, :], in1=lv[:, 3, :], op=ALU.mult), reads=["t_lv"], writes=["t_lv"])
        P.op("dve", lambda e: e.tensor_reduce(out=l1[:, 0:1], in_=lv[:, 0, :], axis=AX.X, op=ALU.add), reads=["t_lv"], writes=["t_l1"])
        P.op("dve", lambda e: e.tensor_reduce(out=l1[:, 1:2], in_=lv[:, 2, :], axis=AX.X, op=ALU.add), reads=["t_lv"], writes=["t_l1"])
        P.op("act", lambda e: e.activation(out=l1[:, 2:4], in_=l1[:, 0:2], func=AF.Exp), reads=["t_l1"], writes=["t_l1"])
        P.op("dve", lambda e: e.tensor_tensor(out=l1[:, 4:5], in0=l1[:, 3:4], in1=l1[:, 2:3], op=ALU.subtract), reads=["t_l1"], writes=["t_l1"])
        P.op("dve", lambda e: e.tensor_scalar(out=l1[:, 4:5], in0=l1[:, 4:5], scalar1=-LAM_INIT1, scalar2=None, op0=ALU.add), reads=["t_l1"], writes=["t_l1"])
        P.op("dve", lambda e: e.tensor_scalar(out=sw[:], in0=sw[:], scalar1=1.0 - LAM_INIT1, scalar2=None, op0=ALU.mult), reads=["t_sw"], writes=["t_sw"])
        kTb = [[_T(nc, es, f"t_kT{b}{c}", [128, NK], BF16) for c in range(2)] for b in range(2)]
        vtb = [_T(nc, es, f"t_v{b}", [128, NKC, 512], BF16) for b in range(2)]
        if io.mode == "ALL":
            ixk = _T(nc, es, "t_ixk", [128, 32], I32)
            ixv = _T(nc, es, "t_ixv", [128, 128], I32)
            P.dma("sp", lambda e: e.dma_start(out=ixk[:], in_=io.idx_k), writes=["t_ixk"])
            P.dma("sp", lambda e: e.dma_start(out=ixv[:], in_=io.idx_v), writes=["t_ixv"])
        qt = [(_T(nc, es, f"t_q{i}", [128, 2, 512], BF16), f"t_q{i}") for i in range(2)]
        pT = [(_T(nc, es, f"t_pT{i}", [128, 512], BF16), f"t_pT{i}") for i in range(3)]
        pS = [(_PS(nc, es, f"t_pS{i}", [128, 512]), f"t_pS{i}") for i in range(2)]
        pO = [[(_PS(nc, es, f"t_pO{c}{j}", [128, 512]), f"t_pO{c}{j}") for j in range(3)] for c in range(2)]
        rc = [_T(nc, es, f"t_rc{c}", [128, 512], F32) for c in range(2)]
        oh = [(_T(nc, es, f"t_oh{i}", [128, 512], F32), f"t_oh{i}") for i in range(2)]
        o2 = [(_T(nc, es, f"t_o2{i}", [128, 512], F32), f"t_o2{i}") for i in range(2)]
        osq = [(_T(nc, es, f"t_osq{i}", [128, 512], BF16), f"t_osq{i}") for i in range(2)]
        rsd = _T(nc, es, "t_rsd", [128, 512], F32)
        oo = [(_T(nc, es, f"t_oo{i}", [128, 512], BF16), f"t_oo{i}") for i in range(2)]
        P.op("pe", lambda e: e.matmul(pS[0][0][:, 0:1], lhsT=onesf[:], rhs=l1[:, 4:5], start=True, stop=True), reads=["t_l1", "t_onesf"], writes=["t_pS0"])
        P.op("dve", lambda e: e.tensor_copy(out=nlam[:], in_=pS[0][0][:, 0:1]), reads=["t_pS0"], writes=["t_nlam"])
        scale = 128 ** -0.5
        qi = 0
        pi = 0
        si = 0
        if io.mode == "ALL":
            kth = _gather_thunks(P, io.kT_own.rearrange("s p t -> (s p) t"), io.k4, 2048, 256, "k_out", "k4")
            vth = _gather_thunks(P, io.v_own.rearrange("g t n -> (g t) n"), io.v4, 8192, 1024, "v_out", "v4")
        else:
            kth, vth = [], []

        def issue_pair_colls(hp):
            if kth and hp < 4:
                for q_ in (2 * hp, 2 * hp + 1):
                    kth[q_]()
                    vth[q_]()

        def load_head(h):
            hp = h // 2
            vt = vtb[hp % 2]
            vk = f"t_v{hp % 2}"
            kT = kTb[h % 2]
            if h % 2 == 0:
                P.dma("sp", lambda e: e.dma_start(out=vt[:, 0:2, :], in_=io.v_ctx[:, hp * 512:(hp + 1) * 512].rearrange("(c p) n -> p c n", p=128)), writes=[vk])
                if io.mode == "ALL":
                    for c32 in range(32):
                        P.dma("pool", lambda e: e.indirect_dma_start(out=vt[:, 2 + c32, :], out_offset=None, in_=io.v4,
                                                                      in_offset=bass.IndirectOffsetOnAxis(ap=ixv[:, hp * 32 + c32:hp * 32 + c32 + 1], axis=0),
                                                                      bounds_check=K.breg_big, oob_is_err=False), reads=[f"v4_{2 * hp + (c32 % 16) // 8}", "t_ixv"], writes=[vk])
                else:
                    for q4 in range(4):
                        P.dma("sp", lambda e: e.dma_start(out=vt[:, 2 + q4 * 8:2 + (q4 + 1) * 8, :],
                                                          in_=io.v_full[hp, q4 * 1024:(q4 + 1) * 1024, :].rearrange("(c p) n -> p c n", p=128)), writes=[vk])
            for c in range(2):
                s = 2 * h + c
                kk = f"t_kT{h % 2}{c}"
                P.dma("sp", lambda e: e.dma_start(out=kT[c][:, 0:NCTX], in_=io.kT_ctx[s]), writes=[kk])
                if io.mode == "ALL":
                    for hf in range(2):
                        P.dma("pool", lambda e: e.indirect_dma_start(out=kT[c][:, NCTX + hf * NLAT:NCTX + (hf + 1) * NLAT], out_offset=None, in_=io.k4,
                                                                      in_offset=bass.IndirectOffsetOnAxis(ap=ixk[:, s * 2 + hf:s * 2 + hf + 1], axis=0),
                                                                      bounds_check=K.breg_big, oob_is_err=False), reads=[f"k4_{h}", "t_ixk"], writes=[kk])
                else:
                    P.dma("sp", lambda e: e.dma_start(out=kT[c][:, NCTX:NK], in_=io.kT_full[s]), writes=[kk])

        issue_pair_colls(0)
        load_head(0)
        for h in range(8):
            hp = h // 2
            vo = (h % 2) * 256
            vt = vtb[hp % 2]
            vk = f"t_v{hp % 2}"
            kT = kTb[h % 2]
            kks = [f"t_kT{h % 2}{c}" for c in range(2)]
            for qb in range(4):
                q_, qk = qt[qi % 2]
                qi += 1
                P.dma("sp", lambda e: e.dma_start(out=q_[:], in_=io.qT[2 * h:2 * h + 2, :, qb * 512:(qb + 1) * 512].rearrange("c p t -> p c t")), writes=[qk])
                steps = [(c, kc) for c in range(2) for kc in range(NKC)]
                bufs = []
                for _ in steps:
                    bufs.append((pS[si % 2], pT[pi % 3]))
                    si += 1
                    pi += 1

                def emit_S(i):
                    c, kc = steps[i]
                    (s_, sk), (p_, pk) = bufs[i]
                    P.op("pe", lambda e: e.matmul(s_[:], lhsT=kT[c][:, kc * 128:(kc + 1) * 128], rhs=q_[:, c, :], start=True, stop=True),
                         reads=[kks[c], qk], writes=[sk])
                    P.op("act", lambda e: e.activation(out=p_[:], in_=s_[:], func=AF.Exp, scale=scale), reads=[sk], writes=[pk])

                def emit_PV(i):
                    c, kc = steps[i]
                    (s_, sk), (p_, pk) = bufs[i]

                    def mmo(e):
                        e.matmul(pO[c][0][0][:], lhsT=vt[:, kc, vo:vo + 128], rhs=p_[:], start=(kc == 0), stop=(kc == NKC - 1))
                        e.matmul(pO[c][1][0][:], lhsT=vt[:, kc, vo + 128:vo + 256], rhs=p_[:], start=(kc == 0), stop=(kc == NKC - 1))
                        return e.matmul(pO[c][2][0][:], lhsT=K.ones[:], rhs=p_[:], start=(kc == 0), stop=(kc == NKC - 1))
                    P.op("pe", mmo, reads=[pk, vk, "k_ones"], writes=[pO[c][0][1], pO[c][1][1], pO[c][2][1]])
                if qb == 1 and h + 1 < 8:
                    load_head(h + 1)
                    if (h + 1) % 2 == 1:
                        issue_pair_colls((h + 1) // 2 + 1)
                emit_S(0)
                for i in range(len(steps)):
                    if i + 1 < len(steps):
                        emit_S(i + 1)
                    emit_PV(i)
                P.op("dve", lambda e: e.reciprocal(out=rc[0][:], in_=pO[0][2][0][:]), reads=[pO[0][2][1]], writes=["t_rc0"])
                P.op("dve", lambda e: e.reciprocal(out=rc[1][:], in_=pO[1][2][0][:]), reads=[pO[1][2][1]], writes=["t_rc1"])
                P.op("dve", lambda e: e.tensor_scalar(out=rc[1][:], in0=rc[1][:], scalar1=nlam[:, 0:1], scalar2=None, op0=ALU.mult), reads=["t_rc1", "t_nlam"], writes=["t_rc1"])
                for hf in range(2):
                    a_, ak = oh[hf]
                    b_, bk = o2[hf]
                    q2_, q2k = osq[hf]
                    P.op("dve", lambda e: e.tensor_tensor(out=a_[:], in0=pO[0][hf][0][:], in1=rc[0][:], op=ALU.mult), reads=[pO[0][hf][1], "t_rc0"], writes=[ak])
                    P.op("dve", lambda e: e.tensor_tensor(out=b_[:], in0=pO[1][hf][0][:], in1=rc[1][:], op=ALU.mult), reads=[pO[1][hf][1], "t_rc1"], writes=[bk])
                    P.op("dve", lambda e: e.tensor_tensor(out=a_[:], in0=a_[:], in1=b_[:], op=ALU.add), reads=[ak, bk], writes=[ak])
                    P.op("act", lambda e: e.activation(out=q2_[:], in_=a_[:], func=AF.Square), reads=[ak], writes=[q2k])
                s_, sk = pS[si % 2]
                si += 1

                def mms(e):
                    e.matmul(s_[:], lhsT=K.ones[:], rhs=osq[0][0][:], start=True, stop=False)
                    return e.matmul(s_[:], lhsT=K.ones[:], rhs=osq[1][0][:], start=False, stop=True)
                P.op("pe", mms, reads=[osq[0][1], osq[1][1], "k_ones"], writes=[sk])
                P.op("act", lambda e: e.activation(out=rsd[:], in_=s_[:], func=AF.Sqrt, scale=1.0 / 256, bias=K.epsb[:, 0:1]), reads=[sk], writes=["t_rsd"])
                P.op("dve", lambda e: e.reciprocal(out=rsd[:], in_=rsd[:]), reads=["t_rsd"], writes=["t_rsd"])
                for hf in range(2):
                    o_, ok = oo[hf]
                    P.op("dve", lambda e: e.scalar_tensor_tensor(out=o_[:], in0=oh[hf][0][:], scalar=sw[:, hf:hf + 1], in1=rsd[:], op0=ALU.mult, op1=ALU.mult),
                         reads=[oh[hf][1], "t_sw", "t_rsd"], writes=[ok])
                    P.dma("sp", lambda e: e.dma_start(out=io.oT[2 * h + hf, :, qb * 512:(qb + 1) * 512], in_=o_[:]), reads=[ok], writes=["oT"])
    barrier(P)


RG4 = [[0, 1, 2, 3], [4, 5, 6, 7]]


def _gather_thunks(P, src2d, dst2d, rows, chunk, rkey, wkey):
    th = []
    for q in range(rows // chunk):
        def f(q=q):
            P.coll(lambda e: e.collective_compute("AllGather", ALU.bypass, replica_groups=RG4,
                                                  ins=[src2d[q * chunk:(q + 1) * chunk, :].opt()],
                                                  outs=[dst2d[q * 4 * chunk:(q + 1) * 4 * chunk, :].opt()]),
                   reads=[rkey], writes=[f"{wkey}_{q}"])
        th.append(f)
    return th


def _gather_chunks(P, src2d, dst2d, rows, chunk, rkey, wkey):
    for f in _gather_thunks(P, src2d, dst2d, rows, chunk, rkey, wkey):
        f()


def phase_xab(P, nc, K, io):
    pass


def phase_xkv(P, nc, K, io):
    pass


PHASE_FN.update({"l1a": phase_l1a, "attn": phase_attn, "xab": phase_xab, "xkv": phase_xkv})


_BF = None
_CACHE = {}
FUSED = True
LAST_DBG = None


def _bf():
    global _BF
    if _BF is None:
        import ml_dtypes
        _BF = ml_dtypes.bfloat16
    return _BF


def _consts(hf):
    BF = _bf()
    c = {}
    c["ident"] = np.eye(128, dtype=np.float32).astype(BF)
    k = np.arange(256)
    ang = 2 * np.pi * np.outer(k, k) / 256
    cs = np.concatenate([np.cos(ang), np.sin(ang)], 1) / 16.0
    c["cs256"] = np.ascontiguousarray(cs.reshape(2, 128, 512).transpose(1, 0, 2)).astype(BF)
    t = np.arange(4096, dtype=np.int64)[:, None]
    n = (hf * 2048 + np.arange(2048, dtype=np.int64))[None, :]
    a2 = 2 * np.pi * ((t * n) % 4096).astype(np.float64) / 4096
    c["dft_c"] = (np.cos(a2) / 64.0).astype(np.float32).astype(BF)
    c["dft_s"] = (-np.sin(a2) / 64.0).astype(np.float32).astype(BF)
    d2 = np.stack([np.cos(ang), -np.sin(ang)], 1) / 16.0
    c["dft256"] = np.ascontiguousarray(d2.reshape(2, 128, 2, 256).transpose(1, 0, 2, 3)).astype(np.float32).astype(BF)
    tok = hf * 2048 + np.arange(2048)
    row, col = (tok // 64).astype(np.float32), (tok % 64).astype(np.float32)
    invf = (10000.0 ** (-np.arange(32, dtype=np.float32) / 32)).astype(np.float32)
    i = np.arange(128)
    pos = np.where((i // 64)[:, None] == 0, row[None, :], col[None, :]).astype(np.float32)
    angr = pos * invf[i % 32][:, None]
    c["rope_cs"] = np.stack([np.cos(angr), np.sin(angr)], 0).astype(np.float32)
    R = np.zeros((128, 128), np.float32)
    for ii in range(128):
        if ii % 64 < 32:
            R[ii, ii + 32] = -1.0
        else:
            R[ii, ii - 32] = 1.0
    c["rotT"] = np.ascontiguousarray(R.T).astype(BF)
    c["tri"] = np.triu(np.ones((128, 128), np.float32), k=1).astype(BF)
    return c


def _core_inputs(r, inp):
    b, hf = r // 2, r % 2
    m = {}
    m["x"] = np.ascontiguousarray(inp["x"][b, hf * 2048:(hf + 1) * 2048])
    m["ctx"] = np.ascontiguousarray(inp["ctx"][b])
    ht = 2048 if hf == 0 else 2047
    xh = np.zeros((128, 2048), np.float32)
    xh[0] = inp["x"][b, ht]
    m["xh"] = xh
    cond = np.stack([inp["c"][b], inp["c_ctx"]], 0)
    m["condT"] = np.ascontiguousarray(cond.reshape(2, 16, 128).transpose(2, 1, 0))
    fl = np.zeros((128, 2), np.float32)
    fl[:, 0] = 1.0 if hf == 1 else 0.0
    fl[:, 1] = 1.0 if hf == 0 else 0.0
    m["flags"] = fl
    for k in ["w_mod", "b_mod", "norm1_w", "norm2_w"]:
        m[k] = inp[k]
    m["even_w_in"] = inp["even_w_in"][0]
    m["conv_wT"] = np.ascontiguousarray(inp["even_conv_w"][0].reshape(3, 8, 128).transpose(2, 1, 0))
    m["even_w_out"] = inp["even_w_out"][0]
    m["odd_w_qkv"] = inp["odd_w_qkv"][0]
    m["qk_norm"] = np.ascontiguousarray(np.stack([inp["odd_q_norm"][0], inp["odd_k_norm"][0]], 1))
    m["lam_vecs"] = np.ascontiguousarray(np.stack([inp["odd_lambda_q1"][0], inp["odd_lambda_k1"][0], inp["odd_lambda_q2"][0], inp["odd_lambda_k2"][0]], 0)[None])
    m["subln_wT"] = np.ascontiguousarray(inp["odd_subln_w"][0].reshape(2, 128).T)
    m["odd_w_out"] = inp["odd_w_out"][0]
    for l in range(2):
        m[f"moe_wr{l}"] = np.ascontiguousarray(np.concatenate([inp["moe_w_group"][l], inp["moe_w_expert"][l]], 1))
        m[f"moe_w_gate{l}"] = inp["moe_w_gate"][l]
        m[f"moe_w_up{l}"] = inp["moe_w_up"][l]
        m[f"moe_w_down{l}"] = inp["moe_w_down"][l]
    m["moe_br"] = np.ascontiguousarray(np.concatenate([inp["moe_b_group"], inp["moe_b_expert"]], 1))
    m.update(_consts(hf))
    pb = (r % 4) // 2
    p = np.arange(128, dtype=np.int64)[:, None]
    def grow(f, rank, chunk):
        return (f // chunk) * (4 * chunk) + rank * chunk + (f % chunk)
    cols = []
    for gp in range(2):
        for c in range(32):
            cols.append(grow(gp * 2048 + (c % 16) * 128 + p, 2 * pb + c // 16, 512))
    m["idx_ab"] = np.concatenate(cols, 1).astype(np.int32)
    cols = []
    for s_ in range(16):
        for hs in range(2):
            cols.append(grow(s_ * 128 + p, 2 * pb + hs, 256))
    m["idx_k"] = np.concatenate(cols, 1).astype(np.int32)
    cols = []
    for hp in range(4):
        for c in range(32):
            cols.append(grow(hp * 2048 + (c % 16) * 128 + p, 2 * pb + c // 16, 1024))
    m["idx_v"] = np.concatenate(cols, 1).astype(np.int32)
    return m


def _get(mode):
    if mode not in _CACHE:
        _CACHE[mode] = build(mode)
    return _CACHE[mode]


def _launch(mode, maps):
    nc, io = _get(mode)
    res = run_bass_kernel_spmd(nc, [{k: m[k] for k in io.ext_in} for m in maps], core_ids=list(range(len(maps))))
    return [{k: np.asarray(r[k]) for k in io.ext_out} for r in res.results]


def kernel(**inputs):
    inp = {k: np.asarray(v) for k, v in inputs.items()}
    maps = [_core_inputs(r, inp) for r in range(8)]
    if FUSED:
        rr = _launch("ALL", maps)
        global LAST_DBG
        LAST_DBG = [r["dbg"] for r in rr]
        out = np.zeros((4, 4096, 2048), np.float32)
        for r in range(8):
            out[r // 2, (r % 2) * 2048:(r % 2 + 1) * 2048] = rr[r]["out"]
        return out
    ra = _launch("A", maps)
    for r in range(8):
        p0, p1 = (r // 2) * 2, (r // 2) * 2 + 1
        maps[r]["modv"] = ra[r]["modv"]
        maps[r]["ycT"] = ra[r]["ycT"]
        maps[r]["ab_ctx"] = ra[r]["ab_ctx"]
        maps[r]["ab_full"] = np.concatenate([ra[p0]["ab_own"], ra[p1]["ab_own"]], 1)
    rb = _launch("B", maps)
    for r in range(8):
        p0, p1 = (r // 2) * 2, (r // 2) * 2 + 1
        for k in ["h_lat", "qT", "kT_ctx", "v_ctx"]:
            maps[r][k] = rb[r][k]
        maps[r]["kT_full"] = np.concatenate([rb[p0]["kT_own"], rb[p1]["kT_own"]], 2)
        maps[r]["v_full"] = np.concatenate([rb[p0]["v_own"], rb[p1]["v_own"]], 1)
    rc = _launch("C", maps)
    out = np.zeros((4, 4096, 2048), np.float32)
    for r in range(8):
        out[r // 2, (r % 2) * 2048:(r % 2 + 1) * 2048] = rc[r]["out"]
    return out
```

```python
import numpy as np
from contextlib import ExitStack
import concourse.bass as bass
import concourse.mybir as mybir
from concourse.bass_utils import run_bass_kernel_spmd

F32 = mybir.dt.float32
BF16 = mybir.dt.bfloat16
I32 = mybir.dt.int32
AF = mybir.ActivationFunctionType
ALU = mybir.AluOpType
AX = mybir.AxisListType

NDS = 24


class Prog:
    def __init__(self, nc, es):
        self.nc = nc
        self.es = es
        self.E = {"pe": nc.tensor, "act": nc.scalar, "dve": nc.vector, "pool": nc.gpsimd, "sp": nc.sync}
        self.csem = {e: es.enter_context(nc.semaphore(f"c_{e}")) for e in ["pe", "act", "dve", "pool"]}
        self.ccnt = {e: 0 for e in self.csem}
        self.dsems = [es.enter_context(nc.semaphore(f"d_{i}")) for i in range(NDS)]
        self.dcnt = [0] * NDS
        self.dnext = 0
        self.seen = {e: {} for e in self.E}
        self.lastw = {}
        self.readers = {}
        self.n_wait = 0
        self.xsem = es.enter_context(nc.semaphore("x_cc"))
        self.xcnt = 0

    def coll(self, fn, reads=(), writes=()):
        self._deps("pool", reads, writes)
        inst = fn(self.E["pool"])
        self.xcnt += 1
        inst.then_inc(self.xsem)
        self._commit((("x", 0), self.xcnt), reads, writes)

    def _wait(self, eng, tok):
        semkey, val = tok
        if semkey == ("c", "pe") and eng == "pe":
            return
        if self.seen[eng].get(semkey, 0) >= val:
            return
        sem = self.csem[semkey[1]] if semkey[0] == "c" else (self.xsem if semkey[0] == "x" else self.dsems[semkey[1]])
        self.E[eng].wait_ge(sem, val)
        self.n_wait += 1
        self.seen[eng][semkey] = val

    def _deps(self, eng, reads, writes, is_dma=False):
        for k in reads:
            for sk, v in self.lastw.get(k, {}).items():
                self._wait(eng, (sk, v))
        for k in writes:
            for sk, v in self.lastw.get(k, {}).items():
                if is_dma and sk[0] == "d":
                    continue
                self._wait(eng, (sk, v))
            for sk, v in self.readers.get(k, {}).items():
                self._wait(eng, (sk, v))

    def _commit(self, tok, reads, writes, is_dma=False):
        for k in reads:
            r = self.readers.setdefault(k, {})
            if r.get(tok[0], 0) < tok[1]:
                r[tok[0]] = tok[1]
        for k in writes:
            w = self.lastw.setdefault(k, {})
            if w.get(tok[0], 0) < tok[1]:
                w[tok[0]] = tok[1]
            if not is_dma:
                self.readers[k] = {}

    def op(self, eng, fn, reads=(), writes=()):
        self._deps(eng, reads, writes)
        inst = fn(self.E[eng])
        self.ccnt[eng] += 1
        inst.then_inc(self.csem[eng], 1)
        self._commit((("c", eng), self.ccnt[eng]), reads, writes)

    def op_if(self, eng, pred, fn, reads=(), writes=()):
        self._deps(eng, reads, writes)
        e = self.E[eng]
        with e.If(pred):
            inst = fn(e)
            inst.then_inc(self.csem[eng], 1)
        with e.Else():
            e.sem_inc(self.csem[eng], 1)
        self.ccnt[eng] += 1
        self._commit((("c", eng), self.ccnt[eng]), reads, writes)

    def dma_if(self, q, pred, fn, reads=(), writes=()):
        self._deps(q, reads, writes, is_dma=True)
        i = self.dnext
        self.dnext = (i + 1) % NDS
        if self.dcnt[i] > 0:
            self._wait(q, (("d", i), self.dcnt[i]))
        e = self.E[q]
        with e.If(pred):
            inst = fn(e)
            inst.then_inc(self.dsems[i], 16)
        with e.Else():
            e.sem_inc(self.dsems[i], 16)
        self.dcnt[i] += 16
        self._commit((("d", i), self.dcnt[i]), reads, writes, is_dma=True)

    def dma(self, q, fn, reads=(), writes=()):
        self._deps(q, reads, writes, is_dma=True)
        i = self.dnext
        self.dnext = (i + 1) % NDS
        if self.dcnt[i] > 0:
            self._wait(q, (("d", i), self.dcnt[i]))
        inst = fn(self.E[q])
        self.dcnt[i] += 16
        inst.then_inc(self.dsems[i], 16)
        self._commit((("d", i), self.dcnt[i]), reads, writes, is_dma=True)

    def finish(self, eng="sp"):
        for i in range(NDS):
            if self.dcnt[i] > 0:
                self._wait(eng, (("d", i), self.dcnt[i]))
        for e, c in self.ccnt.items():
            if c > 0:
                self._wait(eng, (("c", e), c))
        if self.xcnt > 0:
            self._wait(eng, (("x", 0), self.xcnt))


D = 2048
KD = 16
NLAT = 2048
NCTX = 256
NHALO = 128
NTOK0 = NLAT + NCTX
CAP = 1024
HCAP = 512
EPS = 1e-6
LAM_INIT1 = 0.8 - 0.6 * float(np.exp(-0.3 * 1))


class Ctx:
    pass


_UNIQ = [0]


def _T(nc, es, name, shape, dt):
    _UNIQ[0] += 1
    return es.enter_context(nc.sbuf_tensor(f"{name}_{_UNIQ[0]}", shape, dt))


def _PS(nc, es, name, shape, dt=F32):
    _UNIQ[0] += 1
    return es.enter_context(nc.psum_tensor(f"{name}_{_UNIQ[0]}", shape, dt))


def barrier(P):
    for e in ["pe", "act", "dve", "pool", "sp"]:
        P.finish(e)


def load_bcast(P, nc, es, name, src_row_ap, n=D, q="sp"):
    t = _T(nc, es, name, [128, n], F32)
    P.dma(q, lambda e: e.dma_start(out=t[:], in_=src_row_ap.partition_broadcast(128)), writes=[name])
    return t


def norm_block(P, K, src_rows, A, Bt, xf, ub, sq, st, uT=None, uT_off=0, psT=None, tag=""):
    A_t, A_k = A
    B_t, B_k = Bt
    xk, uk, sk = xf[1], ub[1], st[1]
    P.dma("sp", lambda e: e.dma_start(out=xf[0][:], in_=src_rows), writes=[xk])
    P.op("act", lambda e: e.activation(out=sq[0][:], in_=xf[0][:], func=AF.Square, accum_out=st[0][:, 0:1]),
         reads=[xk], writes=[sq[1], sk])
    P.op("act", lambda e: e.activation(out=st[0][:, 1:2], in_=st[0][:, 0:1], func=AF.Sqrt, scale=1.0 / D, bias=K.epsb[:, 0:1]),
         reads=[sk], writes=[sk])
    P.op("dve", lambda e: e.reciprocal(out=st[0][:, 2:3], in_=st[0][:, 1:2]), reads=[sk], writes=[sk])
    P.op("dve", lambda e: e.scalar_tensor_tensor(out=xf[0][:], in0=xf[0][:], scalar=st[0][:, 2:3], in1=A_t[:],
                                                  op0=ALU.mult, op1=ALU.mult), reads=[xk, sk, A_k], writes=[xk])
    P.op("pool", lambda e: e.tensor_tensor(out=ub[0][:], in0=xf[0][:], in1=B_t[:], op=ALU.add), reads=[xk, B_k], writes=[uk])
    if uT is not None:
        for half in range(2):
            pk = psT[half][1]

            def tr(e, half=half):
                for j in range(8):
                    k = half * 8 + j
                    i = e.transpose(out=psT[half][0][:, j, :], in_=ub[0][:, k * 128:(k + 1) * 128], identity=K.ident[:])
                return i
            P.op("pe", tr, reads=[uk], writes=[pk])
            eng = "act" if half == 0 else "dve"
            if eng == "act":
                P.op("act", lambda e, half=half: e.copy(out=uT[0][:, half * 8:(half + 1) * 8, uT_off:uT_off + 128], in_=psT[half][0][:]),
                     reads=[pk], writes=[uT[1]])
            else:
                P.op("dve", lambda e, half=half: e.tensor_copy(out=uT[0][:, half * 8:(half + 1) * 8, uT_off:uT_off + 128], in_=psT[half][0][:]),
                     reads=[pk], writes=[uT[1]])


def phase_mod(P, nc, K, io):
    es = ExitStack()
    K.mod_es = es
    cT = _T(nc, es, "m_cT", [128, KD, 2], F32)
    sT = _T(nc, es, "m_sT", [128, KD, 2], BF16)
    P.dma("sp", lambda e: e.dma_start(out=cT[:], in_=io.condT), writes=["m_cT"])
    P.op("act", lambda e: e.activation(out=sT[:], in_=cT[:], func=AF.Silu), reads=["m_cT"], writes=["m_sT"])
    wm = [_T(nc, es, f"m_w{i}", [128, KD, 512], BF16) for i in range(2)]
    bm = [_T(nc, es, f"m_b{i}", [2, 512], F32) for i in range(2)]
    ob = [_T(nc, es, f"m_o{i}", [2, 512], F32) for i in range(2)]
    ps = [_PS(nc, es, f"m_ps{i}", [2, 512]) for i in range(2)]
    cnt = [0]

    def work(l, cb):
        it = cnt[0]
        cnt[0] += 1
        wl = io.w_mod[l].rearrange("(k p) n -> p k n", p=128)
        w = wm[it % 2]
        wk = f"m_w{it % 2}"
        j = it % 2
        P.dma("pool", lambda e: e.dma_start(out=w[:], in_=wl[:, :, cb * 512:(cb + 1) * 512]), writes=[wk])
        P.dma("sp", lambda e: e.dma_start(out=bm[j][:], in_=io.b_mod[l:l + 1, cb * 512:(cb + 1) * 512].partition_broadcast(2)),
              writes=[f"m_b{j}"])

        def mm(e):
            for k in range(KD):
                i = e.matmul(ps[j][:], lhsT=sT[:, k, :], rhs=w[:, k, :], start=(k == 0), stop=(k == KD - 1))
            return i
        P.op("pe", mm, reads=[wk, "m_sT"], writes=[f"m_ps{j}"])
        P.op("dve", lambda e: e.tensor_tensor(out=ob[j][:], in0=ps[j][:], in1=bm[j][:], op=ALU.add),
             reads=[f"m_ps{j}", f"m_b{j}"], writes=[f"m_o{j}"])
        P.dma("sp", lambda e: e.dma_start(out=io.modv[l, :, cb * 512:(cb + 1) * 512], in_=ob[j][:]),
              reads=[f"m_o{j}"], writes=["modv"])
    for cb in range(24):
        work(0, cb)
    K.modq = [(lambda cb=cb: work(1, cb)) for cb in range(24)]
    barrier(P)


def drain_mod(P, K, n):
    q = getattr(K, "modq", None)
    while q and n > 0:
        q.pop(0)()
        n -= 1


def make_AB(P, nc, es, K, io, l, which, cond, pfx):
    base = which * 3 * D
    A = load_bcast(P, nc, es, pfx + "A", io.modv[l, cond:cond + 1, base + D:base + 2 * D])
    Bt = load_bcast(P, nc, es, pfx + "B", io.modv[l, cond:cond + 1, base:base + D])
    nw = load_bcast(P, nc, es, pfx + "nw", (io.norm1_w if which == 0 else io.norm2_w)[l:l + 1, :])
    P.op("dve", lambda e: e.scalar_tensor_tensor(out=A[:], in0=A[:], scalar=1.0, in1=nw[:], op0=ALU.add, op1=ALU.mult),
         reads=[pfx + "A", pfx + "nw"], writes=[pfx + "A"])
    return (A, pfx + "A"), (Bt, pfx + "B")


def phase_l0a(P, nc, K, io):
    NT = NLAT + NCTX + NHALO
    with ExitStack() as es:
        uT = (_T(nc, es, "a_uT", [128, KD, NT], BF16), "a_uT")
        with ExitStack() as es2:
            Al, Bl = make_AB(P, nc, es2, K, io, 0, 0, 0, "a_l")
            Ac, Bc = make_AB(P, nc, es2, K, io, 0, 0, 1, "a_c")
            xf = [(_T(nc, es2, f"a_xf{i}", [128, D], F32), f"a_xf{i}") for i in range(2)]
            ub = [(_T(nc, es2, f"a_ub{i}", [128, D], BF16), f"a_ub{i}") for i in range(2)]
            sq = (_T(nc, es2, "a_sq", [128, D], BF16), "a_sq")
            st = [(_T(nc, es2, f"a_st{i}", [128, 4], F32), f"a_st{i}") for i in range(2)]
            psT = [(_PS(nc, es2, f"a_pT{i}", [128, 8, 128], BF16), f"a_pT{i}") for i in range(2)]
            nb = NT // 128
            for b in range(nb):
                if b < 16:
                    src, A, Bt = io.x[b * 128:(b + 1) * 128, :], Al, Bl
                elif b < 18:
                    src, A, Bt = io.ctx[(b - 16) * 128:(b - 15) * 128, :], Ac, Bc
                else:
                    src, A, Bt = io.xh[:, :], Al, Bl
                norm_block(P, K, src, A, Bt, xf[b % 2], ub[b % 2], sq, st[b % 2], uT=uT, uT_off=b * 128, psT=psT)
                drain_mod(P, K, 2)
            barrier(P)
        win = io.even_w_in.rearrange("(k p) n -> p k n", p=128)
        wt = [(_T(nc, es, f"a_w{i}", [128, KD, 128], BF16), f"a_w{i}") for i in range(4)]
        pp = [(_PS(nc, es, f"a_pp{i}", [128, 512]), f"a_pp{i}") for i in range(6)]
        cvl = _T(nc, es, "a_cvl", [128, NLAT + 2], F32)
        cvc = _T(nc, es, "a_cvc", [128, NCTX + 2], F32)
        bg = _T(nc, es, "a_bg", [128, NLAT + NCTX], F32)
        cs = [(_T(nc, es, f"a_cs{i}", [128, 512], F32), f"a_cs{i}") for i in range(2)]
        t1 = _T(nc, es, "a_t1", [128, NLAT + NCTX], F32)
        yb = _T(nc, es, "a_yb", [128, NLAT + NCTX], BF16)
        cw = _T(nc, es, "a_cw", [128, 8, 3], F32)
        P.dma("sp", lambda e: e.dma_start(out=cw[:], in_=io.conv_wT), writes=["a_cw"])
        P.op("pool", lambda e: e.memset(cvl[:], 0.0), writes=["a_cvl"])
        P.op("pool", lambda e: e.memset(cvc[:], 0.0), writes=["a_cvc"])
        tblocks = [(0, 512), (512, 512), (1024, 512), (1536, 512), (2048, 256), (2304, 128)]
        wi = 0
        ppi = 0
        fT = _T(nc, es, "a_fT", [128, 2, NLAT + NCTX], BF16)
        csd = _T(nc, es, "a_csd", [128, 2, 512], BF16)
        P.dma("sp", lambda e: e.dma_start(out=csd[:], in_=io.cs256), writes=["a_csd"])
        abt = [(_T(nc, es, f"a_ab{i}", [128, 512], BF16), f"a_ab{i}") for i in range(2)]
        for g in range(4):
            for hh in range(2):
                w, wk = wt[wi % 4]
                wi += 1
                col = 3072 + g * 256 + hh * 128
                P.dma("pool", lambda e, w=w, col=col: e.dma_start(out=w[:], in_=win[:, :, col:col + 128]), writes=[wk])
                for ti, (t0, tn) in enumerate(tblocks[:5]):
                    p_, pk = pp[ppi % 6]
                    ppi += 1

                    def mm(e, p_=p_, w=w):
                        for k in range(KD):
                            i = e.matmul(p_[:, 0:tn], lhsT=w[:, k, :], rhs=uT[0][:, k, t0:t0 + tn], start=(k == 0), stop=(k == KD - 1))
                        return i
                    P.op("pe", mm, reads=[wk, "a_uT"], writes=[pk])
                    P.op("act", lambda e: e.copy(out=fT[:, hh, t0:t0 + tn], in_=p_[:, 0:tn]), reads=[pk], writes=["a_fT"])
            for b in range(18):
                p_, pk = pp[ppi % 6]
                ppi += 1

                def mm2(e, p_=p_):
                    for hh in range(2):
                        i = e.matmul(p_[:], lhsT=fT[:, hh, b * 128:(b + 1) * 128], rhs=csd[:, hh, :], start=(hh == 0), stop=(hh == 1))
                    return i
                P.op("pe", mm2, reads=["a_fT", "a_csd"], writes=[pk])
                a_, ak = abt[b % 2]
                if b % 2 == 0:
                    P.op("act", lambda e: e.copy(out=a_[:], in_=p_[:]), reads=[pk], writes=[ak])
                else:
                    P.op("dve", lambda e: e.tensor_copy(out=a_[:], in_=p_[:]), reads=[pk], writes=[ak])
                if b < 16:
                    P.dma("sp", lambda e: e.dma_start(out=io.ab_own[g // 2, b * 128:(b + 1) * 128, (g % 2) * 512:(g % 2 + 1) * 512], in_=a_[:]),
                          reads=[ak], writes=["ab_own"])
                else:
                    P.dma("sp", lambda e: e.dma_start(out=io.ab_ctx[(b - 16) * 128:(b - 15) * 128, g * 512:(g + 1) * 512], in_=a_[:]),
                          reads=[ak], writes=["ab_ctx"])
        pend = _gather_thunks(P, io.ab_own.rearrange("g t n -> (g t) n"), io.ab4, 4096, 512, "ab_own", "ab4") if io.mode == "ALL" else []
        for c in range(8):
            ws = []
            for part in range(3):
                w, wk = wt[wi % 4]
                wi += 1
                col = part * 1024 + c * 128
                P.dma("pool", lambda e, w=w, col=col: e.dma_start(out=w[:], in_=win[:, :, col:col + 128]), writes=[wk])
                ws.append((w, wk))
            if pend:
                pend.pop(0)()
            drain_mod(P, K, 3)
            for ti, (t0, tn) in enumerate(tblocks):
                pb = []
                for part in range(3):
                    p_, pk = pp[ppi % 6]
                    ppi += 1
                    w, wk = ws[part]

                    def mm(e, p_=p_, w=w):
                        for k in range(KD):
                            i = e.matmul(p_[:, 0:tn], lhsT=w[:, k, :], rhs=uT[0][:, k, t0:t0 + tn], start=(k == 0), stop=(k == KD - 1))
                        return i
                    P.op("pe", mm, reads=[wk, "a_uT"], writes=[pk])
                    pb.append((p_, pk))
                c_, ck = cs[ti % 2]
                P.op("act", lambda e: e.copy(out=c_[:, 0:tn], in_=pb[1][0][:, 0:tn]), reads=[pb[1][1]], writes=[ck])
                if ti < 4:
                    dst = cvl[:, 1 + t0:1 + t0 + tn]
                    dk = "a_cvl"
                elif ti == 4:
                    dst = cvc[:, 1:1 + tn]
                    dk = "a_cvc"
                else:
                    dst = None
                if dst is not None:
                    P.op("dve", lambda e: e.tensor_tensor(out=dst, in0=c_[:, 0:tn], in1=pb[2][0][:, 0:tn], op=ALU.mult),
                         reads=[ck, pb[2][1]], writes=[dk])
                    P.op("act", lambda e: e.copy(out=bg[:, t0:t0 + tn], in_=pb[0][0][:, 0:tn]), reads=[pb[0][1]], writes=["a_bg"])
                else:
                    P.op("dve", lambda e: e.tensor_tensor(out=c_[:, 0:1], in0=c_[:, 0:1], in1=pb[2][0][:, 0:1], op=ALU.mult),
                         reads=[ck, pb[2][1]], writes=[ck])
                    P.op("dve", lambda e: e.tensor_scalar(out=cvl[:, 0:1], in0=c_[:, 0:1], scalar1=K.flags[:, 0:1], scalar2=None, op0=ALU.mult),
                         reads=[ck], writes=["a_cvl"])
                    P.op("dve", lambda e: e.tensor_scalar(out=cvl[:, NLAT + 1:NLAT + 2], in0=c_[:, 0:1], scalar1=K.flags[:, 1:2], scalar2=None, op0=ALU.mult),
                         reads=[ck], writes=["a_cvl"])
            for (cv, cvk, o0, n) in [(cvl, "a_cvl", 0, NLAT), (cvc, "a_cvc", NLAT, NCTX)]:
                tt = t1[:, o0:o0 + n]
                P.op("dve", lambda e: e.tensor_scalar(out=tt, in0=cv[:, 1:n + 1], scalar1=cw[:, c, 1:2], scalar2=None, op0=ALU.mult),
                     reads=[cvk, "a_cw"], writes=["a_t1"])
                P.op("dve", lambda e: e.scalar_tensor_tensor(out=tt, in0=cv[:, 0:n], scalar=cw[:, c, 0:1], in1=tt, op0=ALU.mult, op1=ALU.add),
                     reads=[cvk, "a_t1"], writes=["a_t1"])
                P.op("dve", lambda e: e.scalar_tensor_tensor(out=tt, in0=cv[:, 2:n + 2], scalar=cw[:, c, 2:3], in1=tt, op0=ALU.mult, op1=ALU.add),
                     reads=[cvk, "a_t1"], writes=["a_t1"])
            P.op("pool", lambda e: e.tensor_tensor(out=yb[:], in0=t1[:], in1=bg[:], op=ALU.mult), reads=["a_t1", "a_bg"], writes=["a_yb"])
            P.dma("sp", lambda e: e.dma_start(out=io.ycT[c], in_=yb[:]), reads=["a_yb"], writes=["ycT"])
    drain_mod(P, K, 100)
    barrier(P)
    if getattr(K, "mod_es", None) is not None:
        K.mod_es.close()
        K.mod_es = None


PHASES_OF = {"A": ["mod", "l0a"], "B": ["l0b", "op0", "moe0", "l1a"], "C": ["attn", "op1", "moe1"],
             "ALL": ["mod", "l0a", "xab", "l0b", "op0", "moe0", "l1a", "xkv", "attn", "op1", "moe1"]}
LAUNCH_OF = {"mod": "A", "l0a": "A", "l0b": "B", "op0": "B", "moe0": "B", "l1a": "B", "attn": "C", "op1": "C", "moe1": "C"}


def declare(nc, mode):
    io = Ctx()
    io.ext_in, io.ext_out = [], []

    def inp(name, shape, dt, launches):
        if mode != "ALL" and mode not in launches:
            return None
        io.ext_in.append(name)
        return nc.dram_tensor(name, shape, dt, kind="ExternalInput").ap()

    def mid(name, shape, dt, prod, cons, scratch=""):
        if mode == "ALL" or mode in scratch:
            return nc.dram_tensor(name, shape, dt, kind="Internal").ap()
        if mode == prod:
            io.ext_out.append(name)
            return nc.dram_tensor(name, shape, dt, kind="ExternalOutput").ap()
        if mode in cons:
            io.ext_in.append(name)
            return nc.dram_tensor(name, shape, dt, kind="ExternalInput").ap()
        return None

    io.x = inp("x", [NLAT, D], F32, "AB")
    io.ctx = inp("ctx", [NCTX, D], F32, "AB")
    io.xh = inp("xh", [128, D], F32, "A")
    io.condT = inp("condT", [128, KD, 2], F32, "A")
    io.flags = inp("flags", [128, 2], F32, "A")
    io.w_mod = inp("w_mod", [2, D, 6 * D], F32, "A")
    io.b_mod = inp("b_mod", [2, 6 * D], F32, "A")
    io.norm1_w = inp("norm1_w", [2, D], F32, "AB")
    io.norm2_w = inp("norm2_w", [2, D], F32, "BC")
    io.even_w_in = inp("even_w_in", [D, 4096], F32, "A")
    io.conv_wT = inp("conv_wT", [128, 8, 3], F32, "A")
    io.even_w_out = inp("even_w_out", [D, D], F32, "B")
    io.odd_w_qkv = inp("odd_w_qkv", [D, 6144], F32, "B")
    io.qk_norm = inp("qk_norm", [128, 2], F32, "B")
    io.lam_vecs = inp("lam_vecs", [1, 4, 128], F32, "C")
    io.subln_wT = inp("subln_wT", [128, 2], F32, "C")
    io.odd_w_out = inp("odd_w_out", [D, D], F32, "C")
    io.moe_wr = [inp(f"moe_wr{l}", [D, 20], F32, "BC"[l]) for l in range(2)]
    io.moe_br = inp("moe_br", [2, 20], F32, "BC")
    io.moe_w_gate = [inp(f"moe_w_gate{l}", [16, D, 512], F32, "BC"[l]) for l in range(2)]
    io.moe_w_up = [inp(f"moe_w_up{l}", [16, D, 512], F32, "BC"[l]) for l in range(2)]
    io.moe_w_down = [inp(f"moe_w_down{l}", [16, 512, D], F32, "BC"[l]) for l in range(2)]
    io.ident = inp("ident", [128, 128], BF16, "ABC")
    io.cs256 = inp("cs256", [128, 2, 512], BF16, "A")
    io.dft_c = inp("dft_c", [4096, NLAT], BF16, "B")
    io.dft_s = inp("dft_s", [4096, NLAT], BF16, "B")
    io.dft256 = inp("dft256", [128, 2, 2, 256], BF16, "B")
    io.rope_cs = inp("rope_cs", [2, 128, NLAT], F32, "B")
    io.rotT = inp("rotT", [128, 128], BF16, "B")
    io.tri = inp("tri", [128, 128], BF16, "BC")
    io.modv = mid("modv", [2, 2, 6 * D], F32, "A", "BC")
    io.ycT = mid("ycT", [8, 128, NLAT + NCTX], BF16, "A", "B")
    io.ab_own = mid("ab_own", [2, NLAT, 1024], BF16, "A", "")
    io.ab_ctx = mid("ab_ctx", [NCTX, D], BF16, "A", "B")
    io.ab_full = mid("ab_full", [2, 2 * NLAT, 1024], BF16, "X", "B") if mode != "ALL" else None
    io.mode = mode
    if mode == "ALL":
        io.ab4 = nc.dram_tensor("ab4", [4 * 2 * NLAT, 1024], BF16, kind="Internal").ap()
        io.k4 = nc.dram_tensor("k4", [4 * 16 * 128, NLAT], BF16, kind="Internal").ap()
        io.v4 = nc.dram_tensor("v4", [4 * 4 * NLAT, 512], BF16, kind="Internal").ap()
    io.idx_ab = inp("idx_ab", [128, 64], I32, "")
    io.idx_k = inp("idx_k", [128, 32], I32, "")
    io.idx_v = inp("idx_v", [128, 128], I32, "")
    io.yfT = mid("yfT", [8, 128, NLAT + NCTX], BF16, "-", "", "B")
    io.h_lat = mid("h_lat", [NLAT, D], F32, "B", "C")
    io.h_ctx = mid("h_ctx", [NCTX, D], F32, "-", "", "B")
    io.h_mid = mid("h_mid", [NLAT, D], F32, "-", "", "BC")
    io.xg = mid("xg", [16 * CAP, D], BF16, "-", "", "BC")
    io.yg = mid("yg", [16 * CAP, D], BF16, "-", "", "BC")
    io.qT = mid("qT", [16, 128, NLAT], BF16, "B", "C")
    io.kT_own = mid("kT_own", [16, 128, NLAT], BF16, "B", "")
    io.kT_ctx = mid("kT_ctx", [16, 128, NCTX], BF16, "B", "C")
    io.v_own = mid("v_own", [4, NLAT, 512], BF16, "B", "")
    io.v_ctx = mid("v_ctx", [NCTX, D], BF16, "B", "C")
    io.kT_full = mid("kT_full", [16, 128, 2 * NLAT], BF16, "X", "C") if mode != "ALL" else None
    io.v_full = mid("v_full", [4, 2 * NLAT, 512], BF16, "X", "C") if mode != "ALL" else None
    io.oT = mid("oT", [16, 128, NLAT], BF16, "-", "", "C")
    io.out = mid("out", [NLAT, D], F32, "C", "") if mode != "ALL" else None
    io.dbg = None
    if mode == "ALL":
        io.ext_out.append("dbg")
        io.dbg = nc.dram_tensor("dbg", [2, 128, 16], F32, kind="ExternalOutput").ap()
        io.ext_out.append("out")
        io.out = nc.dram_tensor("out", [NLAT, D], F32, kind="ExternalOutput").ap()
    return io


def build(mode, phases=None):
    nc = bass.Bass("TRN2", target_bir_lowering=False)
    io = declare(nc, mode)
    for nm in ["xg", "yg", "yfT", "oT", "h_ctx", "ab_own", "kT_own", "v_own"]:
        if getattr(io, nm) is None and mode in "BC":
            pass
    es = ExitStack()
    with es:
        P = Prog(nc, es)
        K = Ctx()
        K.ident = _T(nc, es, "k_ident", [128, 128], BF16)
        K.epsb = _T(nc, es, "k_eps", [128, 1], F32)
        K.ones = _T(nc, es, "k_ones", [128, 128], BF16)
        K.flags = _T(nc, es, "k_flags", [128, 2], F32)
        P.dma("sp", lambda e: e.dma_start(out=K.ident[:], in_=io.ident), writes=["k_ident"])
        P.op("pool", lambda e: e.memset(K.epsb[:], EPS), writes=["k_eps"])
        P.op("pool", lambda e: e.memset(K.ones[:], 1.0), writes=["k_ones"])
        if io.flags is not None:
            P.dma("sp", lambda e: e.dma_start(out=K.flags[:], in_=io.flags), writes=["k_flags"])
        K.breg = nc.gpsimd.to_reg(16 * CAP - 1)
        K.breg_big = nc.gpsimd.to_reg(4 * 16 * 128 * 16 - 1)
        barrier(P)
        for ph in (phases or PHASES_OF[mode]):
            PHASE_FN[ph](P, nc, K, io)
            barrier(P)

        print(f"[build {mode}] waits={P.n_wait} counts={P.ccnt}")
    return nc, io


PHASE_FN = {"mod": phase_mod, "l0a": phase_l0a}


def phase_l0b(P, nc, K, io):
    with ExitStack() as es:
        abps = [_T(nc, es, f"b_abp{i}", [128, 32, 1024], BF16) for i in range(2)]
        dc = [(_T(nc, es, f"b_dc{i}", [128, 8, 512], BF16), f"b_dc{i}") for i in range(2)]
        ds = [(_T(nc, es, f"b_ds{i}", [128, 8, 512], BF16), f"b_ds{i}") for i in range(2)]
        acc = [(_PS(nc, es, f"b_acc{i}", [128, 512]), f"b_acc{i}") for i in range(8)]
        yo = [(_T(nc, es, f"b_yo{i}", [128, 512], BF16), f"b_yo{i}") for i in range(2)]
        if io.mode == "ALL":
            ixab = _T(nc, es, "b_ixab", [128, 64], I32)
            P.dma("sp", lambda e: e.dma_start(out=ixab[:], in_=io.idx_ab), writes=["b_ixab"])
        dcv = io.dft_c.rearrange("(c p) n -> p c n", p=128)
        dsv = io.dft_s.rearrange("(c p) n -> p c n", p=128)
        di = 0
        yi = 0
        ai = 0
        for gp in range(2):
            abp = abps[gp]
            if io.mode == "ALL" and gp == 0:
              for gq in range(2):
                for c in range(32):
                    P.dma("pool", lambda e: e.indirect_dma_start(out=abps[gq][:, c, :], out_offset=None, in_=io.ab4,
                                                                  in_offset=bass.IndirectOffsetOnAxis(ap=ixab[:, gq * 32 + c:gq * 32 + c + 1], axis=0),
                                                                  bounds_check=K.breg_big, oob_is_err=False), reads=[f"ab4_{q_}" for q_ in range(8)] + ["b_ixab"], writes=[f"b_abp{gq}"])
            elif io.mode != "ALL":
                abv = io.ab_full[gp].rearrange("(c p) n -> p c n", p=128)
                for q4 in range(4):
                    P.dma("sp", lambda e: e.dma_start(out=abp[:, q4 * 8:(q4 + 1) * 8, :], in_=abv[:, q4 * 8:(q4 + 1) * 8, :]), writes=[f"b_abp{gp}"])
            for nb in range(4):
                banks = [acc[(ai + a) % 8] for a in range(4)]
                ai += 4
                for tq in range(4):
                    c_, ck = dc[di % 2]
                    s_, sk = ds[di % 2]
                    di += 1
                    P.dma("sp", lambda e: e.dma_start(out=c_[:], in_=dcv[:, tq * 8:(tq + 1) * 8, nb * 512:(nb + 1) * 512]), writes=[ck])
                    P.dma("sp", lambda e: e.dma_start(out=s_[:], in_=dsv[:, tq * 8:(tq + 1) * 8, nb * 512:(nb + 1) * 512]), writes=[sk])

                    def mm(e):
                        for tc in range(8):
                            t = tq * 8 + tc
                            for a in range(4):
                                gl, hh = a // 2, a % 2
                                first = (tq == 0 and tc == 0)
                                last = (tq == 3 and tc == 7)
                                e.matmul(banks[a][0][:], lhsT=abp[:, t, gl * 512 + hh * 128:gl * 512 + hh * 128 + 128], rhs=c_[:, tc, :],
                                         start=first, stop=False)
                                i = e.matmul(banks[a][0][:], lhsT=abp[:, t, gl * 512 + 256 + hh * 128:gl * 512 + 256 + hh * 128 + 128], rhs=s_[:, tc, :],
                                             start=False, stop=last)
                        return i
                    P.op("pe", mm, reads=[f"b_abp{gp}", ck, sk], writes=[b[1] for b in banks])
                for a in range(4):
                    y_, yk = yo[yi % 2]
                    yi += 1
                    if a % 2 == 0:
                        P.op("act", lambda e: e.copy(out=y_[:], in_=banks[a][0][:]), reads=[banks[a][1]], writes=[yk])
                    else:
                        P.op("dve", lambda e: e.tensor_copy(out=y_[:], in_=banks[a][0][:]), reads=[banks[a][1]], writes=[yk])
                    ch = (gp * 2 + a // 2) * 2 + a % 2
                    P.dma("sp", lambda e: e.dma_start(out=io.yfT[ch, :, nb * 512:(nb + 1) * 512], in_=y_[:]), reads=[yk], writes=["yfT"])
        barrier(P)
        abc = _T(nc, es, "b_abc", [128, 2, D], BF16)
        d2 = _T(nc, es, "b_d2", [128, 2, 2, 256], BF16)
        P.dma("sp", lambda e: e.dma_start(out=abc[:], in_=io.ab_ctx.rearrange("(c p) n -> p c n", p=128)), writes=["b_abc"])
        P.dma("sp", lambda e: e.dma_start(out=d2[:], in_=io.dft256), writes=["b_d2"])
        for ch in range(8):
            g, hh = ch // 2, ch % 2
            bk, bkk = acc[ch % 8]

            def mmc(e):
                n = 0
                for t in range(2):
                    for part in range(2):
                        i = e.matmul(bk[:, 0:256], lhsT=abc[:, t, g * 512 + part * 256 + hh * 128:g * 512 + part * 256 + hh * 128 + 128],
                                     rhs=d2[:, t, part, :], start=(n == 0), stop=(n == 3))
                        n += 1
                return i
            P.op("pe", mmc, reads=["b_abc", "b_d2"], writes=[bkk])
            y_, yk = yo[ch % 2]
            P.op("act", lambda e: e.copy(out=y_[:, 0:256], in_=bk[:, 0:256]), reads=[bkk], writes=[yk])
            P.dma("sp", lambda e: e.dma_start(out=io.yfT[ch, :, NLAT:NLAT + NCTX], in_=y_[:, 0:256]), reads=[yk], writes=["yfT"])
    barrier(P)


def phase_op(P, nc, K, io, l, srcs, w_dram, segs):
    with ExitStack() as es:
        wo = _T(nc, es, "o_w", [128, KD, D], BF16)
        wv = w_dram.rearrange("(k p) n -> p k n", p=128)
        for q4 in range(4):
            P.dma("pool", lambda e: e.dma_start(out=wo[:, q4 * 4:(q4 + 1) * 4, :], in_=wv[:, q4 * 4:(q4 + 1) * 4, :]), writes=["o_w"])
        Gs = {}
        for cond in sorted(set(s[2] for s in segs)):
            Gs[cond] = load_bcast(P, nc, es, f"o_G{cond}", io.modv[l, cond:cond + 1, 2 * D:3 * D])
        yt = [(_T(nc, es, f"o_yt{i}", [128, KD, 512], BF16), f"o_yt{i}") for i in range(2)]
        hx = [(_T(nc, es, f"o_hx{i}", [128, D], F32), f"o_hx{i}") for i in range(2)]
        mg = [(_T(nc, es, f"o_mg{i}", [128, D], F32), f"o_mg{i}") for i in range(2)]
        ps = [(_PS(nc, es, f"o_ps{i}", [128, 512]), f"o_ps{i}") for i in range(8)]
        si = 0
        bi = 0
        pi = 0
        for (tok0, ntok, cond, h_src, h_dst) in segs:
            for s0 in range(0, ntok, 512):
                sn = min(512, ntok - s0)
                y_, yk = yt[si % 2]
                si += 1
                for half, src in enumerate(srcs):
                    P.dma("sp", lambda e: e.dma_start(out=y_[:, half * 8:(half + 1) * 8, 0:sn],
                                                      in_=src[:, :, tok0 + s0:tok0 + s0 + sn].rearrange("c p t -> p c t")), writes=[yk])
                for b in range(sn // 128):
                    r0 = s0 + b * 128
                    h_, hk = hx[bi % 2]
                    m_, mk = mg[bi % 2]
                    bi += 1
                    P.dma("sp", lambda e: e.dma_start(out=h_[:], in_=h_src[r0:r0 + 128, :]), writes=[hk])
                    for cb in range(4):
                        p_, pk = ps[pi % 8]
                        pi += 1

                        def mm(e):
                            for c in range(KD):
                                i = e.matmul(p_[:], lhsT=y_[:, c, b * 128:(b + 1) * 128], rhs=wo[:, c, cb * 512:(cb + 1) * 512],
                                             start=(c == 0), stop=(c == KD - 1))
                            return i
                        P.op("pe", mm, reads=[yk, "o_w"], writes=[pk])
                        P.op("dve", lambda e: e.tensor_tensor(out=m_[:, cb * 512:(cb + 1) * 512], in0=p_[:], in1=Gs[cond][:, cb * 512:(cb + 1) * 512], op=ALU.mult),
                             reads=[pk, f"o_G{cond}"], writes=[mk])
                    P.op("pool", lambda e: e.tensor_tensor(out=m_[:], in0=m_[:], in1=h_[:], op=ALU.add), reads=[mk, hk], writes=[mk])
                    P.dma("sp", lambda e: e.dma_start(out=h_dst[r0:r0 + 128, :], in_=m_[:]), reads=[mk], writes=["hdst"])
    barrier(P)


def phase_op0(P, nc, K, io):
    phase_op(P, nc, K, io, 0, [io.ycT, io.yfT], io.even_w_out,
             [(0, NLAT, 0, io.x, io.h_mid), (NLAT, NCTX, 1, io.ctx, io.h_ctx)])


def phase_op1(P, nc, K, io):
    oT = io.oT
    phase_op(P, nc, K, io, 1, [oT[0:8], oT[8:16]], io.odd_w_out, [(0, NLAT, 0, io.h_lat, io.h_mid)])


def phase_moe(P, nc, K, io, l, segs):
    blocks = []
    for (ntok, cond, h_src, h_dst) in segs:
        for b in range(ntok // 128):
            blocks.append((cond, h_src[b * 128:(b + 1) * 128, :], h_dst[b * 128:(b + 1) * 128, :]))
    nb = len(blocks)
    NJ = HCAP // 128
    with ExitStack() as es:
        idx1 = _T(nc, es, "e_idx1", [128, nb], I32)
        idx2 = _T(nc, es, "e_idx2", [128, nb], I32)
        wts = _T(nc, es, "e_wts", [128, 2, nb], F32)
        cnti = _T(nc, es, "e_cnti", [128, 16], I32)
        Gs = {}
        with ExitStack() as es2:
            ABs = {}
            for cond in sorted(set(b[0] for b in blocks)):
                ABs[cond] = make_AB(P, nc, es2, K, io, l, 1, cond, f"e_c{cond}")
            xf = [(_T(nc, es2, f"e_xf{i}", [128, D], F32), f"e_xf{i}") for i in range(2)]
            ub = [(_T(nc, es2, f"e_ub{i}", [128, D], BF16), f"e_ub{i}") for i in range(3)]
            sq = (_T(nc, es2, "e_sq", [128, D], BF16), "e_sq")
            st = [(_T(nc, es2, f"e_st{i}", [128, 4], F32), f"e_st{i}") for i in range(2)]
            vT = [(_T(nc, es2, f"e_vT{i}", [128, KD, 128], BF16), f"e_vT{i}") for i in range(2)]
            psT = [(_PS(nc, es2, f"e_pT{i}", [128, 8, 128], BF16), f"e_pT{i}") for i in range(2)]
            pl = [(_PS(nc, es2, f"e_pl{i}", [128, 64]), f"e_pl{i}") for i in range(2)]
            wr = _T(nc, es2, "e_wr", [128, KD, 20], BF16)
            br = _T(nc, es2, "e_br", [128, 20], F32)
            tri = _T(nc, es2, "e_tri", [128, 128], BF16)
            ecap = _T(nc, es2, "e_ecap", [128, 16], F32)
            base = _T(nc, es2, "e_base", [128, 16], F32)
            P.dma("pool", lambda e: e.dma_start(out=wr[:], in_=io.moe_wr[l].rearrange("(k p) n -> p k n", p=128)), writes=["e_wr"])
            P.dma("sp", lambda e: e.dma_start(out=br[:], in_=io.moe_br[l:l + 1, :].partition_broadcast(128)), writes=["e_br"])
            P.dma("sp", lambda e: e.dma_start(out=tri[:], in_=io.tri), writes=["e_tri"])
            P.op("pool", lambda e: e.iota(ecap[:], pattern=[[CAP, 16]], base=0, channel_multiplier=0, allow_small_or_imprecise_dtypes=True), writes=["e_ecap"])
            P.op("pool", lambda e: e.memset(base[:], 0.0), writes=["e_base"])
            sm = [(_T(nc, es2, f"e_sm{i}", [128, 128], F32), f"e_sm{i}") for i in range(2)]
            ac = [(_T(nc, es2, f"e_ac{i}", [128, 16], BF16), f"e_ac{i}") for i in range(2)]
            def route(bi, cond, src, dst):
                    A, Bt = ABs[cond]
                    u_, uk = ub[bi % 3]
                    v_, vk = vT[bi % 2]
                    norm_block(P, K, src, A, Bt, xf[bi % 2], (u_, uk), sq, st[bi % 2], uT=(v_, vk), uT_off=0, psT=psT)
                    p_, pk = pl[bi % 2]
                    s_, smk = sm[bi % 2]
                    a_, ak = ac[bi % 2]

                    def mm(e):
                        for k in range(KD):
                            i = e.matmul(p_[:, 0:20], lhsT=v_[:, k, :], rhs=wr[:, k, :], start=(k == 0), stop=(k == KD - 1))
                        return i
                    P.op("pe", mm, reads=[vk, "e_wr"], writes=[pk])
                    R = [smk]

                    def dv(fn, extra_r=(), extra_w=()):
                        P.op("dve", fn, reads=R + list(extra_r), writes=R + list(extra_w))
                    lg = s_[:, 0:20]
                    gm, ngm, gs, gw = s_[:, 20:21], s_[:, 21:22], s_[:, 22:23], s_[:, 23:24]
                    mgk = s_[:, 24:28]
                    esel = s_[:, 28:32]
                    m1, m2 = s_[:, 32:33], s_[:, 33:34]
                    o1, o2 = s_[:, 34:38], s_[:, 38:42]
                    es2_ = s_[:, 42:46]
                    dd, ed = s_[:, 46:47], s_[:, 47:48]
                    E1, E2 = s_[:, 48:64], s_[:, 64:80]
                    pos, posc, tmp16 = s_[:, 80:96], s_[:, 96:112], s_[:, 112:128]
                    dv(lambda e: e.tensor_tensor(out=lg, in0=p_[:, 0:20], in1=br[:], op=ALU.add), extra_r=[pk, "e_br"])
                    yield
                    dv(lambda e: e.tensor_reduce(out=gm, in_=s_[:, 0:4], axis=AX.X, op=ALU.max))
                    yield
                    dv(lambda e: e.tensor_scalar(out=ngm, in0=gm, scalar1=-1.0, scalar2=None, op0=ALU.mult))
                    yield
                    P.op("act", lambda e: e.activation(out=tmp16[:, 0:4], in_=s_[:, 0:4], func=AF.Exp, bias=ngm, scale=1.0, accum_out=gs), reads=R, writes=R)
                    yield
                    dv(lambda e: e.reciprocal(out=gw, in_=gs))
                    yield
                    dv(lambda e: e.tensor_scalar(out=mgk, in0=s_[:, 0:4], scalar1=gm, scalar2=None, op0=ALU.is_equal))
                    yield
                    dv(lambda e: e.tensor_scalar(out=esel, in0=s_[:, 4:8], scalar1=s_[:, 24:25], scalar2=None, op0=ALU.mult))
                    yield
                    for g in range(1, 4):
                        dv(lambda e: e.scalar_tensor_tensor(out=esel, in0=s_[:, 4 + 4 * g:8 + 4 * g], scalar=s_[:, 24 + g:25 + g], in1=esel, op0=ALU.mult, op1=ALU.add))
                        yield
                    dv(lambda e: e.tensor_reduce(out=m1, in_=esel, axis=AX.X, op=ALU.max))
                    yield
                    dv(lambda e: e.tensor_scalar(out=o1, in0=esel, scalar1=m1, scalar2=None, op0=ALU.is_equal))
                    yield
                    dv(lambda e: e.scalar_tensor_tensor(out=es2_, in0=o1, scalar=-1.0e30, in1=esel, op0=ALU.mult, op1=ALU.add))
                    yield
                    dv(lambda e: e.tensor_reduce(out=m2, in_=es2_, axis=AX.X, op=ALU.max))
                    yield
                    dv(lambda e: e.tensor_scalar(out=o2, in0=es2_, scalar1=m2, scalar2=None, op0=ALU.is_equal))
                    yield
                    dv(lambda e: e.tensor_tensor(out=dd, in0=m2, in1=m1, op=ALU.subtract))
                    yield
                    P.op("act", lambda e: e.activation(out=ed, in_=dd, func=AF.Exp), reads=R, writes=R)
                    yield
                    dv(lambda e: e.tensor_scalar(out=ed, in0=ed, scalar1=1.0, scalar2=None, op0=ALU.add))
                    yield
                    dv(lambda e: e.reciprocal(out=ed, in_=ed))
                    yield
                    dv(lambda e: e.tensor_tensor(out=wts[:, 0, bi:bi + 1], in0=gw, in1=ed, op=ALU.mult), extra_w=["e_wts"])
                    yield
                    dv(lambda e: e.tensor_tensor(out=wts[:, 1, bi:bi + 1], in0=gw, in1=wts[:, 0, bi:bi + 1], op=ALU.subtract), extra_r=["e_wts"], extra_w=["e_wts"])
                    yield
                    for g in range(4):
                        dv(lambda e: e.tensor_scalar(out=s_[:, 48 + 4 * g:52 + 4 * g], in0=o1, scalar1=s_[:, 24 + g:25 + g], scalar2=None, op0=ALU.mult))
                        yield
                        dv(lambda e: e.tensor_scalar(out=s_[:, 64 + 4 * g:68 + 4 * g], in0=o2, scalar1=s_[:, 24 + g:25 + g], scalar2=None, op0=ALU.mult))
                        yield
                    dv(lambda e: e.tensor_tensor(out=a_[:], in0=E1, in1=E2, op=ALU.add), extra_w=[ak])
                    yield

                    def mm2(e):
                        e.matmul(p_[:, 32:48], lhsT=tri[:], rhs=a_[:], start=True, stop=True)
                        return e.matmul(p_[:, 48:64], lhsT=K.ones[:], rhs=a_[:], start=True, stop=True)
                    P.op("pe", mm2, reads=[ak, "e_tri", "k_ones"], writes=[pk])
                    dv(lambda e: e.tensor_tensor(out=pos, in0=p_[:, 32:48], in1=base[:], op=ALU.add), extra_r=[pk, "e_base"])
                    yield
                    dv(lambda e: e.tensor_tensor(out=base[:], in0=p_[:, 48:64], in1=base[:], op=ALU.add), extra_r=[pk, "e_base"], extra_w=["e_base"])
                    yield
                    dv(lambda e: e.tensor_scalar(out=tmp16, in0=pos, scalar1=float(CAP), scalar2=1.0e6, op0=ALU.is_ge, op1=ALU.mult))
                    yield
                    dv(lambda e: e.tensor_tensor(out=posc, in0=pos, in1=ecap[:], op=ALU.add), extra_r=["e_ecap"])
                    yield
                    dv(lambda e: e.tensor_tensor(out=posc, in0=posc, in1=tmp16, op=ALU.add))
                    yield
                    dv(lambda e: e.tensor_tensor(out=tmp16, in0=posc, in1=E1, op=ALU.mult))
                    yield
                    dv(lambda e: e.tensor_reduce(out=dd, in_=tmp16, axis=AX.X, op=ALU.add))
                    yield
                    dv(lambda e: e.tensor_copy(out=idx1[:, bi:bi + 1], in_=dd), extra_w=[f"e_i1_{bi}"])
                    yield
                    dv(lambda e: e.tensor_tensor(out=tmp16, in0=posc, in1=E2, op=ALU.mult))
                    yield
                    dv(lambda e: e.tensor_reduce(out=dd, in_=tmp16, axis=AX.X, op=ALU.add))
                    yield
                    dv(lambda e: e.tensor_copy(out=idx2[:, bi:bi + 1], in_=dd), extra_w=[f"e_i2_{bi}"])
                    yield
                    for (ix, ik) in [(idx1, f"e_i1_{bi}"), (idx2, f"e_i2_{bi}")]:
                        P.dma("pool", lambda e: e.indirect_dma_start(out=io.xg, out_offset=bass.IndirectOffsetOnAxis(ap=ix[:, bi:bi + 1], axis=0),
                                                                      in_=u_[:], in_offset=None, bounds_check=K.breg, oob_is_err=False),
                              reads=[uk, ik], writes=["xg"])
            gens = [route(bi, cond, src, dst) for bi, (cond, src, dst) in enumerate(blocks)]
            active = []

            def step(g_):
                try:
                    next(g_)
                    return True
                except StopIteration:
                    if g_ in active:
                        active.remove(g_)
                    return False
            while gens or active:
                if gens and len(active) < 2:
                    if active:
                        for _ in range(4):
                            if not step(active[0]):
                                break
                    active.append(gens.pop(0))
                for g_ in list(active):
                    step(g_)
            if io.dbg is not None:
                P.dma("sp", lambda e: e.dma_start(out=io.dbg[l], in_=base[:]), reads=["e_base"], writes=["dbg"])
            P.op("dve", lambda e: e.tensor_copy(out=cnti[:], in_=base[:]), reads=["e_base"], writes=["e_cnti"])
            barrier(P)
        with ExitStack() as es3:
            wg = [(_T(nc, es3, f"e_wg{i}", [128, KD, 512], BF16), f"e_wg{i}") for i in range(2)]
            wu = [(_T(nc, es3, f"e_wu{i}", [128, KD, 512], BF16), f"e_wu{i}") for i in range(2)]
            wd = [(_T(nc, es3, f"e_wd{i}", [128, 4, D], BF16), f"e_wd{i}") for i in range(2)]
            xr = [(_T(nc, es3, f"e_xr{i}", [128, NJ, D], BF16), f"e_xr{i}") for i in range(2)]
            xT = _T(nc, es3, "e_xT", [128, KD, HCAP], BF16)
            aT = _T(nc, es3, "e_aT", [128, 4, HCAP], BF16)
            sg = [(_T(nc, es3, f"e_sg{i}", [128, HCAP], F32), f"e_sg{i}") for i in range(2)]
            yb = [(_T(nc, es3, f"e_yb{i}", [128, D], BF16), f"e_yb{i}") for i in range(2)]
            psT = [(_PS(nc, es3, f"e_qT{i}", [128, 8, 128], BF16), f"e_qT{i}") for i in range(2)]
            pg = [(_PS(nc, es3, f"e_pg{i}", [128, 512]), f"e_pg{i}") for i in range(6)]
            cregs = {"pe": es3.enter_context(nc.tensor.register(f"e_cntp{l}")),
                     "act": es3.enter_context(nc.scalar.register(f"e_cnta{l}")),
                     "dve": es3.enter_context(nc.vector.register(f"e_cntv{l}")),
                     "sp": es3.enter_context(nc.sync.register(f"e_cnts{l}"))}
            pgi = 0
            ybi = 0
            xri = 0
            for ex in range(16):
                g_, gk = wg[ex % 2]
                u_, uk = wu[ex % 2]
                d_, dk = wd[ex % 2]
                gv = io.moe_w_gate[l][ex].rearrange("(k p) n -> p k n", p=128)
                uv = io.moe_w_up[l][ex].rearrange("(k p) n -> p k n", p=128)
                dvw = io.moe_w_down[l][ex].rearrange("(k p) n -> p k n", p=128)
                for hq in range(2):
                    P.dma("pool", lambda e: e.dma_start(out=g_[:, hq * 8:(hq + 1) * 8, :], in_=gv[:, hq * 8:(hq + 1) * 8, :]), writes=[gk])
                    P.dma("pool", lambda e: e.dma_start(out=u_[:, hq * 8:(hq + 1) * 8, :], in_=uv[:, hq * 8:(hq + 1) * 8, :]), writes=[uk])
                for hq in range(2):
                    P.dma("pool", lambda e: e.dma_start(out=d_[:, hq * 2:(hq + 1) * 2, :], in_=dvw[:, hq * 2:(hq + 1) * 2, :]), writes=[dk])
                preds = {}
                for en in ["pe", "act", "dve", "sp"]:
                    P._deps(en, ["e_cnti"], [])
                    P.E[en].reg_load(cregs[en], cnti[0:1, ex:ex + 1])
                    preds[en] = P.E[en].snap(cregs[en]) > HCAP
                for hh in range(2):
                    x_, xk = xr[xri % 2]
                    xri += 1
                    r0 = ex * CAP + hh * HCAP

                    def peop(fn, reads, writes):
                        cop("pe", fn, reads, writes)

                    def cop(en, fn, reads, writes):
                        if hh == 0:
                            P.op(en, fn, reads=reads, writes=writes)
                        else:
                            P.op_if(en, preds[en], fn, reads=reads, writes=writes)
                    def cdma(fn, reads, writes):
                        if hh == 0:
                            P.dma("sp", fn, reads=reads, writes=writes)
                        else:
                            P.dma_if("sp", preds["sp"], fn, reads=reads, writes=writes)
                    cdma(lambda e: e.dma_start(out=x_[:], in_=io.xg[r0:r0 + HCAP, :].rearrange("(j p) n -> p j n", p=128)), ["xg"], [xk])
                    for j in range(NJ):
                        if hh == 1:
                            def tr2(e):
                                for k in range(16):
                                    i = e.transpose(out=psT[k // 8][0][:, k % 8, :], in_=x_[:, j, k * 128:(k + 1) * 128], identity=K.ident[:])
                                return i
                            peop(tr2, [xk], [psT[0][1], psT[1][1]])
                        for half in range(2):
                            pt, ptk = psT[half]

                            def tr(e):
                                for jj in range(8):
                                    k = half * 8 + jj
                                    i = e.transpose(out=pt[:, jj, :], in_=x_[:, j, k * 128:(k + 1) * 128], identity=K.ident[:])
                                return i
                            if hh == 0:
                                peop(tr, [xk], [ptk])
                            if half == 0:
                                cop("act", lambda e: e.copy(out=xT[:, 0:8, j * 128:(j + 1) * 128], in_=pt[:]), [ptk], ["e_xT"])
                            else:
                                cop("dve", lambda e: e.tensor_copy(out=xT[:, 8:16, j * 128:(j + 1) * 128], in_=pt[:]), [ptk], ["e_xT"])
                    for fc in range(4):
                        pG, pGk = pg[pgi % 6]
                        pU, pUk = pg[(pgi + 1) % 6]
                        pgi += 2

                        def mmg(e):
                            for k in range(KD):
                                i = e.matmul(pG[:, 0:HCAP], lhsT=g_[:, k, fc * 128:(fc + 1) * 128], rhs=xT[:, k, :], start=(k == 0), stop=(k == KD - 1))
                            return i

                        def mmu(e):
                            for k in range(KD):
                                i = e.matmul(pU[:, 0:HCAP], lhsT=u_[:, k, fc * 128:(fc + 1) * 128], rhs=xT[:, k, :], start=(k == 0), stop=(k == KD - 1))
                            return i
                        if hh == 0:
                            peop(mmg, [gk, "e_xT"], [pGk])
                            peop(mmu, [uk, "e_xT"], [pUk])
                        else:
                            peop(lambda e: (mmg(e), mmu(e))[1], [gk, uk, "e_xT"], [pGk, pUk])
                        s_, sk = sg[fc % 2]
                        cop("act", lambda e: e.activation(out=s_[:], in_=pG[:, 0:HCAP], func=AF.Silu), [pGk], [sk])
                        cop("dve", lambda e: e.tensor_tensor(out=aT[:, fc, :], in0=s_[:], in1=pU[:, 0:HCAP], op=ALU.mult), [sk, pUk], ["e_aT"])
                    for j in range(NJ):
                        y_, yk = yb[ybi % 2]
                        ybi += 1
                        if hh == 1:
                            bks = [pg[(pgi + cb_) % 6] for cb_ in range(4)]

                            def mmd4(e):
                                for cb_ in range(4):
                                    for fc in range(4):
                                        i = e.matmul(bks[cb_][0][:], lhsT=aT[:, fc, j * 128:(j + 1) * 128], rhs=d_[:, fc, cb_ * 512:(cb_ + 1) * 512],
                                                     start=(fc == 0), stop=(fc == 3))
                                return i
                            peop(mmd4, ["e_aT", dk], [b_[1] for b_ in bks])
                        for cb in range(4):
                            pY, pYk = pg[pgi % 6]
                            pgi += 1

                            def mmd(e):
                                for fc in range(4):
                                    i = e.matmul(pY[:], lhsT=aT[:, fc, j * 128:(j + 1) * 128], rhs=d_[:, fc, cb * 512:(cb + 1) * 512], start=(fc == 0), stop=(fc == 3))
                                return i
                            if hh == 0:
                                peop(mmd, ["e_aT", dk], [pYk])
                            if cb % 2 == 0:
                                cop("act", lambda e: e.copy(out=y_[:, cb * 512:(cb + 1) * 512], in_=pY[:]), [pYk], [yk])
                            else:
                                cop("dve", lambda e: e.tensor_copy(out=y_[:, cb * 512:(cb + 1) * 512], in_=pY[:]), [pYk], [yk])
                        cdma(lambda e: e.dma_start(out=io.yg[r0 + j * 128:r0 + (j + 1) * 128, :], in_=y_[:]), [yk], ["yg"])
            barrier(P)
        with ExitStack() as es4:
            for cond in sorted(set(b[0] for b in blocks)):
                Gs[cond] = load_bcast(P, nc, es4, f"e_G{cond}", io.modv[l, cond:cond + 1, 5 * D:6 * D])
            y1 = [(_T(nc, es4, f"e_y1{i}", [128, D], BF16), f"e_y1{i}") for i in range(2)]
            y2 = [(_T(nc, es4, f"e_y2{i}", [128, D], BF16), f"e_y2{i}") for i in range(2)]
            hx = [(_T(nc, es4, f"e_hx{i}", [128, D], F32), f"e_hx{i}") for i in range(2)]
            tt = [(_T(nc, es4, f"e_tt{i}", [128, D], F32), f"e_tt{i}") for i in range(2)]
            t1 = [(_T(nc, es4, f"e_tq{i}", [128, D], F32), f"e_tq{i}") for i in range(2)]
            for i in range(2):
                P.op("pool", lambda e: e.memset(y1[i][0][:], 0.0), writes=[y1[i][1]])
                P.op("pool", lambda e: e.memset(y2[i][0][:], 0.0), writes=[y2[i][1]])
            for bi, (cond, src, dst) in enumerate(blocks):
                a_, ak = y1[bi % 2]
                b_, bk = y2[bi % 2]
                h_, hk = hx[bi % 2]
                t_, tk = tt[bi % 2]
                q_, qk = t1[bi % 2]
                P.dma("pool", lambda e: e.indirect_dma_start(out=a_[:], out_offset=None, in_=io.yg, in_offset=bass.IndirectOffsetOnAxis(ap=idx1[:, bi:bi + 1], axis=0),
                                                              bounds_check=K.breg, oob_is_err=False), reads=["yg", f"e_i1_{bi}"], writes=[ak])
                P.dma("pool", lambda e: e.indirect_dma_start(out=b_[:], out_offset=None, in_=io.yg, in_offset=bass.IndirectOffsetOnAxis(ap=idx2[:, bi:bi + 1], axis=0),
                                                              bounds_check=K.breg, oob_is_err=False), reads=["yg", f"e_i2_{bi}"], writes=[bk])
                P.dma("sp", lambda e: e.dma_start(out=h_[:], in_=src), writes=[hk])
                P.op("act", lambda e: e.activation(out=q_[:], in_=a_[:], func=AF.Identity, scale=wts[:, 0, bi:bi + 1]), reads=[ak, "e_wts"], writes=[qk])
                P.op("dve", lambda e: e.scalar_tensor_tensor(out=t_[:], in0=b_[:], scalar=wts[:, 1, bi:bi + 1], in1=q_[:], op0=ALU.mult, op1=ALU.add),
                     reads=[bk, "e_wts", qk], writes=[tk])
                P.op("dve", lambda e: e.tensor_tensor(out=t_[:], in0=t_[:], in1=Gs[cond][:], op=ALU.mult), reads=[tk, f"e_G{cond}"], writes=[tk])
                P.op("dve", lambda e: e.tensor_tensor(out=t_[:], in0=t_[:], in1=h_[:], op=ALU.add), reads=[tk, hk], writes=[tk])
                P.dma("sp", lambda e: e.dma_start(out=dst, in_=t_[:]), reads=[tk], writes=["hdst"])
    barrier(P)


def phase_moe0(P, nc, K, io):
    phase_moe(P, nc, K, io, 0, [(NLAT, 0, io.h_mid, io.h_lat), (NCTX, 1, io.h_ctx, io.h_ctx)])


def phase_moe1(P, nc, K, io):
    phase_moe(P, nc, K, io, 1, [(NLAT, 0, io.h_mid, io.out)])


PHASE_FN.update({"l0b": phase_l0b, "op0": phase_op0, "op1": phase_op1, "moe0": phase_moe0, "moe1": phase_moe1})


def phase_l1a(P, nc, K, io):
    NT = NLAT + NCTX
    with ExitStack() as es:
        uT = (_T(nc, es, "q_uT", [128, KD, NT], BF16), "q_uT")
        with ExitStack() as es2:
            Al, Bl = make_AB(P, nc, es2, K, io, 1, 0, 0, "q_l")
            Ac, Bc = make_AB(P, nc, es2, K, io, 1, 0, 1, "q_c")
            xf = [(_T(nc, es2, f"q_xf{i}", [128, D], F32), f"q_xf{i}") for i in range(2)]
            ub = [(_T(nc, es2, f"q_ub{i}", [128, D], BF16), f"q_ub{i}") for i in range(2)]
            sq = (_T(nc, es2, "q_sq", [128, D], BF16), "q_sq")
            st = [(_T(nc, es2, f"q_st{i}", [128, 4], F32), f"q_st{i}") for i in range(2)]
            psT = [(_PS(nc, es2, f"q_pT{i}", [128, 8, 128], BF16), f"q_pT{i}") for i in range(2)]
            for b in range(NT // 128):
                if b < 16:
                    src, A, Bt = io.h_lat[b * 128:(b + 1) * 128, :], Al, Bl
                else:
                    src, A, Bt = io.h_ctx[(b - 16) * 128:(b - 15) * 128, :], Ac, Bc
                norm_block(P, K, src, A, Bt, xf[b % 2], ub[b % 2], sq, st[b % 2], uT=uT, uT_off=b * 128, psT=psT)
            barrier(P)
        wv = io.odd_w_qkv.rearrange("(k p) n -> p k n", p=128)
        wt = [(_T(nc, es, f"q_w{i}", [128, KD, 128], BF16), f"q_w{i}") for i in range(3)]
        cosT = _T(nc, es, "q_cos", [128, NLAT], F32)
        sinT = _T(nc, es, "q_sin", [128, NLAT], F32)
        rotT = _T(nc, es, "q_rot", [128, 128], BF16)
        gn = _T(nc, es, "q_gn", [128, 2], F32)
        P.dma("sp", lambda e: e.dma_start(out=cosT[:], in_=io.rope_cs[0]), writes=["q_cos"])
        P.dma("sp", lambda e: e.dma_start(out=sinT[:], in_=io.rope_cs[1]), writes=["q_sin"])
        P.dma("sp", lambda e: e.dma_start(out=rotT[:], in_=io.rotT), writes=["q_rot"])
        P.dma("sp", lambda e: e.dma_start(out=gn[:], in_=io.qk_norm), writes=["q_gn"])
        px = [(_PS(nc, es, f"q_px{i}", [128, 512]), f"q_px{i}") for i in range(3)]
        pss = [(_PS(nc, es, f"q_pss{i}", [128, 512]), f"q_pss{i}") for i in range(2)]
        pr = [(_PS(nc, es, f"q_pr{i}", [128, 512]), f"q_pr{i}") for i in range(2)]
        sqb = [(_T(nc, es, f"q_sqb{i}", [128, 512], BF16), f"q_sqb{i}") for i in range(3)]
        rs = [(_T(nc, es, f"q_rs{i}", [128, 512], F32), f"q_rs{i}") for i in range(2)]
        yb = [(_T(nc, es, f"q_yb{i}", [128, 512], BF16), f"q_yb{i}") for i in range(3)]
        t1 = [(_T(nc, es, f"q_t1{i}", [128, 512], F32), f"q_t1{i}") for i in range(2)]
        t2 = [(_T(nc, es, f"q_t2{i}", [128, 512], F32), f"q_t2{i}") for i in range(2)]
        ob = [(_T(nc, es, f"q_ob{i}", [128, 512], BF16), f"q_ob{i}") for i in range(2)]
        it = 0

        def run_slabs(slabs, pend=()):
            items = []
            for s in slabs:
                isq = s < 16
                tbl = [(0, 512), (512, 512), (1024, 512), (1536, 512)] + ([] if isq else [(2048, 256)])
                for (t0, tn) in tbl:
                    items.append((s, isq, t0, tn))
            n = len(items)
            wcur = {}

            def s1(i):
                s, isq, t0, tn = items[i]
                def ensure(sx):
                    if sx not in wcur:
                        w, wk = wt[sx % 3]
                        col = sx * 128
                        P.dma("pool", lambda e: e.dma_start(out=w[:], in_=wv[:, :, col:col + 128]), writes=[wk])
                        wcur[sx] = (w, wk)
                ensure(s)
                if s + 1 in slabs and (s + 1) not in wcur:
                    ensure(s + 1)
                    if pend:
                        pend.pop(0)()
                w, wk = wcur[s]
                p_, pk = px[i % 3]
                sq_, sqk = sqb[i % 3]

                def mm(e):
                    for k in range(KD):
                        ii = e.matmul(p_[:, 0:tn], lhsT=w[:, k, :], rhs=uT[0][:, k, t0:t0 + tn], start=(k == 0), stop=(k == KD - 1))
                    return ii
                P.op("pe", mm, reads=[wk, "q_uT"], writes=[pk])
                P.op("act", lambda e: e.activation(out=sq_[:, 0:tn], in_=p_[:, 0:tn], func=AF.Square), reads=[pk], writes=[sqk])

            def s2(i):
                s, isq, t0, tn = items[i]
                p_, pk = px[i % 3]
                sq_, sqk = sqb[i % 3]
                ps_, psk = pss[i % 2]
                r_, rk = rs[i % 2]
                y_, yk = yb[i % 3]
                P.op("pe", lambda e: e.matmul(ps_[:, 0:tn], lhsT=K.ones[:], rhs=sq_[:, 0:tn], start=True, stop=True), reads=[sqk, "k_ones"], writes=[psk])
                P.op("act", lambda e: e.activation(out=r_[:, 0:tn], in_=ps_[:, 0:tn], func=AF.Sqrt, scale=1.0 / 128, bias=K.epsb[:, 0:1]), reads=[psk], writes=[rk])
                P.op("dve", lambda e: e.reciprocal(out=r_[:, 0:tn], in_=r_[:, 0:tn]), reads=[rk], writes=[rk])
                gcol = gn[:, 0:1] if isq else gn[:, 1:2]
                P.op("dve", lambda e: e.scalar_tensor_tensor(out=y_[:, 0:tn], in0=p_[:, 0:tn], scalar=gcol, in1=r_[:, 0:tn], op0=ALU.mult, op1=ALU.mult),
                     reads=[pk, rk, "q_gn"], writes=[yk])

            def s3(i):
                s, isq, t0, tn = items[i]
                y_, yk = yb[i % 3]
                pr_, prk = pr[i % 2]
                a_, ak = t1[i % 2]
                b_, bk = t2[i % 2]
                o_, ok = ob[i % 2]
                if t0 < NLAT:
                    P.op("pe", lambda e: e.matmul(pr_[:, 0:tn], lhsT=rotT[:], rhs=y_[:, 0:tn], start=True, stop=True), reads=[yk, "q_rot"], writes=[prk])
                    P.op("pool", lambda e: e.tensor_tensor(out=a_[:, 0:tn], in0=y_[:, 0:tn], in1=cosT[:, t0:t0 + tn], op=ALU.mult), reads=[yk, "q_cos"], writes=[ak])
                    P.op("dve", lambda e: e.tensor_tensor(out=b_[:, 0:tn], in0=pr_[:, 0:tn], in1=sinT[:, t0:t0 + tn], op=ALU.mult), reads=[prk, "q_sin"], writes=[bk])
                    P.op("pool", lambda e: e.tensor_tensor(out=o_[:, 0:tn], in0=a_[:, 0:tn], in1=b_[:, 0:tn], op=ALU.add), reads=[ak, bk], writes=[ok])
                    dst = io.qT[s, :, t0:t0 + tn] if isq else io.kT_own[s - 16, :, t0:t0 + tn]
                    P.dma("sp", lambda e: e.dma_start(out=dst, in_=o_[:, 0:tn]), reads=[ok], writes=["q_out" if isq else "k_out"])
                else:
                    P.dma("sp", lambda e: e.dma_start(out=io.kT_ctx[s - 16, :, 0:tn], in_=y_[:, 0:tn]), reads=[yk], writes=["k_out"])
            for t in range(n + 2):
                if t < n:
                    s1(t)
                if 0 <= t - 1 < n:
                    s2(t - 1)
                if 0 <= t - 2 < n:
                    s3(t - 2)

        run_slabs(list(range(16, 32)))
        wvt = [(_T(nc, es, f"q_wv{i}", [128, KD, 512], BF16), f"q_wv{i}") for i in range(4)]
        for cb in range(4):
            w, wk = wvt[cb]
            for hq in range(2):
                P.dma("pool", lambda e: e.dma_start(out=w[:, hq * 8:(hq + 1) * 8, :], in_=wv[:, hq * 8:(hq + 1) * 8, 4096 + cb * 512:4096 + (cb + 1) * 512]), writes=[wk])
        pend = []
        for cb in range(4):
            w, wk = wvt[cb]
            for _ in range(2):
                if pend:
                    pend.pop(0)()
            for b in range(NT // 128):
                i2 = it % 2
                it += 1
                p_, pk = px[i2]
                o_, ok = ob[i2]

                def mmv(e):
                    for k in range(KD):
                        i = e.matmul(p_[:], lhsT=uT[0][:, k, b * 128:(b + 1) * 128], rhs=w[:, k, :], start=(k == 0), stop=(k == KD - 1))
                    return i
                P.op("pe", mmv, reads=[wk, "q_uT"], writes=[pk])
                if b % 2 == 0:
                    P.op("act", lambda e: e.copy(out=o_[:], in_=p_[:]), reads=[pk], writes=[ok])
                else:
                    P.op("dve", lambda e: e.tensor_copy(out=o_[:], in_=p_[:]), reads=[pk], writes=[ok])
                if b < 16:
                    dst = io.v_own[cb, b * 128:(b + 1) * 128, :]
                else:
                    dst = io.v_ctx[(b - 16) * 128:(b - 15) * 128, cb * 512:(cb + 1) * 512]
                P.dma("sp", lambda e: e.dma_start(out=dst, in_=o_[:]), reads=[ok], writes=["v_out"])
        run_slabs(list(range(16)), pend)
        while pend:
            pend.pop(0)()
    barrier(P)


def phase_attn(P, nc, K, io):
    NK = 2 * NLAT + NCTX
    NKC = NK // 128
    with ExitStack() as es:
        lv = _T(nc, es, "t_lv", [1, 4, 128], F32)
        l1 = _T(nc, es, "t_l1", [1, 8], F32)
        onesf = _T(nc, es, "t_onesf", [1, 128], F32)
        nlam = _T(nc, es, "t_nlam", [128, 1], F32)
        sw = _T(nc, es, "t_sw", [128, 2], F32)
        P.dma("sp", lambda e: e.dma_start(out=lv[:], in_=io.lam_vecs), writes=["t_lv"])
        P.dma("sp", lambda e: e.dma_start(out=sw[:], in_=io.subln_wT), writes=["t_sw"])
        P.op("pool", lambda e: e.memset(onesf[:], 1.0), writes=["t_onesf"])
        P.op("dve", lambda e: e.tensor_tensor(out=lv[:, 0, :], in0=lv[:, 0, :], in1=lv[:, 1, :], op=ALU.mult), reads=["t_lv"], writes=["t_lv"])
        P.op("dve", lambda e: e.tensor_tensor(out=lv[:, 2, :], in0=lv[:, 2, :], in1=lv[:, 3, :], op=ALU.mult), reads=["t_lv"], writes=["t_lv"])
        P.op("dve", lambda e: e.tensor_reduce(out=l1[:, 0:1], in_=lv[:, 0, :], axis=AX.X, op=ALU.add), reads=["t_lv"], writes=["t_l1"])
        P.op("dve", lambda e: e.tensor_reduce(out=l1[:, 1:2], in_=lv[:, 2, :], axis=AX.X, op=ALU.add), reads=["t_lv"], writes=["t_l1"])
        P.op("act", lambda e: e.activation(out=l1[:, 2:4], in_=l1[:, 0:2], func=AF.Exp), reads=["t_l1"], writes=["t_l1"])
        P.op("dve", lambda e: e.tensor_tensor(out=l1[:, 4:5], in0=l1[:, 3:4], in1=l1[:, 2:3], op=ALU.subtract), reads=["t_l1"], writes=["t_l1"])
        P.op("dve", lambda e: e.tensor_scalar(out=l1[:, 4:5], in0=l1[:, 4:5], scalar1=-LAM_INIT1, scalar2=None, op0=ALU.add), reads=["t_l1"], writes=["t_l1"])
        P.op("dve", lambda e: e.tensor_scalar(out=sw[:], in0=sw[:], scalar1=1.0 - LAM_INIT1, scalar2=None, op0=ALU.mult), reads=["t_sw"], writes=["t_sw"])
        kTb = [[_T(nc, es, f"t_kT{b}{c}", [128, NK], BF16) for c in range(2)] for b in range(2)]
        vtb = [_T(nc, es, f"t_v{b}", [128, NKC, 512], BF16) for b in range(2)]
        if io.mode == "ALL":
            ixk = _T(nc, es, "t_ixk", [128, 32], I32)
            ixv = _T(nc, es, "t_ixv", [128, 128], I32)
            P.dma("sp", lambda e: e.dma_start(out=ixk[:], in_=io.idx_k), writes=["t_ixk"])
            P.dma("sp", lambda e: e.dma_start(out=ixv[:], in_=io.idx_v), writes=["t_ixv"])
        qt = [(_T(nc, es, f"t_q{i}", [128, 2, 512], BF16), f"t_q{i}") for i in range(2)]
        pT = [(_T(nc, es, f"t_pT{i}", [128, 512], BF16), f"t_pT{i}") for i in range(3)]
        pS = [(_PS(nc, es, f"t_pS{i}", [128, 512]), f"t_pS{i}") for i in range(2)]
        pO = [[(_PS(nc, es, f"t_pO{c}{j}", [128, 512]), f"t_pO{c}{j}") for j in range(3)] for c in range(2)]
        rc = [_T(nc, es, f"t_rc{c}", [128, 512], F32) for c in range(2)]
        oh = [(_T(nc, es, f"t_oh{i}", [128, 512], F32), f"t_oh{i}") for i in range(2)]
        o2 = [(_T(nc, es, f"t_o2{i}", [128, 512], F32), f"t_o2{i}") for i in range(2)]
        osq = [(_T(nc, es, f"t_osq{i}", [128, 512], BF16), f"t_osq{i}") for i in range(2)]
        rsd = _T(nc, es, "t_rsd", [128, 512], F32)
        oo = [(_T(nc, es, f"t_oo{i}", [128, 512], BF16), f"t_oo{i}") for i in range(2)]
        P.op("pe", lambda e: e.matmul(pS[0][0][:, 0:1], lhsT=onesf[:], rhs=l1[:, 4:5], start=True, stop=True), reads=["t_l1", "t_onesf"], writes=["t_pS0"])
        P.op("dve", lambda e: e.tensor_copy(out=nlam[:], in_=pS[0][0][:, 0:1]), reads=["t_pS0"], writes=["t_nlam"])
        scale = 128 ** -0.5
        qi = 0
        pi = 0
        si = 0
        if io.mode == "ALL":
            kth = _gather_thunks(P, io.kT_own.rearrange("s p t -> (s p) t"), io.k4, 2048, 256, "k_out", "k4")
            vth = _gather_thunks(P, io.v_own.rearrange("g t n -> (g t) n"), io.v4, 8192, 1024, "v_out", "v4")
        else:
            kth, vth = [], []

        def issue_pair_colls(hp):
            if kth and hp < 4:
                for q_ in (2 * hp, 2 * hp + 1):
                    kth[q_]()
                    vth[q_]()

        def load_head(h):
            hp = h // 2
            vt = vtb[hp % 2]
            vk = f"t_v{hp % 2}"
            kT = kTb[h % 2]
            if h % 2 == 0:
                P.dma("sp", lambda e: e.dma_start(out=vt[:, 0:2, :], in_=io.v_ctx[:, hp * 512:(hp + 1) * 512].rearrange("(c p) n -> p c n", p=128)), writes=[vk])
                if io.mode == "ALL":
                    for c32 in range(32):
                        P.dma("pool", lambda e: e.indirect_dma_start(out=vt[:, 2 + c32, :], out_offset=None, in_=io.v4,
                                                                      in_offset=bass.IndirectOffsetOnAxis(ap=ixv[:, hp * 32 + c32:hp * 32 + c32 + 1], axis=0),
                                                                      bounds_check=K.breg_big, oob_is_err=False), reads=[f"v4_{2 * hp + (c32 % 16) // 8}", "t_ixv"], writes=[vk])
                else:
                    for q4 in range(4):
                        P.dma("sp", lambda e: e.dma_start(out=vt[:, 2 + q4 * 8:2 + (q4 + 1) * 8, :],
                                                          in_=io.v_full[hp, q4 * 1024:(q4 + 1) * 1024, :].rearrange("(c p) n -> p c n", p=128)), writes=[vk])
            for c in range(2):
                s = 2 * h + c
                kk = f"t_kT{h % 2}{c}"
                P.dma("sp", lambda e: e.dma_start(out=kT[c][:, 0:NCTX], in_=io.kT_ctx[s]), writes=[kk])
                if io.mode == "ALL":
                    for hf in range(2):
                        P.dma("pool", lambda e: e.indirect_dma_start(out=kT[c][:, NCTX + hf * NLAT:NCTX + (hf + 1) * NLAT], out_offset=None, in_=io.k4,
                                                                      in_offset=bass.IndirectOffsetOnAxis(ap=ixk[:, s * 2 + hf:s * 2 + hf + 1], axis=0),
                                                                      bounds_check=K.breg_big, oob_is_err=False), reads=[f"k4_{h}", "t_ixk"], writes=[kk])
                else:
                    P.dma("sp", lambda e: e.dma_start(out=kT[c][:, NCTX:NK], in_=io.kT_full[s]), writes=[kk])

        issue_pair_colls(0)
        load_head(0)
        for h in range(8):
            hp = h // 2
            vo = (h % 2) * 256
            vt = vtb[hp % 2]
            vk = f"t_v{hp % 2}"
            kT = kTb[h % 2]
            kks = [f"t_kT{h % 2}{c}" for c in range(2)]
            for qb in range(4):
                q_, qk = qt[qi % 2]
                qi += 1
                P.dma("sp", lambda e: e.dma_start(out=q_[:], in_=io.qT[2 * h:2 * h + 2, :, qb * 512:(qb + 1) * 512].rearrange("c p t -> p c t")), writes=[qk])
                steps = [(c, kc) for c in range(2) for kc in range(NKC)]
                bufs = []
                for _ in steps:
                    bufs.append((pS[si % 2], pT[pi % 3]))
                    si += 1
                    pi += 1

                def emit_S(i):
                    c, kc = steps[i]
                    (s_, sk), (p_, pk) = bufs[i]
                    P.op("pe", lambda e: e.matmul(s_[:], lhsT=kT[c][:, kc * 128:(kc + 1) * 128], rhs=q_[:, c, :], start=True, stop=True),
                         reads=[kks[c], qk], writes=[sk])
                    P.op("act", lambda e: e.activation(out=p_[:], in_=s_[:], func=AF.Exp, scale=scale), reads=[sk], writes=[pk])

                def emit_PV(i):
                    c, kc = steps[i]
                    (s_, sk), (p_, pk) = bufs[i]

                    def mmo(e):
                        e.matmul(pO[c][0][0][:], lhsT=vt[:, kc, vo:vo + 128], rhs=p_[:], start=(kc == 0), stop=(kc == NKC - 1))
                        e.matmul(pO[c][1][0][:], lhsT=vt[:, kc, vo + 128:vo + 256], rhs=p_[:], start=(kc == 0), stop=(kc == NKC - 1))
                        return e.matmul(pO[c][2][0][:], lhsT=K.ones[:], rhs=p_[:], start=(kc == 0), stop=(kc == NKC - 1))
                    P.op("pe", mmo, reads=[pk, vk, "k_ones"], writes=[pO[c][0][1], pO[c][1][1], pO[c][2][1]])
                if qb == 1 and h + 1 < 8:
                    load_head(h + 1)
                    if (h + 1) % 2 == 1:
                        issue_pair_colls((h + 1) // 2 + 1)
                emit_S(0)
                for i in range(len(steps)):
                    if i + 1 < len(steps):
                        emit_S(i + 1)
                    emit_PV(i)
                P.op("dve", lambda e: e.reciprocal(out=rc[0][:], in_=pO[0][2][0][:]), reads=[pO[0][2][1]], writes=["t_rc0"])
                P.op("dve", lambda e: e.reciprocal(out=rc[1][:], in_=pO[1][2][0][:]), reads=[pO[1][2][1]], writes=["t_rc1"])
                P.op("dve", lambda e: e.tensor_scalar(out=rc[1][:], in0=rc[1][:], scalar1=nlam[:, 0:1], scalar2=None, op0=ALU.mult), reads=["t_rc1", "t_nlam"], writes=["t_rc1"])
                for hf in range(2):
                    a_, ak = oh[hf]
                    b_, bk = o2[hf]
                    q2_, q2k = osq[hf]
                    P.op("dve", lambda e: e.tensor_tensor(out=a_[:], in0=pO[0][hf][0][:], in1=rc[0][:], op=ALU.mult), reads=[pO[0][hf][1], "t_rc0"], writes=[ak])
                    P.op("dve", lambda e: e.tensor_tensor(out=b_[:], in0=pO[1][hf][0][:], in1=rc[1][:], op=ALU.mult), reads=[pO[1][hf][1], "t_rc1"], writes=[bk])
                    P.op("dve", lambda e: e.tensor_tensor(out=a_[:], in0=a_[:], in1=b_[:], op=ALU.add), reads=[ak, bk], writes=[ak])
                    P.op("act", lambda e: e.activation(out=q2_[:], in_=a_[:], func=AF.Square), reads=[ak], writes=[q2k])
                s_, sk = pS[si % 2]
                si += 1

                def mms(e):
                    e.matmul(s_[:], lhsT=K.ones[:], rhs=osq[0][0][:], start=True, stop=False)
                    return e.matmul(s_[:], lhsT=K.ones[:], rhs=osq[1][0][:], start=False, stop=True)
                P.op("pe", mms, reads=[osq[0][1], osq[1][1], "k_ones"], writes=[sk])
                P.op("act", lambda e: e.activation(out=rsd[:], in_=s_[:], func=AF.Sqrt, scale=1.0 / 256, bias=K.epsb[:, 0:1]), reads=[sk], writes=["t_rsd"])
                P.op("dve", lambda e: e.reciprocal(out=rsd[:], in_=rsd[:]), reads=["t_rsd"], writes=["t_rsd"])
                for hf in range(2):
                    o_, ok = oo[hf]
                    P.op("dve", lambda e: e.scalar_tensor_tensor(out=o_[:], in0=oh[hf][0][:], scalar=sw[:, hf:hf + 1], in1=rsd[:], op0=ALU.mult, op1=ALU.mult),
                         reads=[oh[hf][1], "t_sw", "t_rsd"], writes=[ok])
                    P.dma("sp", lambda e: e.dma_start(out=io.oT[2 * h + hf, :, qb * 512:(qb + 1) * 512], in_=o_[:]), reads=[ok], writes=["oT"])
    barrier(P)


RG4 = [[0, 1, 2, 3], [4, 5, 6, 7]]


def _gather_thunks(P, src2d, dst2d, rows, chunk, rkey, wkey):
    th = []
    for q in range(rows // chunk):
        def f(q=q):
            P.coll(lambda e: e.collective_compute("AllGather", ALU.bypass, replica_groups=RG4,
                                                  ins=[src2d[q * chunk:(q + 1) * chunk, :].opt()],
                                                  outs=[dst2d[q * 4 * chunk:(q + 1) * 4 * chunk, :].opt()]),
                   reads=[rkey], writes=[f"{wkey}_{q}"])
        th.append(f)
    return th


def _gather_chunks(P, src2d, dst2d, rows, chunk, rkey, wkey):
    for f in _gather_thunks(P, src2d, dst2d, rows, chunk, rkey, wkey):
        f()


def phase_xab(P, nc, K, io):
    pass


def phase_xkv(P, nc, K, io):
    pass


PHASE_FN.update({"l1a": phase_l1a, "attn": phase_attn, "xab": phase_xab, "xkv": phase_xkv})


_BF = None
_CACHE = {}
FUSED = True
LAST_DBG = None


def _bf():
    global _BF
    if _BF is None:
        import ml_dtypes
        _BF = ml_dtypes.bfloat16
    return _BF


def _consts(hf):
    BF = _bf()
    c = {}
    c["ident"] = np.eye(128, dtype=np.float32).astype(BF)
    k = np.arange(256)
    ang = 2 * np.pi * np.outer(k, k) / 256
    cs = np.concatenate([np.cos(ang), np.sin(ang)], 1) / 16.0
    c["cs256"] = np.ascontiguousarray(cs.reshape(2, 128, 512).transpose(1, 0, 2)).astype(BF)
    t = np.arange(4096, dtype=np.int64)[:, None]
    n = (hf * 2048 + np.arange(2048, dtype=np.int64))[None, :]
    a2 = 2 * np.pi * ((t * n) % 4096).astype(np.float64) / 4096
    c["dft_c"] = (np.cos(a2) / 64.0).astype(np.float32).astype(BF)
    c["dft_s"] = (-np.sin(a2) / 64.0).astype(np.float32).astype(BF)
    d2 = np.stack([np.cos(ang), -np.sin(ang)], 1) / 16.0
    c["dft256"] = np.ascontiguousarray(d2.reshape(2, 128, 2, 256).transpose(1, 0, 2, 3)).astype(np.float32).astype(BF)
    tok = hf * 2048 + np.arange(2048)
    row, col = (tok // 64).astype(np.float32), (tok % 64).astype(np.float32)
    invf = (10000.0 ** (-np.arange(32, dtype=np.float32) / 32)).astype(np.float32)
    i = np.arange(128)
    pos = np.where((i // 64)[:, None] == 0, row[None, :], col[None, :]).astype(np.float32)
    angr = pos * invf[i % 32][:, None]
    c["rope_cs"] = np.stack([np.cos(angr), np.sin(angr)], 0).astype(np.float32)
    R = np.zeros((128, 128), np.float32)
    for ii in range(128):
        if ii % 64 < 32:
            R[ii, ii + 32] = -1.0
        else:
            R[ii, ii - 32] = 1.0
    c["rotT"] = np.ascontiguousarray(R.T).astype(BF)
    c["tri"] = np.triu(np.ones((128, 128), np.float32), k=1).astype(BF)
    return c


def _core_inputs(r, inp):
    b, hf = r // 2, r % 2
    m = {}
    m["x"] = np.ascontiguousarray(inp["x"][b, hf * 2048:(hf + 1) * 2048])
    m["ctx"] = np.ascontiguousarray(inp["ctx"][b])
    ht = 2048 if hf == 0 else 2047
    xh = np.zeros((128, 2048), np.float32)
    xh[0] = inp["x"][b, ht]
    m["xh"] = xh
    cond = np.stack([inp["c"][b], inp["c_ctx"]], 0)
    m["condT"] = np.ascontiguousarray(cond.reshape(2, 16, 128).transpose(2, 1, 0))
    fl = np.zeros((128, 2), np.float32)
    fl[:, 0] = 1.0 if hf == 1 else 0.0
    fl[:, 1] = 1.0 if hf == 0 else 0.0
    m["flags"] = fl
    for k in ["w_mod", "b_mod", "norm1_w", "norm2_w"]:
        m[k] = inp[k]
    m["even_w_in"] = inp["even_w_in"][0]
    m["conv_wT"] = np.ascontiguousarray(inp["even_conv_w"][0].reshape(3, 8, 128).transpose(2, 1, 0))
    m["even_w_out"] = inp["even_w_out"][0]
    m["odd_w_qkv"] = inp["odd_w_qkv"][0]
    m["qk_norm"] = np.ascontiguousarray(np.stack([inp["odd_q_norm"][0], inp["odd_k_norm"][0]], 1))
    m["lam_vecs"] = np.ascontiguousarray(np.stack([inp["odd_lambda_q1"][0], inp["odd_lambda_k1"][0], inp["odd_lambda_q2"][0], inp["odd_lambda_k2"][0]], 0)[None])
    m["subln_wT"] = np.ascontiguousarray(inp["odd_subln_w"][0].reshape(2, 128).T)
    m["odd_w_out"] = inp["odd_w_out"][0]
    for l in range(2):
        m[f"moe_wr{l}"] = np.ascontiguousarray(np.concatenate([inp["moe_w_group"][l], inp["moe_w_expert"][l]], 1))
        m[f"moe_w_gate{l}"] = inp["moe_w_gate"][l]
        m[f"moe_w_up{l}"] = inp["moe_w_up"][l]
        m[f"moe_w_down{l}"] = inp["moe_w_down"][l]
    m["moe_br"] = np.ascontiguousarray(np.concatenate([inp["moe_b_group"], inp["moe_b_expert"]], 1))
    m.update(_consts(hf))
    pb = (r % 4) // 2
    p = np.arange(128, dtype=np.int64)[:, None]
    def grow(f, rank, chunk):
        return (f // chunk) * (4 * chunk) + rank * chunk + (f % chunk)
    cols = []
    for gp in range(2):
        for c in range(32):
            cols.append(grow(gp * 2048 + (c % 16) * 128 + p, 2 * pb + c // 16, 512))
    m["idx_ab"] = np.concatenate(cols, 1).astype(np.int32)
    cols = []
    for s_ in range(16):
        for hs in range(2):
            cols.append(grow(s_ * 128 + p, 2 * pb + hs, 256))
    m["idx_k"] = np.concatenate(cols, 1).astype(np.int32)
    cols = []
    for hp in range(4):
        for c in range(32):
            cols.append(grow(hp * 2048 + (c % 16) * 128 + p, 2 * pb + c // 16, 1024))
    m["idx_v"] = np.concatenate(cols, 1).astype(np.int32)
    return m


def _get(mode):
    if mode not in _CACHE:
        _CACHE[mode] = build(mode)
    return _CACHE[mode]


def _launch(mode, maps):
    nc, io = _get(mode)
    res = run_bass_kernel_spmd(nc, [{k: m[k] for k in io.ext_in} for m in maps], core_ids=list(range(len(maps))))
    return [{k: np.asarray(r[k]) for k in io.ext_out} for r in res.results]


def kernel(**inputs):
    inp = {k: np.asarray(v) for k, v in inputs.items()}
    maps = [_core_inputs(r, inp) for r in range(8)]
    if FUSED:
        rr = _launch("ALL", maps)
        global LAST_DBG
        LAST_DBG = [r["dbg"] for r in rr]
        out = np.zeros((4, 4096, 2048), np.float32)
        for r in range(8):
            out[r // 2, (r % 2) * 2048:(r % 2 + 1) * 2048] = rr[r]["out"]
        return out
    ra = _launch("A", maps)
    for r in range(8):
        p0, p1 = (r // 2) * 2, (r // 2) * 2 + 1
        maps[r]["modv"] = ra[r]["modv"]
        maps[r]["ycT"] = ra[r]["ycT"]
        maps[r]["ab_ctx"] = ra[r]["ab_ctx"]
        maps[r]["ab_full"] = np.concatenate([ra[p0]["ab_own"], ra[p1]["ab_own"]], 1)
    rb = _launch("B", maps)
    for r in range(8):
        p0, p1 = (r // 2) * 2, (r // 2) * 2 + 1
        for k in ["h_lat", "qT", "kT_ctx", "v_ctx"]:
            maps[r][k] = rb[r][k]
        maps[r]["kT_full"] = np.concatenate([rb[p0]["kT_own"], rb[p1]["kT_own"]], 2)
        maps[r]["v_full"] = np.concatenate([rb[p0]["v_own"], rb[p1]["v_own"]], 1)
    rc = _launch("C", maps)
    out = np.zeros((4, 4096, 2048), np.float32)
    for r in range(8):
        out[r // 2, (r % 2) * 2048:(r % 2 + 1) * 2048] = rc[r]["out"]
    return out
```

```python
import numpy as np
from contextlib import ExitStack
import concourse.bass as bass
import concourse.mybir as mybir
from concourse.bass_utils import run_bass_kernel_spmd

F32 = mybir.dt.float32
BF16 = mybir.dt.bfloat16
I32 = mybir.dt.int32
AF = mybir.ActivationFunctionType
ALU = mybir.AluOpType
AX = mybir.AxisListType

NDS = 48


class Prog:
    def __init__(self, nc, es):
        self.nc = nc
        self.es = es
        self.E = {"pe": nc.tensor, "act": nc.scalar, "dve": nc.vector, "pool": nc.gpsimd, "sp": nc.sync}
        self.csem = {e: es.enter_context(nc.semaphore(f"c_{e}")) for e in ["pe", "act", "dve", "pool"]}
        self.ccnt = {e: 0 for e in self.csem}
        self.dsems = [es.enter_context(nc.semaphore(f"d_{i}")) for i in range(NDS)]
        self.dcnt = [0] * NDS
        self.dnext = 0
        self.seen = {e: {} for e in self.E}
        self.lastw = {}
        self.readers = {}
        self.n_wait = 0
        self.xsem = es.enter_context(nc.semaphore("x_cc"))
        self.xcnt = 0

    def coll(self, fn, reads=(), writes=()):
        self._deps("pool", reads, writes)
        inst = fn(self.E["pool"])
        self.xcnt += 1
        inst.then_inc(self.xsem)
        self._commit((("x", 0), self.xcnt), reads, writes)

    def _wait(self, eng, tok):
        semkey, val = tok
        if semkey == ("c", "pe") and eng == "pe":
            return
        if self.seen[eng].get(semkey, 0) >= val:
            return
        sem = self.csem[semkey[1]] if semkey[0] == "c" else (self.xsem if semkey[0] == "x" else self.dsems[semkey[1]])
        self.E[eng].wait_ge(sem, val)
        self.n_wait += 1
        self.seen[eng][semkey] = val

    def _deps(self, eng, reads, writes, is_dma=False):
        for k in reads:
            for sk, v in self.lastw.get(k, {}).items():
                self._wait(eng, (sk, v))
        for k in writes:
            for sk, v in self.lastw.get(k, {}).items():
                if is_dma and sk[0] == "d":
                    continue
                self._wait(eng, (sk, v))
            for sk, v in self.readers.get(k, {}).items():
                self._wait(eng, (sk, v))

    def _commit(self, tok, reads, writes, is_dma=False):
        for k in reads:
            r = self.readers.setdefault(k, {})
            if r.get(tok[0], 0) < tok[1]:
                r[tok[0]] = tok[1]
        for k in writes:
            w = self.lastw.setdefault(k, {})
            if w.get(tok[0], 0) < tok[1]:
                w[tok[0]] = tok[1]
            if not is_dma:
                self.readers[k] = {}

    def op(self, eng, fn, reads=(), writes=()):
        self._deps(eng, reads, writes)
        inst = fn(self.E[eng])
        self.ccnt[eng] += 1
        inst.then_inc(self.csem[eng], 1)
        self._commit((("c", eng), self.ccnt[eng]), reads, writes)

    def op_if(self, eng, pred, fn, reads=(), writes=()):
        self._deps(eng, reads, writes)
        e = self.E[eng]
        with e.If(pred):
            inst = fn(e)
            inst.then_inc(self.csem[eng], 1)
        with e.Else():
            e.sem_inc(self.csem[eng], 1)
        self.ccnt[eng] += 1
        self._commit((("c", eng), self.ccnt[eng]), reads, writes)

    def dma_if(self, q, pred, fn, reads=(), writes=()):
        self._deps(q, reads, writes, is_dma=True)
        i = self.dnext
        self.dnext = (i + 1) % NDS
        if self.dcnt[i] > 0:
            self._wait(q, (("d", i), self.dcnt[i]))
        e = self.E[q]
        with e.If(pred):
            inst = fn(e)
            inst.then_inc(self.dsems[i], 16)
        with e.Else():
            e.sem_inc(self.dsems[i], 16)
        self.dcnt[i] += 16
        self._commit((("d", i), self.dcnt[i]), reads, writes, is_dma=True)

    def dma(self, q, fn, reads=(), writes=()):
        self._deps(q, reads, writes, is_dma=True)
        i = self.dnext
        self.dnext = (i + 1) % NDS
        if self.dcnt[i] > 0:
            self._wait(q, (("d", i), self.dcnt[i]))
        inst = fn(self.E[q])
        self.dcnt[i] += 16
        inst.then_inc(self.dsems[i], 16)
        self._commit((("d", i), self.dcnt[i]), reads, writes, is_dma=True)

    def finish(self, eng="sp"):
        for i in range(NDS):
            if self.dcnt[i] > 0:
                self._wait(eng, (("d", i), self.dcnt[i]))
        for e, c in self.ccnt.items():
            if c > 0:
                self._wait(eng, (("c", e), c))
        if self.xcnt > 0:
            self._wait(eng, (("x", 0), self.xcnt))


D = 2048
KD = 16
NLAT = 2048
NCTX = 256
NHALO = 128
NTOK0 = NLAT + NCTX
CAP = 1024
HCAP = 512
EPS = 1e-6
LAM_INIT1 = 0.8 - 0.6 * float(np.exp(-0.3 * 1))


class Ctx:
    pass


_UNIQ = [0]


def _T(nc, es, name, shape, dt):
    _UNIQ[0] += 1
    return es.enter_context(nc.sbuf_tensor(f"{name}_{_UNIQ[0]}", shape, dt))


def _PS(nc, es, name, shape, dt=F32):
    _UNIQ[0] += 1
    return es.enter_context(nc.psum_tensor(f"{name}_{_UNIQ[0]}", shape, dt))


def barrier(P):
    for e in ["pe", "act", "dve", "pool", "sp"]:
        P.finish(e)


def load_bcast(P, nc, es, name, src_row_ap, n=D, q="sp"):
    t = _T(nc, es, name, [128, n], F32)
    P.dma(q, lambda e: e.dma_start(out=t[:], in_=src_row_ap.partition_broadcast(128)), writes=[name])
    return t


def norm_block(P, K, src_rows, A, Bt, xf, ub, sq, st, uT=None, uT_off=0, psT=None, tag=""):
    A_t, A_k = A
    B_t, B_k = Bt
    xk, uk, sk = xf[1], ub[1], st[1]
    P.dma("sp", lambda e: e.dma_start(out=xf[0][:], in_=src_rows), writes=[xk])
    P.op("act", lambda e: e.activation(out=sq[0][:], in_=xf[0][:], func=AF.Square, accum_out=st[0][:, 0:1]),
         reads=[xk], writes=[sq[1], sk])
    P.op("act", lambda e: e.activation(out=st[0][:, 1:2], in_=st[0][:, 0:1], func=AF.Sqrt, scale=1.0 / D, bias=K.epsb[:, 0:1]),
         reads=[sk], writes=[sk])
    P.op("dve", lambda e: e.reciprocal(out=st[0][:, 2:3], in_=st[0][:, 1:2]), reads=[sk], writes=[sk])
    P.op("dve", lambda e: e.scalar_tensor_tensor(out=xf[0][:], in0=xf[0][:], scalar=st[0][:, 2:3], in1=A_t[:],
                                                  op0=ALU.mult, op1=ALU.mult), reads=[xk, sk, A_k], writes=[xk])
    P.op("pool", lambda e: e.tensor_tensor(out=ub[0][:], in0=xf[0][:], in1=B_t[:], op=ALU.add), reads=[xk, B_k], writes=[uk])
    if uT is not None:
        for half in range(2):
            pk = psT[half][1]

            def tr(e, half=half):
                for j in range(8):
                    k = half * 8 + j
                    i = e.transpose(out=psT[half][0][:, j, :], in_=ub[0][:, k * 128:(k + 1) * 128], identity=K.ident[:])
                return i
            P.op("pe", tr, reads=[uk], writes=[pk])
            eng = "act" if half == 0 else "dve"
            if eng == "act":
                P.op("act", lambda e, half=half: e.copy(out=uT[0][:, half * 8:(half + 1) * 8, uT_off:uT_off + 128], in_=psT[half][0][:]),
                     reads=[pk], writes=[uT[1]])
            else:
                P.op("dve", lambda e, half=half: e.tensor_copy(out=uT[0][:, half * 8:(half + 1) * 8, uT_off:uT_off + 128], in_=psT[half][0][:]),
                     reads=[pk], writes=[uT[1]])


def phase_mod(P, nc, K, io):
    es = ExitStack()
    K.mod_es = es
    cT = _T(nc, es, "m_cT", [128, KD, 2], F32)
    sT = _T(nc, es, "m_sT", [128, KD, 2], BF16)
    P.dma("sp", lambda e: e.dma_start(out=cT[:], in_=io.condT), writes=["m_cT"])
    P.op("act", lambda e: e.activation(out=sT[:], in_=cT[:], func=AF.Silu), reads=["m_cT"], writes=["m_sT"])
    wm = [_T(nc, es, f"m_w{i}", [128, KD, 512], BF16) for i in range(2)]
    bm = [_T(nc, es, f"m_b{i}", [2, 512], F32) for i in range(2)]
    ob = [_T(nc, es, f"m_o{i}", [2, 512], F32) for i in range(2)]
    ps = [_PS(nc, es, f"m_ps{i}", [2, 512]) for i in range(2)]
    cnt = [0]

    def work(l, cb):
        it = cnt[0]
        cnt[0] += 1
        wl = io.w_mod[l].rearrange("(k p) n -> p k n", p=128)
        w = wm[it % 2]
        wk = f"m_w{it % 2}"
        j = it % 2
        P.dma("pool", lambda e: e.dma_start(out=w[:], in_=wl[:, :, cb * 512:(cb + 1) * 512]), writes=[wk])
        P.dma("sp", lambda e: e.dma_start(out=bm[j][:], in_=io.b_mod[l:l + 1, cb * 512:(cb + 1) * 512].partition_broadcast(2)),
              writes=[f"m_b{j}"])

        def mm(e):
            for k in range(KD):
                i = e.matmul(ps[j][:], lhsT=sT[:, k, :], rhs=w[:, k, :], start=(k == 0), stop=(k == KD - 1))
            return i
        P.op("pe", mm, reads=[wk, "m_sT"], writes=[f"m_ps{j}"])
        P.op("dve", lambda e: e.tensor_tensor(out=ob[j][:], in0=ps[j][:], in1=bm[j][:], op=ALU.add),
             reads=[f"m_ps{j}", f"m_b{j}"], writes=[f"m_o{j}"])
        P.dma("sp", lambda e: e.dma_start(out=io.modv[l, :, cb * 512:(cb + 1) * 512], in_=ob[j][:]),
              reads=[f"m_o{j}"], writes=["modv"])
    for cb in range(24):
        work(0, cb)
    K.modq = [(lambda cb=cb: work(1, cb)) for cb in range(24)]
    barrier(P)


def drain_mod(P, K, n):
    q = getattr(K, "modq", None)
    while q and n > 0:
        q.pop(0)()
        n -= 1


def make_AB(P, nc, es, K, io, l, which, cond, pfx):
    base = which * 3 * D
    A = load_bcast(P, nc, es, pfx + "A", io.modv[l, cond:cond + 1, base + D:base + 2 * D])
    Bt = load_bcast(P, nc, es, pfx + "B", io.modv[l, cond:cond + 1, base:base + D])
    nw = load_bcast(P, nc, es, pfx + "nw", (io.norm1_w if which == 0 else io.norm2_w)[l:l + 1, :])
    P.op("dve", lambda e: e.scalar_tensor_tensor(out=A[:], in0=A[:], scalar=1.0, in1=nw[:], op0=ALU.add, op1=ALU.mult),
         reads=[pfx + "A", pfx + "nw"], writes=[pfx + "A"])
    return (A, pfx + "A"), (Bt, pfx + "B")


def phase_l0a(P, nc, K, io):
    NT = NLAT + NCTX + NHALO
    with ExitStack() as es:
        uT = (_T(nc, es, "a_uT", [128, KD, NT], BF16), "a_uT")
        with ExitStack() as es2:
            Al, Bl = make_AB(P, nc, es2, K, io, 0, 0, 0, "a_l")
            Ac, Bc = make_AB(P, nc, es2, K, io, 0, 0, 1, "a_c")
            xf = [(_T(nc, es2, f"a_xf{i}", [128, D], F32), f"a_xf{i}") for i in range(2)]
            ub = [(_T(nc, es2, f"a_ub{i}", [128, D], BF16), f"a_ub{i}") for i in range(2)]
            sq = (_T(nc, es2, "a_sq", [128, D], BF16), "a_sq")
            st = [(_T(nc, es2, f"a_st{i}", [128, 4], F32), f"a_st{i}") for i in range(2)]
            psT = [(_PS(nc, es2, f"a_pT{i}", [128, 8, 128], BF16), f"a_pT{i}") for i in range(2)]
            nb = NT // 128
            for b in range(nb):
                if b < 16:
                    src, A, Bt = io.x[b * 128:(b + 1) * 128, :], Al, Bl
                elif b < 18:
                    src, A, Bt = io.ctx[(b - 16) * 128:(b - 15) * 128, :], Ac, Bc
                else:
                    src, A, Bt = io.xh[:, :], Al, Bl
                norm_block(P, K, src, A, Bt, xf[b % 2], ub[b % 2], sq, st[b % 2], uT=uT, uT_off=b * 128, psT=psT)
                drain_mod(P, K, 2)
            barrier(P)
        win = io.even_w_in.rearrange("(k p) n -> p k n", p=128)
        wt = [(_T(nc, es, f"a_w{i}", [128, KD, 128], BF16), f"a_w{i}") for i in range(4)]
        pp = [(_PS(nc, es, f"a_pp{i}", [128, 512]), f"a_pp{i}") for i in range(6)]
        cvl = _T(nc, es, "a_cvl", [128, NLAT + 2], F32)
        cvc = _T(nc, es, "a_cvc", [128, NCTX + 2], F32)
        bg = _T(nc, es, "a_bg", [128, NLAT + NCTX], F32)
        cs = [(_T(nc, es, f"a_cs{i}", [128, 512], F32), f"a_cs{i}") for i in range(2)]
        t1 = _T(nc, es, "a_t1", [128, NLAT + NCTX], F32)
        yb = _T(nc, es, "a_yb", [128, NLAT + NCTX], BF16)
        cw = _T(nc, es, "a_cw", [128, 8, 3], F32)
        P.dma("sp", lambda e: e.dma_start(out=cw[:], in_=io.conv_wT), writes=["a_cw"])
        P.op("pool", lambda e: e.memset(cvl[:], 0.0), writes=["a_cvl"])
        P.op("pool", lambda e: e.memset(cvc[:], 0.0), writes=["a_cvc"])
        tblocks = [(0, 512), (512, 512), (1024, 512), (1536, 512), (2048, 256), (2304, 128)]
        wi = 0
        ppi = 0
        fT = _T(nc, es, "a_fT", [128, 2, NLAT + NCTX], BF16)
        csd = _T(nc, es, "a_csd", [128, 2, 512], BF16)
        P.dma("sp", lambda e: e.dma_start(out=csd[:], in_=io.cs256), writes=["a_csd"])
        abt = [(_T(nc, es, f"a_ab{i}", [128, 512], BF16), f"a_ab{i}") for i in range(2)]
        for g in range(4):
            for hh in range(2):
                w, wk = wt[wi % 4]
                wi += 1
                col = 3072 + g * 256 + hh * 128
                P.dma("pool", lambda e, w=w, col=col: e.dma_start(out=w[:], in_=win[:, :, col:col + 128]), writes=[wk])
                for ti, (t0, tn) in enumerate(tblocks[:5]):
                    p_, pk = pp[ppi % 6]
                    ppi += 1

                    def mm(e, p_=p_, w=w):
                        for k in range(KD):
                            i = e.matmul(p_[:, 0:tn], lhsT=w[:, k, :], rhs=uT[0][:, k, t0:t0 + tn], start=(k == 0), stop=(k == KD - 1))
                        return i
                    P.op("pe", mm, reads=[wk, "a_uT"], writes=[pk])
                    P.op("act", lambda e: e.copy(out=fT[:, hh, t0:t0 + tn], in_=p_[:, 0:tn]), reads=[pk], writes=["a_fT"])
            for b in range(18):
                p_, pk = pp[ppi % 6]
                ppi += 1

                def mm2(e, p_=p_):
                    for hh in range(2):
                        i = e.matmul(p_[:], lhsT=fT[:, hh, b * 128:(b + 1) * 128], rhs=csd[:, hh, :], start=(hh == 0), stop=(hh == 1))
                    return i
                P.op("pe", mm2, reads=["a_fT", "a_csd"], writes=[pk])
                a_, ak = abt[b % 2]
                if b % 2 == 0:
                    P.op("act", lambda e: e.copy(out=a_[:], in_=p_[:]), reads=[pk], writes=[ak])
                else:
                    P.op("dve", lambda e: e.tensor_copy(out=a_[:], in_=p_[:]), reads=[pk], writes=[ak])
                if b < 16:
                    P.dma("sp", lambda e: e.dma_start(out=io.ab_own[g // 2, b * 128:(b + 1) * 128, (g % 2) * 512:(g % 2 + 1) * 512], in_=a_[:]),
                          reads=[ak], writes=["ab_own"])
                else:
                    P.dma("sp", lambda e: e.dma_start(out=io.ab_ctx[(b - 16) * 128:(b - 15) * 128, g * 512:(g + 1) * 512], in_=a_[:]),
                          reads=[ak], writes=["ab_ctx"])
        pend = _gather_thunks(P, io.ab_own.rearrange("g t n -> (g t) n"), io.ab4, 4096, 512, "ab_own", "ab4") if io.mode == "ALL" else []
        for c in range(8):
            ws = []
            for part in range(3):
                w, wk = wt[wi % 4]
                wi += 1
                col = part * 1024 + c * 128
                P.dma("pool", lambda e, w=w, col=col: e.dma_start(out=w[:], in_=win[:, :, col:col + 128]), writes=[wk])
                ws.append((w, wk))
            if pend:
                pend.pop(0)()
            drain_mod(P, K, 3)
            for ti, (t0, tn) in enumerate(tblocks):
                pb = []
                for part in range(3):
                    p_, pk = pp[ppi % 6]
                    ppi += 1
                    w, wk = ws[part]

                    def mm(e, p_=p_, w=w):
                        for k in range(KD):
                            i = e.matmul(p_[:, 0:tn], lhsT=w[:, k, :], rhs=uT[0][:, k, t0:t0 + tn], start=(k == 0), stop=(k == KD - 1))
                        return i
                    P.op("pe", mm, reads=[wk, "a_uT"], writes=[pk])
                    pb.append((p_, pk))
                c_, ck = cs[ti % 2]
                P.op("act", lambda e: e.copy(out=c_[:, 0:tn], in_=pb[1][0][:, 0:tn]), reads=[pb[1][1]], writes=[ck])
                if ti < 4:
                    dst = cvl[:, 1 + t0:1 + t0 + tn]
                    dk = "a_cvl"
                elif ti == 4:
                    dst = cvc[:, 1:1 + tn]
                    dk = "a_cvc"
                else:
                    dst = None
                if dst is not None:
                    P.op("dve", lambda e: e.tensor_tensor(out=dst, in0=c_[:, 0:tn], in1=pb[2][0][:, 0:tn], op=ALU.mult),
                         reads=[ck, pb[2][1]], writes=[dk])
                    P.op("act", lambda e: e.copy(out=bg[:, t0:t0 + tn], in_=pb[0][0][:, 0:tn]), reads=[pb[0][1]], writes=["a_bg"])
                else:
                    P.op("dve", lambda e: e.tensor_tensor(out=c_[:, 0:1], in0=c_[:, 0:1], in1=pb[2][0][:, 0:1], op=ALU.mult),
                         reads=[ck, pb[2][1]], writes=[ck])
                    P.op("dve", lambda e: e.tensor_scalar(out=cvl[:, 0:1], in0=c_[:, 0:1], scalar1=K.flags[:, 0:1], scalar2=None, op0=ALU.mult),
                         reads=[ck], writes=["a_cvl"])
                    P.op("dve", lambda e: e.tensor_scalar(out=cvl[:, NLAT + 1:NLAT + 2], in0=c_[:, 0:1], scalar1=K.flags[:, 1:2], scalar2=None, op0=ALU.mult),
                         reads=[ck], writes=["a_cvl"])
            for (cv, cvk, o0, n) in [(cvl, "a_cvl", 0, NLAT), (cvc, "a_cvc", NLAT, NCTX)]:
                tt = t1[:, o0:o0 + n]
                P.op("dve", lambda e: e.tensor_scalar(out=tt, in0=cv[:, 1:n + 1], scalar1=cw[:, c, 1:2], scalar2=None, op0=ALU.mult),
                     reads=[cvk, "a_cw"], writes=["a_t1"])
                P.op("dve", lambda e: e.scalar_tensor_tensor(out=tt, in0=cv[:, 0:n], scalar=cw[:, c, 0:1], in1=tt, op0=ALU.mult, op1=ALU.add),
                     reads=[cvk, "a_t1"], writes=["a_t1"])
                P.op("dve", lambda e: e.scalar_tensor_tensor(out=tt, in0=cv[:, 2:n + 2], scalar=cw[:, c, 2:3], in1=tt, op0=ALU.mult, op1=ALU.add),
                     reads=[cvk, "a_t1"], writes=["a_t1"])
            P.op("pool", lambda e: e.tensor_tensor(out=yb[:], in0=t1[:], in1=bg[:], op=ALU.mult), reads=["a_t1", "a_bg"], writes=["a_yb"])
            P.dma("sp", lambda e: e.dma_start(out=io.ycT[c], in_=yb[:]), reads=["a_yb"], writes=["ycT"])
    drain_mod(P, K, 100)
    barrier(P)
    if getattr(K, "mod_es", None) is not None:
        K.mod_es.close()
        K.mod_es = None


PHASES_OF = {"A": ["mod", "l0a"], "B": ["l0b", "op0", "moe0", "l1a"], "C": ["attn", "op1", "moe1"],
             "ALL": ["mod", "l0a", "xab", "l0b", "op0", "moe0", "l1a", "xkv", "attn", "op1", "moe1"]}
LAUNCH_OF = {"mod": "A", "l0a": "A", "l0b": "B", "op0": "B", "moe0": "B", "l1a": "B", "attn": "C", "op1": "C", "moe1": "C"}


def declare(nc, mode):
    io = Ctx()
    io.ext_in, io.ext_out = [], []

    def inp(name, shape, dt, launches):
        if mode != "ALL" and mode not in launches:
            return None
        io.ext_in.append(name)
        return nc.dram_tensor(name, shape, dt, kind="ExternalInput").ap()

    def mid(name, shape, dt, prod, cons, scratch=""):
        if mode == "ALL" or mode in scratch:
            return nc.dram_tensor(name, shape, dt, kind="Internal").ap()
        if mode == prod:
            io.ext_out.append(name)
            return nc.dram_tensor(name, shape, dt, kind="ExternalOutput").ap()
        if mode in cons:
            io.ext_in.append(name)
            return nc.dram_tensor(name, shape, dt, kind="ExternalInput").ap()
        return None

    io.x = inp("x", [NLAT, D], F32, "AB")
    io.ctx = inp("ctx", [NCTX, D], F32, "AB")
    io.xh = inp("xh", [128, D], F32, "A")
    io.condT = inp("condT", [128, KD, 2], F32, "A")
    io.flags = inp("flags", [128, 2], F32, "A")
    io.w_mod = inp("w_mod", [2, D, 6 * D], F32, "A")
    io.b_mod = inp("b_mod", [2, 6 * D], F32, "A")
    io.norm1_w = inp("norm1_w", [2, D], F32, "AB")
    io.norm2_w = inp("norm2_w", [2, D], F32, "BC")
    io.even_w_in = inp("even_w_in", [D, 4096], F32, "A")
    io.conv_wT = inp("conv_wT", [128, 8, 3], F32, "A")
    io.even_w_out = inp("even_w_out", [D, D], F32, "B")
    io.odd_w_qkv = inp("odd_w_qkv", [D, 6144], F32, "B")
    io.qk_norm = inp("qk_norm", [128, 2], F32, "B")
    io.lam_vecs = inp("lam_vecs", [1, 4, 128], F32, "C")
    io.subln_wT = inp("subln_wT", [128, 2], F32, "C")
    io.odd_w_out = inp("odd_w_out", [D, D], F32, "C")
    io.moe_wr = [inp(f"moe_wr{l}", [D, 20], F32, "BC"[l]) for l in range(2)]
    io.moe_br = inp("moe_br", [2, 20], F32, "BC")
    io.moe_w_gate = [inp(f"moe_w_gate{l}", [16, D, 512], F32, "BC"[l]) for l in range(2)]
    io.moe_w_up = [inp(f"moe_w_up{l}", [16, D, 512], F32, "BC"[l]) for l in range(2)]
    io.moe_w_down = [inp(f"moe_w_down{l}", [16, 512, D], F32, "BC"[l]) for l in range(2)]
    io.ident = inp("ident", [128, 128], BF16, "ABC")
    io.cs256 = inp("cs256", [128, 2, 512], BF16, "A")
    io.dft_c = inp("dft_c", [4096, NLAT], BF16, "B")
    io.dft_s = inp("dft_s", [4096, NLAT], BF16, "B")
    io.dft256 = inp("dft256", [128, 2, 2, 256], BF16, "B")
    io.rope_cs = inp("rope_cs", [2, 128, NLAT], F32, "B")
    io.rotT = inp("rotT", [128, 128], BF16, "B")
    io.tri = inp("tri", [128, 128], BF16, "BC")
    io.modv = mid("modv", [2, 2, 6 * D], F32, "A", "BC")
    io.ycT = mid("ycT", [8, 128, NLAT + NCTX], BF16, "A", "B")
    io.ab_own = mid("ab_own", [2, NLAT, 1024], BF16, "A", "")
    io.ab_ctx = mid("ab_ctx", [NCTX, D], BF16, "A", "B")
    io.ab_full = mid("ab_full", [2, 2 * NLAT, 1024], BF16, "X", "B") if mode != "ALL" else None
    io.mode = mode
    if mode == "ALL":
        io.ab4 = nc.dram_tensor("ab4", [4 * 2 * NLAT, 1024], BF16, kind="Internal").ap()
        io.k4 = nc.dram_tensor("k4", [4 * 16 * 128, NLAT], BF16, kind="Internal").ap()
        io.v4 = nc.dram_tensor("v4", [4 * 4 * NLAT, 512], BF16, kind="Internal").ap()
    io.idx_ab = inp("idx_ab", [128, 64], I32, "")
    io.idx_k = inp("idx_k", [128, 32], I32, "")
    io.idx_v = inp("idx_v", [128, 128], I32, "")
    io.yfT = mid("yfT", [8, 128, NLAT + NCTX], BF16, "-", "", "B")
    io.h_lat = mid("h_lat", [NLAT, D], F32, "B", "C")
    io.h_ctx = mid("h_ctx", [NCTX, D], F32, "-", "", "B")
    io.h_mid = mid("h_mid", [NLAT, D], F32, "-", "", "BC")
    io.xg = mid("xg", [16 * CAP, D], BF16, "-", "", "BC")
    io.yg = mid("yg", [16 * CAP, D], BF16, "-", "", "BC")
    io.qT = mid("qT", [16, 128, NLAT], BF16, "B", "C")
    io.kT_own = mid("kT_own", [16, 128, NLAT], BF16, "B", "")
    io.kT_ctx = mid("kT_ctx", [16, 128, NCTX], BF16, "B", "C")
    io.v_own = mid("v_own", [4, NLAT, 512], BF16, "B", "")
    io.v_ctx = mid("v_ctx", [NCTX, D], BF16, "B", "C")
    io.kT_full = mid("kT_full", [16, 128, 2 * NLAT], BF16, "X", "C") if mode != "ALL" else None
    io.v_full = mid("v_full", [4, 2 * NLAT, 512], BF16, "X", "C") if mode != "ALL" else None
    io.oT = mid("oT", [16, 128, NLAT], BF16, "-", "", "C")
    io.out = mid("out", [NLAT, D], F32, "C", "") if mode != "ALL" else None
    io.dbg = None
    if mode == "ALL":
        io.ext_out.append("dbg")
        io.dbg = nc.dram_tensor("dbg", [2, 128, 16], F32, kind="ExternalOutput").ap()
        io.ext_out.append("out")
        io.out = nc.dram_tensor("out", [NLAT, D], F32, kind="ExternalOutput").ap()
    return io


def build(mode, phases=None):
    nc = bass.Bass("TRN2", target_bir_lowering=False)
    io = declare(nc, mode)
    for nm in ["xg", "yg", "yfT", "oT", "h_ctx", "ab_own", "kT_own", "v_own"]:
        if getattr(io, nm) is None and mode in "BC":
            pass
    es = ExitStack()
    with es:
        P = Prog(nc, es)
        K = Ctx()
        K.ident = _T(nc, es, "k_ident", [128, 128], BF16)
        K.epsb = _T(nc, es, "k_eps", [128, 1], F32)
        K.ones = _T(nc, es, "k_ones", [128, 128], BF16)
        K.flags = _T(nc, es, "k_flags", [128, 2], F32)
        P.dma("sp", lambda e: e.dma_start(out=K.ident[:], in_=io.ident), writes=["k_ident"])
        P.op("pool", lambda e: e.memset(K.epsb[:], EPS), writes=["k_eps"])
        P.op("pool", lambda e: e.memset(K.ones[:], 1.0), writes=["k_ones"])
        if io.flags is not None:
            P.dma("sp", lambda e: e.dma_start(out=K.flags[:], in_=io.flags), writes=["k_flags"])
        K.breg = nc.gpsimd.to_reg(16 * CAP - 1)
        K.breg_big = nc.gpsimd.to_reg(4 * 16 * 128 * 16 - 1)
        barrier(P)
        for ph in (phases or PHASES_OF[mode]):
            PHASE_FN[ph](P, nc, K, io)
            barrier(P)

        print(f"[build {mode}] waits={P.n_wait} counts={P.ccnt}")
    return nc, io


PHASE_FN = {"mod": phase_mod, "l0a": phase_l0a}


def phase_l0b(P, nc, K, io):
    with ExitStack() as es:
        abps = [_T(nc, es, f"b_abp{i}", [128, 32, 1024], BF16) for i in range(2)]
        dc = [(_T(nc, es, f"b_dc{i}", [128, 8, 512], BF16), f"b_dc{i}") for i in range(2)]
        ds = [(_T(nc, es, f"b_ds{i}", [128, 8, 512], BF16), f"b_ds{i}") for i in range(2)]
        acc = [(_PS(nc, es, f"b_acc{i}", [128, 512]), f"b_acc{i}") for i in range(8)]
        yo = [(_T(nc, es, f"b_yo{i}", [128, 512], BF16), f"b_yo{i}") for i in range(2)]
        if io.mode == "ALL":
            ixab = _T(nc, es, "b_ixab", [128, 64], I32)
            P.dma("sp", lambda e: e.dma_start(out=ixab[:], in_=io.idx_ab), writes=["b_ixab"])
        dcv = io.dft_c.rearrange("(c p) n -> p c n", p=128)
        dsv = io.dft_s.rearrange("(c p) n -> p c n", p=128)
        di = 0
        yi = 0
        ai = 0
        for gp in range(2):
            abp = abps[gp]
            if io.mode == "ALL" and gp == 0:
              for gq in range(2):
                for c in range(32):
                    P.dma("pool", lambda e: e.indirect_dma_start(out=abps[gq][:, c, :], out_offset=None, in_=io.ab4,
                                                                  in_offset=bass.IndirectOffsetOnAxis(ap=ixab[:, gq * 32 + c:gq * 32 + c + 1], axis=0),
                                                                  bounds_check=K.breg_big, oob_is_err=False), reads=[f"ab4_{q_}" for q_ in range(8)] + ["b_ixab"], writes=[f"b_abp{gq}"])
            elif io.mode != "ALL":
                abv = io.ab_full[gp].rearrange("(c p) n -> p c n", p=128)
                for q4 in range(4):
                    P.dma("sp", lambda e: e.dma_start(out=abp[:, q4 * 8:(q4 + 1) * 8, :], in_=abv[:, q4 * 8:(q4 + 1) * 8, :]), writes=[f"b_abp{gp}"])
            for nb in range(4):
                banks = [acc[(ai + a) % 8] for a in range(4)]
                ai += 4
                for tq in range(4):
                    c_, ck = dc[di % 2]
                    s_, sk = ds[di % 2]
                    di += 1
                    P.dma("sp", lambda e: e.dma_start(out=c_[:], in_=dcv[:, tq * 8:(tq + 1) * 8, nb * 512:(nb + 1) * 512]), writes=[ck])
                    P.dma("sp", lambda e: e.dma_start(out=s_[:], in_=dsv[:, tq * 8:(tq + 1) * 8, nb * 512:(nb + 1) * 512]), writes=[sk])

                    def mm(e):
                        for tc in range(8):
                            t = tq * 8 + tc
                            for a in range(4):
                                gl, hh = a // 2, a % 2
                                first = (tq == 0 and tc == 0)
                                last = (tq == 3 and tc == 7)
                                e.matmul(banks[a][0][:], lhsT=abp[:, t, gl * 512 + hh * 128:gl * 512 + hh * 128 + 128], rhs=c_[:, tc, :],
                                         start=first, stop=False)
                                i = e.matmul(banks[a][0][:], lhsT=abp[:, t, gl * 512 + 256 + hh * 128:gl * 512 + 256 + hh * 128 + 128], rhs=s_[:, tc, :],
                                             start=False, stop=last)
                        return i
                    P.op("pe", mm, reads=[f"b_abp{gp}", ck, sk], writes=[b[1] for b in banks])
                for a in range(4):
                    y_, yk = yo[yi % 2]
                    yi += 1
                    if a % 2 == 0:
                        P.op("act", lambda e: e.copy(out=y_[:], in_=banks[a][0][:]), reads=[banks[a][1]], writes=[yk])
                    else:
                        P.op("dve", lambda e: e.tensor_copy(out=y_[:], in_=banks[a][0][:]), reads=[banks[a][1]], writes=[yk])
                    ch = (gp * 2 + a // 2) * 2 + a % 2
                    P.dma("sp", lambda e: e.dma_start(out=io.yfT[ch, :, nb * 512:(nb + 1) * 512], in_=y_[:]), reads=[yk], writes=["yfT"])
        barrier(P)
        abc = _T(nc, es, "b_abc", [128, 2, D], BF16)
        d2 = _T(nc, es, "b_d2", [128, 2, 2, 256], BF16)
        P.dma("sp", lambda e: e.dma_start(out=abc[:], in_=io.ab_ctx.rearrange("(c p) n -> p c n", p=128)), writes=["b_abc"])
        P.dma("sp", lambda e: e.dma_start(out=d2[:], in_=io.dft256), writes=["b_d2"])
        for ch in range(8):
            g, hh = ch // 2, ch % 2
            bk, bkk = acc[ch % 8]

            def mmc(e):
                n = 0
                for t in range(2):
                    for part in range(2):
                        i = e.matmul(bk[:, 0:256], lhsT=abc[:, t, g * 512 + part * 256 + hh * 128:g * 512 + part * 256 + hh * 128 + 128],
                                     rhs=d2[:, t, part, :], start=(n == 0), stop=(n == 3))
                        n += 1
                return i
            P.op("pe", mmc, reads=["b_abc", "b_d2"], writes=[bkk])
            y_, yk = yo[ch % 2]
            P.op("act", lambda e: e.copy(out=y_[:, 0:256], in_=bk[:, 0:256]), reads=[bkk], writes=[yk])
            P.dma("sp", lambda e: e.dma_start(out=io.yfT[ch, :, NLAT:NLAT + NCTX], in_=y_[:, 0:256]), reads=[yk], writes=["yfT"])
    barrier(P)


def phase_op(P, nc, K, io, l, srcs, w_dram, segs):
    with ExitStack() as es:
        wo = _T(nc, es, "o_w", [128, KD, D], BF16)
        wv = w_dram.rearrange("(k p) n -> p k n", p=128)
        for q4 in range(4):
            P.dma("pool", lambda e: e.dma_start(out=wo[:, q4 * 4:(q4 + 1) * 4, :], in_=wv[:, q4 * 4:(q4 + 1) * 4, :]), writes=["o_w"])
        Gs = {}
        for cond in sorted(set(s[2] for s in segs)):
            Gs[cond] = load_bcast(P, nc, es, f"o_G{cond}", io.modv[l, cond:cond + 1, 2 * D:3 * D])
        yt = [(_T(nc, es, f"o_yt{i}", [128, KD, 512], BF16), f"o_yt{i}") for i in range(2)]
        hx = [(_T(nc, es, f"o_hx{i}", [128, D], F32), f"o_hx{i}") for i in range(2)]
        mg = [(_T(nc, es, f"o_mg{i}", [128, D], F32), f"o_mg{i}") for i in range(2)]
        ps = [(_PS(nc, es, f"o_ps{i}", [128, 512]), f"o_ps{i}") for i in range(8)]
        si = 0
        bi = 0
        pi = 0
        for (tok0, ntok, cond, h_src, h_dst) in segs:
            for s0 in range(0, ntok, 512):
                sn = min(512, ntok - s0)
                y_, yk = yt[si % 2]
                si += 1
                for half, src in enumerate(srcs):
                    P.dma("sp", lambda e: e.dma_start(out=y_[:, half * 8:(half + 1) * 8, 0:sn],
                                                      in_=src[:, :, tok0 + s0:tok0 + s0 + sn].rearrange("c p t -> p c t")), writes=[yk])
                for b in range(sn // 128):
                    r0 = s0 + b * 128
                    h_, hk = hx[bi % 2]
                    m_, mk = mg[bi % 2]
                    bi += 1
                    P.dma("sp", lambda e: e.dma_start(out=h_[:], in_=h_src[r0:r0 + 128, :]), writes=[hk])
                    for cb in range(4):
                        p_, pk = ps[pi % 8]
                        pi += 1

                        def mm(e):
                            for c in range(KD):
                                i = e.matmul(p_[:], lhsT=y_[:, c, b * 128:(b + 1) * 128], rhs=wo[:, c, cb * 512:(cb + 1) * 512],
                                             start=(c == 0), stop=(c == KD - 1))
                            return i
                        P.op("pe", mm, reads=[yk, "o_w"], writes=[pk])
                        P.op("dve", lambda e: e.tensor_tensor(out=m_[:, cb * 512:(cb + 1) * 512], in0=p_[:], in1=Gs[cond][:, cb * 512:(cb + 1) * 512], op=ALU.mult),
                             reads=[pk, f"o_G{cond}"], writes=[mk])
                    P.op("pool", lambda e: e.tensor_tensor(out=m_[:], in0=m_[:], in1=h_[:], op=ALU.add), reads=[mk, hk], writes=[mk])
                    P.dma("sp", lambda e: e.dma_start(out=h_dst[r0:r0 + 128, :], in_=m_[:]), reads=[mk], writes=["hdst"])
    barrier(P)


def phase_op0(P, nc, K, io):
    phase_op(P, nc, K, io, 0, [io.ycT, io.yfT], io.even_w_out,
             [(0, NLAT, 0, io.x, io.h_mid), (NLAT, NCTX, 1, io.ctx, io.h_ctx)])


def phase_op1(P, nc, K, io):
    oT = io.oT
    phase_op(P, nc, K, io, 1, [oT[0:8], oT[8:16]], io.odd_w_out, [(0, NLAT, 0, io.h_lat, io.h_mid)])


def phase_moe(P, nc, K, io, l, segs):
    blocks = []
    for (ntok, cond, h_src, h_dst) in segs:
        for b in range(ntok // 128):
            blocks.append((cond, h_src[b * 128:(b + 1) * 128, :], h_dst[b * 128:(b + 1) * 128, :]))
    nb = len(blocks)
    NJ = HCAP // 128
    with ExitStack() as es:
        idx1 = _T(nc, es, "e_idx1", [128, nb], I32)
        idx2 = _T(nc, es, "e_idx2", [128, nb], I32)
        wts = _T(nc, es, "e_wts", [128, 2, nb], F32)
        cnti = _T(nc, es, "e_cnti", [128, 16], I32)
        Gs = {}
        with ExitStack() as es2:
            ABs = {}
            for cond in sorted(set(b[0] for b in blocks)):
                ABs[cond] = make_AB(P, nc, es2, K, io, l, 1, cond, f"e_c{cond}")
            xf = [(_T(nc, es2, f"e_xf{i}", [128, D], F32), f"e_xf{i}") for i in range(2)]
            ub = [(_T(nc, es2, f"e_ub{i}", [128, D], BF16), f"e_ub{i}") for i in range(3)]
            sq = (_T(nc, es2, "e_sq", [128, D], BF16), "e_sq")
            st = [(_T(nc, es2, f"e_st{i}", [128, 4], F32), f"e_st{i}") for i in range(2)]
            vT = [(_T(nc, es2, f"e_vT{i}", [128, KD, 128], BF16), f"e_vT{i}") for i in range(2)]
            psT = [(_PS(nc, es2, f"e_pT{i}", [128, 8, 128], BF16), f"e_pT{i}") for i in range(2)]
            pl = [(_PS(nc, es2, f"e_pl{i}", [128, 64]), f"e_pl{i}") for i in range(2)]
            wr = _T(nc, es2, "e_wr", [128, KD, 20], BF16)
            br = _T(nc, es2, "e_br", [128, 20], F32)
            tri = _T(nc, es2, "e_tri", [128, 128], BF16)
            ecap = _T(nc, es2, "e_ecap", [128, 16], F32)
            base = _T(nc, es2, "e_base", [128, 16], F32)
            P.dma("pool", lambda e: e.dma_start(out=wr[:], in_=io.moe_wr[l].rearrange("(k p) n -> p k n", p=128)), writes=["e_wr"])
            P.dma("sp", lambda e: e.dma_start(out=br[:], in_=io.moe_br[l:l + 1, :].partition_broadcast(128)), writes=["e_br"])
            P.dma("sp", lambda e: e.dma_start(out=tri[:], in_=io.tri), writes=["e_tri"])
            P.op("pool", lambda e: e.iota(ecap[:], pattern=[[CAP, 16]], base=0, channel_multiplier=0, allow_small_or_imprecise_dtypes=True), writes=["e_ecap"])
            P.op("pool", lambda e: e.memset(base[:], 0.0), writes=["e_base"])
            sm = [(_T(nc, es2, f"e_sm{i}", [128, 128], F32), f"e_sm{i}") for i in range(2)]
            ac = [(_T(nc, es2, f"e_ac{i}", [128, 16], BF16), f"e_ac{i}") for i in range(2)]
            def route(bi, cond, src, dst):
                    A, Bt = ABs[cond]
                    u_, uk = ub[bi % 3]
                    v_, vk = vT[bi % 2]
                    norm_block(P, K, src, A, Bt, xf[bi % 2], (u_, uk), sq, st[bi % 2], uT=(v_, vk), uT_off=0, psT=psT)
                    p_, pk = pl[bi % 2]
                    s_, smk = sm[bi % 2]
                    a_, ak = ac[bi % 2]

                    def mm(e):
                        for k in range(KD):
                            i = e.matmul(p_[:, 0:20], lhsT=v_[:, k, :], rhs=wr[:, k, :], start=(k == 0), stop=(k == KD - 1))
                        return i
                    P.op("pe", mm, reads=[vk, "e_wr"], writes=[pk])
                    R = [smk]

                    def dv(fn, extra_r=(), extra_w=()):
                        P.op("dve", fn, reads=R + list(extra_r), writes=R + list(extra_w))
                    lg = s_[:, 0:20]
                    gm, ngm, gs, gw = s_[:, 20:21], s_[:, 21:22], s_[:, 22:23], s_[:, 23:24]
                    mgk = s_[:, 24:28]
                    esel = s_[:, 28:32]
                    m1, m2 = s_[:, 32:33], s_[:, 33:34]
                    o1, o2 = s_[:, 34:38], s_[:, 38:42]
                    es2_ = s_[:, 42:46]
                    dd, ed = s_[:, 46:47], s_[:, 47:48]
                    E1, E2 = s_[:, 48:64], s_[:, 64:80]
                    pos, posc, tmp16 = s_[:, 80:96], s_[:, 96:112], s_[:, 112:128]
                    dv(lambda e: e.tensor_tensor(out=lg, in0=p_[:, 0:20], in1=br[:], op=ALU.add), extra_r=[pk, "e_br"])
                    yield
                    dv(lambda e: e.tensor_reduce(out=gm, in_=s_[:, 0:4], axis=AX.X, op=ALU.max))
                    yield
                    dv(lambda e: e.tensor_scalar(out=ngm, in0=gm, scalar1=-1.0, scalar2=None, op0=ALU.mult))
                    yield
                    P.op("act", lambda e: e.activation(out=tmp16[:, 0:4], in_=s_[:, 0:4], func=AF.Exp, bias=ngm, scale=1.0, accum_out=gs), reads=R, writes=R)
                    yield
                    dv(lambda e: e.reciprocal(out=gw, in_=gs))
                    yield
                    dv(lambda e: e.tensor_scalar(out=mgk, in0=s_[:, 0:4], scalar1=gm, scalar2=None, op0=ALU.is_equal))
                    yield
                    dv(lambda e: e.tensor_scalar(out=esel, in0=s_[:, 4:8], scalar1=s_[:, 24:25], scalar2=None, op0=ALU.mult))
                    yield
                    for g in range(1, 4):
                        dv(lambda e: e.scalar_tensor_tensor(out=esel, in0=s_[:, 4 + 4 * g:8 + 4 * g], scalar=s_[:, 24 + g:25 + g], in1=esel, op0=ALU.mult, op1=ALU.add))
                        yield
                    dv(lambda e: e.tensor_reduce(out=m1, in_=esel, axis=AX.X, op=ALU.max))
                    yield
                    dv(lambda e: e.tensor_scalar(out=o1, in0=esel, scalar1=m1, scalar2=None, op0=ALU.is_equal))
                    yield
                    dv(lambda e: e.scalar_tensor_tensor(out=es2_, in0=o1, scalar=-1.0e30, in1=esel, op0=ALU.mult, op1=ALU.add))
                    yield
                    dv(lambda e: e.tensor_reduce(out=m2, in_=es2_, axis=AX.X, op=ALU.max))
                    yield
                    dv(lambda e: e.tensor_scalar(out=o2, in0=es2_, scalar1=m2, scalar2=None, op0=ALU.is_equal))
                    yield
                    dv(lambda e: e.tensor_tensor(out=dd, in0=m2, in1=m1, op=ALU.subtract))
                    yield
                    P.op("act", lambda e: e.activation(out=ed, in_=dd, func=AF.Exp), reads=R, writes=R)
                    yield
                    dv(lambda e: e.tensor_scalar(out=ed, in0=ed, scalar1=1.0, scalar2=None, op0=ALU.add))
                    yield
                    dv(lambda e: e.reciprocal(out=ed, in_=ed))
                    yield
                    dv(lambda e: e.tensor_tensor(out=wts[:, 0, bi:bi + 1], in0=gw, in1=ed, op=ALU.mult), extra_w=["e_wts"])
                    yield
                    dv(lambda e: e.tensor_tensor(out=wts[:, 1, bi:bi + 1], in0=gw, in1=wts[:, 0, bi:bi + 1], op=ALU.subtract), extra_r=["e_wts"], extra_w=["e_wts"])
                    yield
                    for g in range(4):
                        dv(lambda e: e.tensor_scalar(out=s_[:, 48 + 4 * g:52 + 4 * g], in0=o1, scalar1=s_[:, 24 + g:25 + g], scalar2=None, op0=ALU.mult))
                        yield
                        dv(lambda e: e.tensor_scalar(out=s_[:, 64 + 4 * g:68 + 4 * g], in0=o2, scalar1=s_[:, 24 + g:25 + g], scalar2=None, op0=ALU.mult))
                        yield
                    dv(lambda e: e.tensor_tensor(out=a_[:], in0=E1, in1=E2, op=ALU.add), extra_w=[ak])
                    yield

                    def mm2(e):
                        e.matmul(p_[:, 32:48], lhsT=tri[:], rhs=a_[:], start=True, stop=True)
                        return e.matmul(p_[:, 48:64], lhsT=K.ones[:], rhs=a_[:], start=True, stop=True)
                    P.op("pe", mm2, reads=[ak, "e_tri", "k_ones"], writes=[pk])
                    dv(lambda e: e.tensor_tensor(out=pos, in0=p_[:, 32:48], in1=base[:], op=ALU.add), extra_r=[pk, "e_base"])
                    yield
                    dv(lambda e: e.tensor_tensor(out=base[:], in0=p_[:, 48:64], in1=base[:], op=ALU.add), extra_r=[pk, "e_base"], extra_w=["e_base"])
                    yield
                    dv(lambda e: e.tensor_scalar(out=tmp16, in0=pos, scalar1=float(CAP), scalar2=1.0e6, op0=ALU.is_ge, op1=ALU.mult))
                    yield
                    dv(lambda e: e.tensor_tensor(out=posc, in0=pos, in1=ecap[:], op=ALU.add), extra_r=["e_ecap"])
                    yield
                    dv(lambda e: e.tensor_tensor(out=posc, in0=posc, in1=tmp16, op=ALU.add))
                    yield
                    dv(lambda e: e.tensor_tensor(out=tmp16, in0=posc, in1=E1, op=ALU.mult))
                    yield
                    dv(lambda e: e.tensor_reduce(out=dd, in_=tmp16, axis=AX.X, op=ALU.add))
                    yield
                    dv(lambda e: e.tensor_copy(out=idx1[:, bi:bi + 1], in_=dd), extra_w=[f"e_i1_{bi}"])
                    yield
                    dv(lambda e: e.tensor_tensor(out=tmp16, in0=posc, in1=E2, op=ALU.mult))
                    yield
                    dv(lambda e: e.tensor_reduce(out=dd, in_=tmp16, axis=AX.X, op=ALU.add))
                    yield
                    dv(lambda e: e.tensor_copy(out=idx2[:, bi:bi + 1], in_=dd), extra_w=[f"e_i2_{bi}"])
                    yield
                    for (ix, ik) in [(idx1, f"e_i1_{bi}"), (idx2, f"e_i2_{bi}")]:
                        P.dma("pool", lambda e: e.indirect_dma_start(out=io.xg, out_offset=bass.IndirectOffsetOnAxis(ap=ix[:, bi:bi + 1], axis=0),
                                                                      in_=u_[:], in_offset=None, bounds_check=K.breg, oob_is_err=False),
                              reads=[uk, ik], writes=["xg"])
            gens = [route(bi, cond, src, dst) for bi, (cond, src, dst) in enumerate(blocks)]
            active = []

            def step(g_):
                try:
                    next(g_)
                    return True
                except StopIteration:
                    if g_ in active:
                        active.remove(g_)
                    return False
            while gens or active:
                if gens and len(active) < 2:
                    if active:
                        for _ in range(4):
                            if not step(active[0]):
                                break
                    active.append(gens.pop(0))
                for g_ in list(active):
                    step(g_)
            if io.dbg is not None:
                P.dma("sp", lambda e: e.dma_start(out=io.dbg[l], in_=base[:]), reads=["e_base"], writes=["dbg"])
            P.op("dve", lambda e: e.tensor_copy(out=cnti[:], in_=base[:]), reads=["e_base"], writes=["e_cnti"])
            barrier(P)
        with ExitStack() as es3:
            wg = [(_T(nc, es3, f"e_wg{i}", [128, KD, 512], BF16), f"e_wg{i}") for i in range(2)]
            wu = [(_T(nc, es3, f"e_wu{i}", [128, KD, 512], BF16), f"e_wu{i}") for i in range(2)]
            wd = [(_T(nc, es3, f"e_wd{i}", [128, 4, D], BF16), f"e_wd{i}") for i in range(2)]
            xr = [(_T(nc, es3, f"e_xr{i}", [128, NJ, D], BF16), f"e_xr{i}") for i in range(2)]
            xT = _T(nc, es3, "e_xT", [128, KD, HCAP], BF16)
            aT = _T(nc, es3, "e_aT", [128, 4, HCAP], BF16)
            sg = [(_T(nc, es3, f"e_sg{i}", [128, HCAP], F32), f"e_sg{i}") for i in range(2)]
            yb = [(_T(nc, es3, f"e_yb{i}", [128, D], BF16), f"e_yb{i}") for i in range(2)]
            psT = [(_PS(nc, es3, f"e_qT{i}", [128, 8, 128], BF16), f"e_qT{i}") for i in range(2)]
            pg = [(_PS(nc, es3, f"e_pg{i}", [128, 512]), f"e_pg{i}") for i in range(6)]
            cregs = {"pe": es3.enter_context(nc.tensor.register(f"e_cntp{l}")),
                     "act": es3.enter_context(nc.scalar.register(f"e_cnta{l}")),
                     "dve": es3.enter_context(nc.vector.register(f"e_cntv{l}")),
                     "sp": es3.enter_context(nc.sync.register(f"e_cnts{l}"))}
            pgi = 0
            ybi = 0
            xri = 0
            for ex in range(16):
                g_, gk = wg[ex % 2]
                u_, uk = wu[ex % 2]
                d_, dk = wd[ex % 2]
                gv = io.moe_w_gate[l][ex].rearrange("(k p) n -> p k n", p=128)
                uv = io.moe_w_up[l][ex].rearrange("(k p) n -> p k n", p=128)
                dvw = io.moe_w_down[l][ex].rearrange("(k p) n -> p k n", p=128)
                for hq in range(2):
                    P.dma("pool", lambda e: e.dma_start(out=g_[:, hq * 8:(hq + 1) * 8, :], in_=gv[:, hq * 8:(hq + 1) * 8, :]), writes=[gk])
                    P.dma("pool", lambda e: e.dma_start(out=u_[:, hq * 8:(hq + 1) * 8, :], in_=uv[:, hq * 8:(hq + 1) * 8, :]), writes=[uk])
                for hq in range(2):
                    P.dma("pool", lambda e: e.dma_start(out=d_[:, hq * 2:(hq + 1) * 2, :], in_=dvw[:, hq * 2:(hq + 1) * 2, :]), writes=[dk])
                preds = {}
                for en in ["pe", "act", "dve", "sp"]:
                    P._deps(en, ["e_cnti"], [])
                    P.E[en].reg_load(cregs[en], cnti[0:1, ex:ex + 1])
                    preds[en] = P.E[en].snap(cregs[en]) > HCAP
                for hh in range(2):
                    x_, xk = xr[xri % 2]
                    xri += 1
                    r0 = ex * CAP + hh * HCAP

                    def peop(fn, reads, writes):
                        cop("pe", fn, reads, writes)

                    def cop(en, fn, reads, writes):
                        if hh == 0:
                            P.op(en, fn, reads=reads, writes=writes)
                        else:
                            P.op_if(en, preds[en], fn, reads=reads, writes=writes)
                    def cdma(fn, reads, writes):
                        if hh == 0:
                            P.dma("sp", fn, reads=reads, writes=writes)
                        else:
                            P.dma_if("sp", preds["sp"], fn, reads=reads, writes=writes)
                    cdma(lambda e: e.dma_start(out=x_[:], in_=io.xg[r0:r0 + HCAP, :].rearrange("(j p) n -> p j n", p=128)), ["xg"], [xk])
                    for j in range(NJ):
                        if hh == 1:
                            def tr2(e):
                                for k in range(16):
                                    i = e.transpose(out=psT[k // 8][0][:, k % 8, :], in_=x_[:, j, k * 128:(k + 1) * 128], identity=K.ident[:])
                                return i
                            peop(tr2, [xk], [psT[0][1], psT[1][1]])
                        for half in range(2):
                            pt, ptk = psT[half]

                            def tr(e):
                                for jj in range(8):
                                    k = half * 8 + jj
                                    i = e.transpose(out=pt[:, jj, :], in_=x_[:, j, k * 128:(k + 1) * 128], identity=K.ident[:])
                                return i
                            if hh == 0:
                                peop(tr, [xk], [ptk])
                            if half == 0:
                                cop("act", lambda e: e.copy(out=xT[:, 0:8, j * 128:(j + 1) * 128], in_=pt[:]), [ptk], ["e_xT"])
                            else:
                                cop("dve", lambda e: e.tensor_copy(out=xT[:, 8:16, j * 128:(j + 1) * 128], in_=pt[:]), [ptk], ["e_xT"])
                    for fc in range(4):
                        pG, pGk = pg[pgi % 6]
                        pU, pUk = pg[(pgi + 1) % 6]
                        pgi += 2

                        def mmg(e):
                            for k in range(KD):
                                i = e.matmul(pG[:, 0:HCAP], lhsT=g_[:, k, fc * 128:(fc + 1) * 128], rhs=xT[:, k, :], start=(k == 0), stop=(k == KD - 1))
                            return i

                        def mmu(e):
                            for k in range(KD):
                                i = e.matmul(pU[:, 0:HCAP], lhsT=u_[:, k, fc * 128:(fc + 1) * 128], rhs=xT[:, k, :], start=(k == 0), stop=(k == KD - 1))
                            return i
                        if hh == 0:
                            peop(mmg, [gk, "e_xT"], [pGk])
                            peop(mmu, [uk, "e_xT"], [pUk])
                        else:
                            peop(lambda e: (mmg(e), mmu(e))[1], [gk, uk, "e_xT"], [pGk, pUk])
                        s_, sk = sg[fc % 2]
                        cop("act", lambda e: e.activation(out=s_[:], in_=pG[:, 0:HCAP], func=AF.Silu), [pGk], [sk])
                        cop("dve", lambda e: e.tensor_tensor(out=aT[:, fc, :], in0=s_[:], in1=pU[:, 0:HCAP], op=ALU.mult), [sk, pUk], ["e_aT"])
                    for j in range(NJ):
                        y_, yk = yb[ybi % 2]
                        ybi += 1
                        if hh == 1:
                            bks = [pg[(pgi + cb_) % 6] for cb_ in range(4)]

                            def mmd4(e):
                                for cb_ in range(4):
                                    for fc in range(4):
                                        i = e.matmul(bks[cb_][0][:], lhsT=aT[:, fc, j * 128:(j + 1) * 128], rhs=d_[:, fc, cb_ * 512:(cb_ + 1) * 512],
                                                     start=(fc == 0), stop=(fc == 3))
                                return i
                            peop(mmd4, ["e_aT", dk], [b_[1] for b_ in bks])
                        for cb in range(4):
                            pY, pYk = pg[pgi % 6]
                            pgi += 1

                            def mmd(e):
                                for fc in range(4):
                                    i = e.matmul(pY[:], lhsT=aT[:, fc, j * 128:(j + 1) * 128], rhs=d_[:, fc, cb * 512:(cb + 1) * 512], start=(fc == 0), stop=(fc == 3))
                                return i
                            if hh == 0:
                                peop(mmd, ["e_aT", dk], [pYk])
                            if cb % 2 == 0:
                                cop("act", lambda e: e.copy(out=y_[:, cb * 512:(cb + 1) * 512], in_=pY[:]), [pYk], [yk])
                            else:
                                cop("dve", lambda e: e.tensor_copy(out=y_[:, cb * 512:(cb + 1) * 512], in_=pY[:]), [pYk], [yk])
                        cdma(lambda e: e.dma_start(out=io.yg[r0 + j * 128:r0 + (j + 1) * 128, :], in_=y_[:]), [yk], ["yg"])
            barrier(P)
        with ExitStack() as es4:
            for cond in sorted(set(b[0] for b in blocks)):
                Gs[cond] = load_bcast(P, nc, es4, f"e_G{cond}", io.modv[l, cond:cond + 1, 5 * D:6 * D])
            y1 = [(_T(nc, es4, f"e_y1{i}", [128, D], BF16), f"e_y1{i}") for i in range(2)]
            y2 = [(_T(nc, es4, f"e_y2{i}", [128, D], BF16), f"e_y2{i}") for i in range(2)]
            hx = [(_T(nc, es4, f"e_hx{i}", [128, D], F32), f"e_hx{i}") for i in range(2)]
            tt = [(_T(nc, es4, f"e_tt{i}", [128, D], F32), f"e_tt{i}") for i in range(2)]
            t1 = [(_T(nc, es4, f"e_tq{i}", [128, D], F32), f"e_tq{i}") for i in range(2)]
            for i in range(2):
                P.op("pool", lambda e: e.memset(y1[i][0][:], 0.0), writes=[y1[i][1]])
                P.op("pool", lambda e: e.memset(y2[i][0][:], 0.0), writes=[y2[i][1]])
            for bi, (cond, src, dst) in enumerate(blocks):
                a_, ak = y1[bi % 2]
                b_, bk = y2[bi % 2]
                h_, hk = hx[bi % 2]
                t_, tk = tt[bi % 2]
                q_, qk = t1[bi % 2]
                P.dma("pool", lambda e: e.indirect_dma_start(out=a_[:], out_offset=None, in_=io.yg, in_offset=bass.IndirectOffsetOnAxis(ap=idx1[:, bi:bi + 1], axis=0),
                                                              bounds_check=K.breg, oob_is_err=False), reads=["yg", f"e_i1_{bi}"], writes=[ak])
                P.dma("pool", lambda e: e.indirect_dma_start(out=b_[:], out_offset=None, in_=io.yg, in_offset=bass.IndirectOffsetOnAxis(ap=idx2[:, bi:bi + 1], axis=0),
                                                              bounds_check=K.breg, oob_is_err=False), reads=["yg", f"e_i2_{bi}"], writes=[bk])
                P.dma("sp", lambda e: e.dma_start(out=h_[:], in_=src), writes=[hk])
                P.op("act", lambda e: e.activation(out=q_[:], in_=a_[:], func=AF.Identity, scale=wts[:, 0, bi:bi + 1]), reads=[ak, "e_wts"], writes=[qk])
                P.op("dve", lambda e: e.scalar_tensor_tensor(out=t_[:], in0=b_[:], scalar=wts[:, 1, bi:bi + 1], in1=q_[:], op0=ALU.mult, op1=ALU.add),
                     reads=[bk, "e_wts", qk], writes=[tk])
                P.op("dve", lambda e: e.tensor_tensor(out=t_[:], in0=t_[:], in1=Gs[cond][:], op=ALU.mult), reads=[tk, f"e_G{cond}"], writes=[tk])
                P.op("dve", lambda e: e.tensor_tensor(out=t_[:], in0=t_[:], in1=h_[:], op=ALU.add), reads=[tk, hk], writes=[tk])
                P.dma("sp", lambda e: e.dma_start(out=dst, in_=t_[:]), reads=[tk], writes=["hdst"])
    barrier(P)


def phase_moe0(P, nc, K, io):
    phase_moe(P, nc, K, io, 0, [(NLAT, 0, io.h_mid, io.h_lat), (NCTX, 1, io.h_ctx, io.h_ctx)])


def phase_moe1(P, nc, K, io):
    phase_moe(P, nc, K, io, 1, [(NLAT, 0, io.h_mid, io.out)])


PHASE_FN.update({"l0b": phase_l0b, "op0": phase_op0, "op1": phase_op1, "moe0": phase_moe0, "moe1": phase_moe1})


def phase_l1a(P, nc, K, io):
    NT = NLAT + NCTX
    with ExitStack() as es:
        uT = (_T(nc, es, "q_uT", [128, KD, NT], BF16), "q_uT")
        with ExitStack() as es2:
            Al, Bl = make_AB(P, nc, es2, K, io, 1, 0, 0, "q_l")
            Ac, Bc = make_AB(P, nc, es2, K, io, 1, 0, 1, "q_c")
            xf = [(_T(nc, es2, f"q_xf{i}", [128, D], F32), f"q_xf{i}") for i in range(2)]
            ub = [(_T(nc, es2, f"q_ub{i}", [128, D], BF16), f"q_ub{i}") for i in range(2)]
            sq = (_T(nc, es2, "q_sq", [128, D], BF16), "q_sq")
            st = [(_T(nc, es2, f"q_st{i}", [128, 4], F32), f"q_st{i}") for i in range(2)]
            psT = [(_PS(nc, es2, f"q_pT{i}", [128, 8, 128], BF16), f"q_pT{i}") for i in range(2)]
            for b in range(NT // 128):
                if b < 16:
                    src, A, Bt = io.h_lat[b * 128:(b + 1) * 128, :], Al, Bl
                else:
                    src, A, Bt = io.h_ctx[(b - 16) * 128:(b - 15) * 128, :], Ac, Bc
                norm_block(P, K, src, A, Bt, xf[b % 2], ub[b % 2], sq, st[b % 2], uT=uT, uT_off=b * 128, psT=psT)
            barrier(P)
        wv = io.odd_w_qkv.rearrange("(k p) n -> p k n", p=128)
        wt = [(_T(nc, es, f"q_w{i}", [128, KD, 128], BF16), f"q_w{i}") for i in range(3)]
        cosT = _T(nc, es, "q_cos", [128, NLAT], F32)
        sinT = _T(nc, es, "q_sin", [128, NLAT], F32)
        rotT = _T(nc, es, "q_rot", [128, 128], BF16)
        gn = _T(nc, es, "q_gn", [128, 2], F32)
        P.dma("sp", lambda e: e.dma_start(out=cosT[:], in_=io.rope_cs[0]), writes=["q_cos"])
        P.dma("sp", lambda e: e.dma_start(out=sinT[:], in_=io.rope_cs[1]), writes=["q_sin"])
        P.dma("sp", lambda e: e.dma_start(out=rotT[:], in_=io.rotT), writes=["q_rot"])
        P.dma("sp", lambda e: e.dma_start(out=gn[:], in_=io.qk_norm), writes=["q_gn"])
        px = [(_PS(nc, es, f"q_px{i}", [128, 512]), f"q_px{i}") for i in range(3)]
        pss = [(_PS(nc, es, f"q_pss{i}", [128, 512]), f"q_pss{i}") for i in range(2)]
        pr = [(_PS(nc, es, f"q_pr{i}", [128, 512]), f"q_pr{i}") for i in range(2)]
        sqb = [(_T(nc, es, f"q_sqb{i}", [128, 512], BF16), f"q_sqb{i}") for i in range(3)]
        rs = [(_T(nc, es, f"q_rs{i}", [128, 512], F32), f"q_rs{i}") for i in range(2)]
        yb = [(_T(nc, es, f"q_yb{i}", [128, 512], BF16), f"q_yb{i}") for i in range(3)]
        t1 = [(_T(nc, es, f"q_t1{i}", [128, 512], F32), f"q_t1{i}") for i in range(2)]
        t2 = [(_T(nc, es, f"q_t2{i}", [128, 512], F32), f"q_t2{i}") for i in range(2)]
        ob = [(_T(nc, es, f"q_ob{i}", [128, 512], BF16), f"q_ob{i}") for i in range(2)]
        it = 0

        def run_slabs(slabs, pend=()):
            items = []
            for s in slabs:
                isq = s < 16
                tbl = [(0, 512), (512, 512), (1024, 512), (1536, 512)] + ([] if isq else [(2048, 256)])
                for (t0, tn) in tbl:
                    items.append((s, isq, t0, tn))
            n = len(items)
            wcur = {}

            def s1(i):
                s, isq, t0, tn = items[i]
                def ensure(sx):
                    if sx not in wcur:
                        w, wk = wt[sx % 3]
                        col = sx * 128
                        P.dma("pool", lambda e: e.dma_start(out=w[:], in_=wv[:, :, col:col + 128]), writes=[wk])
                        wcur[sx] = (w, wk)
                ensure(s)
                if s + 1 in slabs and (s + 1) not in wcur:
                    ensure(s + 1)
                    if pend:
                        pend.pop(0)()
                w, wk = wcur[s]
                p_, pk = px[i % 3]
                sq_, sqk = sqb[i % 3]

                def mm(e):
                    for k in range(KD):
                        ii = e.matmul(p_[:, 0:tn], lhsT=w[:, k, :], rhs=uT[0][:, k, t0:t0 + tn], start=(k == 0), stop=(k == KD - 1))
                    return ii
                P.op("pe", mm, reads=[wk, "q_uT"], writes=[pk])
                P.op("act", lambda e: e.activation(out=sq_[:, 0:tn], in_=p_[:, 0:tn], func=AF.Square), reads=[pk], writes=[sqk])

            def s2(i):
                s, isq, t0, tn = items[i]
                p_, pk = px[i % 3]
                sq_, sqk = sqb[i % 3]
                ps_, psk = pss[i % 2]
                r_, rk = rs[i % 2]
                y_, yk = yb[i % 3]
                P.op("pe", lambda e: e.matmul(ps_[:, 0:tn], lhsT=K.ones[:], rhs=sq_[:, 0:tn], start=True, stop=True), reads=[sqk, "k_ones"], writes=[psk])
                P.op("act", lambda e: e.activation(out=r_[:, 0:tn], in_=ps_[:, 0:tn], func=AF.Sqrt, scale=1.0 / 128, bias=K.epsb[:, 0:1]), reads=[psk], writes=[rk])
                P.op("dve", lambda e: e.reciprocal(out=r_[:, 0:tn], in_=r_[:, 0:tn]), reads=[rk], writes=[rk])
                gcol = gn[:, 0:1] if isq else gn[:, 1:2]
                P.op("dve", lambda e: e.scalar_tensor_tensor(out=y_[:, 0:tn], in0=p_[:, 0:tn], scalar=gcol, in1=r_[:, 0:tn], op0=ALU.mult, op1=ALU.mult),
                     reads=[pk, rk, "q_gn"], writes=[yk])

            def s3(i):
                s, isq, t0, tn = items[i]
                y_, yk = yb[i % 3]
                pr_, prk = pr[i % 2]
                a_, ak = t1[i % 2]
                b_, bk = t2[i % 2]
                o_, ok = ob[i % 2]
                if t0 < NLAT:
                    P.op("pe", lambda e: e.matmul(pr_[:, 0:tn], lhsT=rotT[:], rhs=y_[:, 0:tn], start=True, stop=True), reads=[yk, "q_rot"], writes=[prk])
                    P.op("pool", lambda e: e.tensor_tensor(out=a_[:, 0:tn], in0=y_[:, 0:tn], in1=cosT[:, t0:t0 + tn], op=ALU.mult), reads=[yk, "q_cos"], writes=[ak])
                    P.op("dve", lambda e: e.tensor_tensor(out=b_[:, 0:tn], in0=pr_[:, 0:tn], in1=sinT[:, t0:t0 + tn], op=ALU.mult), reads=[prk, "q_sin"], writes=[bk])
                    P.op("pool", lambda e: e.tensor_tensor(out=o_[:, 0:tn], in0=a_[:, 0:tn], in1=b_[:, 0:tn], op=ALU.add), reads=[ak, bk], writes=[ok])
                    dst = io.qT[s, :, t0:t0 + tn] if isq else io.kT_own[s - 16, :, t0:t0 + tn]
                    P.dma("sp", lambda e: e.dma_start(out=dst, in_=o_[:, 0:tn]), reads=[ok], writes=["q_out" if isq else "k_out"])
                else:
                    P.dma("sp", lambda e: e.dma_start(out=io.kT_ctx[s - 16, :, 0:tn], in_=y_[:, 0:tn]), reads=[yk], writes=["k_out"])
            for t in range(n + 2):
                if t < n:
                    s1(t)
                if 0 <= t - 1 < n:
                    s2(t - 1)
                if 0 <= t - 2 < n:
                    s3(t - 2)

        run_slabs(list(range(16, 32)))
        wvt = [(_T(nc, es, f"q_wv{i}", [128, KD, 512], BF16), f"q_wv{i}") for i in range(4)]
        for cb in range(4):
            w, wk = wvt[cb]
            for hq in range(2):
                P.dma("pool", lambda e: e.dma_start(out=w[:, hq * 8:(hq + 1) * 8, :], in_=wv[:, hq * 8:(hq + 1) * 8, 4096 + cb * 512:4096 + (cb + 1) * 512]), writes=[wk])
        pend = []
        for cb in range(4):
            w, wk = wvt[cb]
            for _ in range(2):
                if pend:
                    pend.pop(0)()
            for b in range(NT // 128):
                i2 = it % 2
                it += 1
                p_, pk = px[i2]
                o_, ok = ob[i2]

                def mmv(e):
                    for k in range(KD):
                        i = e.matmul(p_[:], lhsT=uT[0][:, k, b * 128:(b + 1) * 128], rhs=w[:, k, :], start=(k == 0), stop=(k == KD - 1))
                    return i
                P.op("pe", mmv, reads=[wk, "q_uT"], writes=[pk])
                if b % 2 == 0:
                    P.op("act", lambda e: e.copy(out=o_[:], in_=p_[:]), reads=[pk], writes=[ok])
                else:
                    P.op("dve", lambda e: e.tensor_copy(out=o_[:], in_=p_[:]), reads=[pk], writes=[ok])
                if b < 16:
                    dst = io.v_own[cb, b * 128:(b + 1) * 128, :]
                else:
                    dst = io.v_ctx[(b - 16) * 128:(b - 15) * 128, cb * 512:(cb + 1) * 512]
                P.dma("sp", lambda e: e.dma_start(out=dst, in_=o_[:]), reads=[ok], writes=["v_out"])
        run_slabs(list(range(16)), pend)
        while pend:
            pend.pop(0)()
    barrier(P)


def phase_attn(P, nc, K, io):
    NK = 2 * NLAT + NCTX
    NKC = NK // 128
    with ExitStack() as es:
        lv = _T(nc, es, "t_lv", [1, 4, 128], F32)
        l1 = _T(nc, es, "t_l1", [1, 8], F32)
        onesf = _T(nc, es, "t_onesf", [1, 128], F32)
        nlam = _T(nc, es, "t_nlam", [128, 1], F32)
        sw = _T(nc, es, "t_sw", [128, 2], F32)
        P.dma("sp", lambda e: e.dma_start(out=lv[:], in_=io.lam_vecs), writes=["t_lv"])
        P.dma("sp", lambda e: e.dma_start(out=sw[:], in_=io.subln_wT), writes=["t_sw"])
        P.op("pool", lambda e: e.memset(onesf[:], 1.0), writes=["t_onesf"])
        P.op("dve", lambda e: e.tensor_tensor(out=lv[:, 0, :], in0=lv[:, 0, :], in1=lv[:, 1, :], op=ALU.mult), reads=["t_lv"], writes=["t_lv"])
        P.op("dve", lambda e: e.tensor_tensor(out=lv[:, 2, :], in0=lv[:, 2, :], in1=lv[:, 3, :], op=ALU.mult), reads=["t_lv"], writes=["t_lv"])
        P.op("dve", lambda e: e.tensor_reduce(out=l1[:, 0:1], in_=lv[:, 0, :], axis=AX.X, op=ALU.add), reads=["t_lv"], writes=["t_l1"])
        P.op("dve", lambda e: e.tensor_reduce(out=l1[:, 1:2], in_=lv[:, 2, :], axis=AX.X, op=ALU.add), reads=["t_lv"], writes=["t_l1"])
        P.op("act", lambda e: e.activation(out=l1[:, 2:4], in_=l1[:, 0:2], func=AF.Exp), reads=["t_l1"], writes=["t_l1"])
        P.op("dve", lambda e: e.tensor_tensor(out=l1[:, 4:5], in0=l1[:, 3:4], in1=l1[:, 2:3], op=ALU.subtract), reads=["t_l1"], writes=["t_l1"])
        P.op("dve", lambda e: e.tensor_scalar(out=l1[:, 4:5], in0=l1[:, 4:5], scalar1=-LAM_INIT1, scalar2=None, op0=ALU.add), reads=["t_l1"], writes=["t_l1"])
        P.op("dve", lambda e: e.tensor_scalar(out=sw[:], in0=sw[:], scalar1=1.0 - LAM_INIT1, scalar2=None, op0=ALU.mult), reads=["t_sw"], writes=["t_sw"])
        kTb = [[_T(nc, es, f"t_kT{b}{c}", [128, NK], BF16) for c in range(2)] for b in range(2)]
        vtb = [_T(nc, es, f"t_v{b}", [128, NKC, 512], BF16) for b in range(2)]
        if io.mode == "ALL":
            ixk = _T(nc, es, "t_ixk", [128, 32], I32)
            ixv = _T(nc, es, "t_ixv", [128, 128], I32)
            P.dma("sp", lambda e: e.dma_start(out=ixk[:], in_=io.idx_k), writes=["t_ixk"])
            P.dma("sp", lambda e: e.dma_start(out=ixv[:], in_=io.idx_v), writes=["t_ixv"])
        qt = [(_T(nc, es, f"t_q{i}", [128, 2, 512], BF16), f"t_q{i}") for i in range(2)]
        pT = [(_T(nc, es, f"t_pT{i}", [128, 512], BF16), f"t_pT{i}") for i in range(3)]
        pS = [(_PS(nc, es, f"t_pS{i}", [128, 512]), f"t_pS{i}") for i in range(2)]
        pO = [[(_PS(nc, es, f"t_pO{c}{j}", [128, 512]), f"t_pO{c}{j}") for j in range(3)] for c in range(2)]
        rc = [_T(nc, es, f"t_rc{c}", [128, 512], F32) for c in range(2)]
        oh = [(_T(nc, es, f"t_oh{i}", [128, 512], F32), f"t_oh{i}") for i in range(2)]
        o2 = [(_T(nc, es, f"t_o2{i}", [128, 512], F32), f"t_o2{i}") for i in range(2)]
        osq = [(_T(nc, es, f"t_osq{i}", [128, 512], BF16), f"t_osq{i}") for i in range(2)]
        rsd = _T(nc, es, "t_rsd", [128, 512], F32)
        oo = [(_T(nc, es, f"t_oo{i}", [128, 512], BF16), f"t_oo{i}") for i in range(2)]
        P.op("pe", lambda e: e.matmul(pS[0][0][:, 0:1], lhsT=onesf[:], rhs=l1[:, 4:5], start=True, stop=True), reads=["t_l1", "t_onesf"], writes=["t_pS0"])
        P.op("dve", lambda e: e.tensor_copy(out=nlam[:], in_=pS[0][0][:, 0:1]), reads=["t_pS0"], writes=["t_nlam"])
        scale = 128 ** -0.5
        qi = 0
        pi = 0
        si = 0
        if io.mode == "ALL":
            kth = _gather_thunks(P, io.kT_own.rearrange("s p t -> (s p) t"), io.k4, 2048, 256, "k_out", "k4")
            vth = _gather_thunks(P, io.v_own.rearrange("g t n -> (g t) n"), io.v4, 8192, 1024, "v_out", "v4")
        else:
            kth, vth = [], []

        def issue_pair_colls(hp):
            if kth and hp < 4:
                for q_ in (2 * hp, 2 * hp + 1):
                    kth[q_]()
                    vth[q_]()

        def load_head(h):
            hp = h // 2
            vt = vtb[hp % 2]
            vk = f"t_v{hp % 2}"
            kT = kTb[h % 2]
            if h % 2 == 0:
                P.dma("sp", lambda e: e.dma_start(out=vt[:, 0:2, :], in_=io.v_ctx[:, hp * 512:(hp + 1) * 512].rearrange("(c p) n -> p c n", p=128)), writes=[vk])
                if io.mode == "ALL":
                    for c32 in range(32):
                        P.dma("pool", lambda e: e.indirect_dma_start(out=vt[:, 2 + c32, :], out_offset=None, in_=io.v4,
                                                                      in_offset=bass.IndirectOffsetOnAxis(ap=ixv[:, hp * 32 + c32:hp * 32 + c32 + 1], axis=0),
                                                                      bounds_check=K.breg_big, oob_is_err=False), reads=[f"v4_{2 * hp + (c32 % 16) // 8}", "t_ixv"], writes=[vk])
                else:
                    for q4 in range(4):
                        P.dma("sp", lambda e: e.dma_start(out=vt[:, 2 + q4 * 8:2 + (q4 + 1) * 8, :],
                                                          in_=io.v_full[hp, q4 * 1024:(q4 + 1) * 1024, :].rearrange("(c p) n -> p c n", p=128)), writes=[vk])
            for c in range(2):
                s = 2 * h + c
                kk = f"t_kT{h % 2}{c}"
                P.dma("sp", lambda e: e.dma_start(out=kT[c][:, 0:NCTX], in_=io.kT_ctx[s]), writes=[kk])
                if io.mode == "ALL":
                    for hf in range(2):
                        P.dma("pool", lambda e: e.indirect_dma_start(out=kT[c][:, NCTX + hf * NLAT:NCTX + (hf + 1) * NLAT], out_offset=None, in_=io.k4,
                                                                      in_offset=bass.IndirectOffsetOnAxis(ap=ixk[:, s * 2 + hf:s * 2 + hf + 1], axis=0),
                                                                      bounds_check=K.breg_big, oob_is_err=False), reads=[f"k4_{h}", "t_ixk"], writes=[kk])
                else:
                    P.dma("sp", lambda e: e.dma_start(out=kT[c][:, NCTX:NK], in_=io.kT_full[s]), writes=[kk])

        issue_pair_colls(0)
        load_head(0)
        for h in range(8):
            hp = h // 2
            vo = (h % 2) * 256
            vt = vtb[hp % 2]
            vk = f"t_v{hp % 2}"
            kT = kTb[h % 2]
            kks = [f"t_kT{h % 2}{c}" for c in range(2)]
            for qb in range(4):
                q_, qk = qt[qi % 2]
                qi += 1
                P.dma("sp", lambda e: e.dma_start(out=q_[:], in_=io.qT[2 * h:2 * h + 2, :, qb * 512:(qb + 1) * 512].rearrange("c p t -> p c t")), writes=[qk])
                steps = [(c, kc) for c in range(2) for kc in range(NKC)]
                bufs = []
                for _ in steps:
                    bufs.append((pS[si % 2], pT[pi % 3]))
                    si += 1
                    pi += 1

                def emit_S(i):
                    c, kc = steps[i]
                    (s_, sk), (p_, pk) = bufs[i]
                    P.op("pe", lambda e: e.matmul(s_[:], lhsT=kT[c][:, kc * 128:(kc + 1) * 128], rhs=q_[:, c, :], start=True, stop=True),
                         reads=[kks[c], qk], writes=[sk])
                    P.op("act", lambda e: e.activation(out=p_[:], in_=s_[:], func=AF.Exp, scale=scale), reads=[sk], writes=[pk])

                def emit_PV(i):
                    c, kc = steps[i]
                    (s_, sk), (p_, pk) = bufs[i]

                    def mmo(e):
                        e.matmul(pO[c][0][0][:], lhsT=vt[:, kc, vo:vo + 128], rhs=p_[:], start=(kc == 0), stop=(kc == NKC - 1))
                        e.matmul(pO[c][1][0][:], lhsT=vt[:, kc, vo + 128:vo + 256], rhs=p_[:], start=(kc == 0), stop=(kc == NKC - 1))
                        return e.matmul(pO[c][2][0][:], lhsT=K.ones[:], rhs=p_[:], start=(kc == 0), stop=(kc == NKC - 1))
                    P.op("pe", mmo, reads=[pk, vk, "k_ones"], writes=[pO[c][0][1], pO[c][1][1], pO[c][2][1]])
                if qb == 1 and h + 1 < 8:
                    load_head(h + 1)
                    if (h + 1) % 2 == 1:
                        issue_pair_colls((h + 1) // 2 + 1)
                emit_S(0)
                for i in range(len(steps)):
                    if i + 1 < len(steps):
                        emit_S(i + 1)
                    emit_PV(i)
                P.op("dve", lambda e: e.reciprocal(out=rc[0][:], in_=pO[0][2][0][:]), reads=[pO[0][2][1]], writes=["t_rc0"])
                P.op("dve", lambda e: e.reciprocal(out=rc[1][:], in_=pO[1][2][0][:]), reads=[pO[1][2][1]], writes=["t_rc1"])
                P.op("dve", lambda e: e.tensor_scalar(out=rc[1][:], in0=rc[1][:], scalar1=nlam[:, 0:1], scalar2=None, op0=ALU.mult), reads=["t_rc1", "t_nlam"], writes=["t_rc1"])
                for hf in range(2):
                    a_, ak = oh[hf]
                    b_, bk = o2[hf]
                    q2_, q2k = osq[hf]
                    P.op("dve", lambda e: e.tensor_tensor(out=a_[:], in0=pO[0][hf][0][:], in1=rc[0][:], op=ALU.mult), reads=[pO[0][hf][1], "t_rc0"], writes=[ak])
                    P.op("dve", lambda e: e.tensor_tensor(out=b_[:], in0=pO[1][hf][0][:], in1=rc[1][:], op=ALU.mult), reads=[pO[1][hf][1], "t_rc1"], writes=[bk])
                    P.op("dve", lambda e: e.tensor_tensor(out=a_[:], in0=a_[:], in1=b_[:], op=ALU.add), reads=[ak, bk], writes=[ak])
                    P.op("act", lambda e: e.activation(out=q2_[:], in_=a_[:], func=AF.Square), reads=[ak], writes=[q2k])
                s_, sk = pS[si % 2]
                si += 1

                def mms(e):
                    e.matmul(s_[:], lhsT=K.ones[:], rhs=osq[0][0][:], start=True, stop=False)
                    return e.matmul(s_[:], lhsT=K.ones[:], rhs=osq[1][0][:], start=False, stop=True)
                P.op("pe", mms, reads=[osq[0][1], osq[1][1], "k_ones"], writes=[sk])
                P.op("act", lambda e: e.activation(out=rsd[:], in_=s_[:], func=AF.Sqrt, scale=1.0 / 256, bias=K.epsb[:, 0:1]), reads=[sk], writes=["t_rsd"])
                P.op("dve", lambda e: e.reciprocal(out=rsd[:], in_=rsd[:]), reads=["t_rsd"], writes=["t_rsd"])
                for hf in range(2):
                    o_, ok = oo[hf]
                    P.op("dve", lambda e: e.scalar_tensor_tensor(out=o_[:], in0=oh[hf][0][:], scalar=sw[:, hf:hf + 1], in1=rsd[:], op0=ALU.mult, op1=ALU.mult),
                         reads=[oh[hf][1], "t_sw", "t_rsd"], writes=[ok])
                    P.dma("sp", lambda e: e.dma_start(out=io.oT[2 * h + hf, :, qb * 512:(qb + 1) * 512], in_=o_[:]), reads=[ok], writes=["oT"])
    barrier(P)


RG4 = [[0, 1, 2, 3], [4, 5, 6, 7]]


def _gather_thunks(P, src2d, dst2d, rows, chunk, rkey, wkey):
    th = []
    for q in range(rows // chunk):
        def f(q=q):
            P.coll(lambda e: e.collective_compute("AllGather", ALU.bypass, replica_groups=RG4,
                                                  ins=[src2d[q * chunk:(q + 1) * chunk, :].opt()],
                                                  outs=[dst2d[q * 4 * chunk:(q + 1) * 4 * chunk, :].opt()]),
                   reads=[rkey], writes=[f"{wkey}_{q}"])
        th.append(f)
    return th


def _gather_chunks(P, src2d, dst2d, rows, chunk, rkey, wkey):
    for f in _gather_thunks(P, src2d, dst2d, rows, chunk, rkey, wkey):
        f()


def phase_xab(P, nc, K, io):
    pass


def phase_xkv(P, nc, K, io):
    pass


PHASE_FN.update({"l1a": phase_l1a, "attn": phase_attn, "xab": phase_xab, "xkv": phase_xkv})


_BF = None
_CACHE = {}
FUSED = True
LAST_DBG = None


def _bf():
    global _BF
    if _BF is None:
        import ml_dtypes
        _BF = ml_dtypes.bfloat16
    return _BF


def _consts(hf):
    BF = _bf()
    c = {}
    c["ident"] = np.eye(128, dtype=np.float32).astype(BF)
    k = np.arange(256)
    ang = 2 * np.pi * np.outer(k, k) / 256
    cs = np.concatenate([np.cos(ang), np.sin(ang)], 1) / 16.0
    c["cs256"] = np.ascontiguousarray(cs.reshape(2, 128, 512).transpose(1, 0, 2)).astype(BF)
    t = np.arange(4096, dtype=np.int64)[:, None]
    n = (hf * 2048 + np.arange(2048, dtype=np.int64))[None, :]
    a2 = 2 * np.pi * ((t * n) % 4096).astype(np.float64) / 4096
    c["dft_c"] = (np.cos(a2) / 64.0).astype(np.float32).astype(BF)
    c["dft_s"] = (-np.sin(a2) / 64.0).astype(np.float32).astype(BF)
    d2 = np.stack([np.cos(ang), -np.sin(ang)], 1) / 16.0
    c["dft256"] = np.ascontiguousarray(d2.reshape(2, 128, 2, 256).transpose(1, 0, 2, 3)).astype(np.float32).astype(BF)
    tok = hf * 2048 + np.arange(2048)
    row, col = (tok // 64).astype(np.float32), (tok % 64).astype(np.float32)
    invf = (10000.0 ** (-np.arange(32, dtype=np.float32) / 32)).astype(np.float32)
    i = np.arange(128)
    pos = np.where((i // 64)[:, None] == 0, row[None, :], col[None, :]).astype(np.float32)
    angr = pos * invf[i % 32][:, None]
    c["rope_cs"] = np.stack([np.cos(angr), np.sin(angr)], 0).astype(np.float32)
    R = np.zeros((128, 128), np.float32)
    for ii in range(128):
        if ii % 64 < 32:
            R[ii, ii + 32] = -1.0
        else:
            R[ii, ii - 32] = 1.0
    c["rotT"] = np.ascontiguousarray(R.T).astype(BF)
    c["tri"] = np.triu(np.ones((128, 128), np.float32), k=1).astype(BF)
    return c


def _core_inputs(r, inp):
    b, hf = r // 2, r % 2
    m = {}
    m["x"] = np.ascontiguousarray(inp["x"][b, hf * 2048:(hf + 1) * 2048])
    m["ctx"] = np.ascontiguousarray(inp["ctx"][b])
    ht = 2048 if hf == 0 else 2047
    xh = np.zeros((128, 2048), np.float32)
    xh[0] = inp["x"][b, ht]
    m["xh"] = xh
    cond = np.stack([inp["c"][b], inp["c_ctx"]], 0)
    m["condT"] = np.ascontiguousarray(cond.reshape(2, 16, 128).transpose(2, 1, 0))
    fl = np.zeros((128, 2), np.float32)
    fl[:, 0] = 1.0 if hf == 1 else 0.0
    fl[:, 1] = 1.0 if hf == 0 else 0.0
    m["flags"] = fl
    for k in ["w_mod", "b_mod", "norm1_w", "norm2_w"]:
        m[k] = inp[k]
    m["even_w_in"] = inp["even_w_in"][0]
    m["conv_wT"] = np.ascontiguousarray(inp["even_conv_w"][0].reshape(3, 8, 128).transpose(2, 1, 0))
    m["even_w_out"] = inp["even_w_out"][0]
    m["odd_w_qkv"] = inp["odd_w_qkv"][0]
    m["qk_norm"] = np.ascontiguousarray(np.stack([inp["odd_q_norm"][0], inp["odd_k_norm"][0]], 1))
    m["lam_vecs"] = np.ascontiguousarray(np.stack([inp["odd_lambda_q1"][0], inp["odd_lambda_k1"][0], inp["odd_lambda_q2"][0], inp["odd_lambda_k2"][0]], 0)[None])
    m["subln_wT"] = np.ascontiguousarray(inp["odd_subln_w"][0].reshape(2, 128).T)
    m["odd_w_out"] = inp["odd_w_out"][0]
    for l in range(2):
        m[f"moe_wr{l}"] = np.ascontiguousarray(np.concatenate([inp["moe_w_group"][l], inp["moe_w_expert"][l]], 1))
        m[f"moe_w_gate{l}"] = inp["moe_w_gate"][l]
        m[f"moe_w_up{l}"] = inp["moe_w_up"][l]
        m[f"moe_w_down{l}"] = inp["moe_w_down"][l]
    m["moe_br"] = np.ascontiguousarray(np.concatenate([inp["moe_b_group"], inp["moe_b_expert"]], 1))
    m.update(_consts(hf))
    pb = (r % 4) // 2
    p = np.arange(128, dtype=np.int64)[:, None]
    def grow(f, rank, chunk):
        return (f // chunk) * (4 * chunk) + rank * chunk + (f % chunk)
    cols = []
    for gp in range(2):
        for c in range(32):
            cols.append(grow(gp * 2048 + (c % 16) * 128 + p, 2 * pb + c // 16, 512))
    m["idx_ab"] = np.concatenate(cols, 1).astype(np.int32)
    cols = []
    for s_ in range(16):
        for hs in range(2):
            cols.append(grow(s_ * 128 + p, 2 * pb + hs, 256))
    m["idx_k"] = np.concatenate(cols, 1).astype(np.int32)
    cols = []
    for hp in range(4):
        for c in range(32):
            cols.append(grow(hp * 2048 + (c % 16) * 128 + p, 2 * pb + c // 16, 1024))
    m["idx_v"] = np.concatenate(cols, 1).astype(np.int32)
    return m


def _get(mode):
    if mode not in _CACHE:
        _CACHE[mode] = build(mode)
    return _CACHE[mode]


def _launch(mode, maps):
    nc, io = _get(mode)
    res = run_bass_kernel_spmd(nc, [{k: m[k] for k in io.ext_in} for m in maps], core_ids=list(range(len(maps))))
    return [{k: np.asarray(r[k]) for k in io.ext_out} for r in res.results]


def kernel(**inputs):
    inp = {k: np.asarray(v) for k, v in inputs.items()}
    maps = [_core_inputs(r, inp) for r in range(8)]
    if FUSED:
        rr = _launch("ALL", maps)
        global LAST_DBG
        LAST_DBG = [r["dbg"] for r in rr]
        out = np.zeros((4, 4096, 2048), np.float32)
        for r in range(8):
            out[r // 2, (r % 2) * 2048:(r % 2 + 1) * 2048] = rr[r]["out"]
        return out
    ra = _launch("A", maps)
    for r in range(8):
        p0, p1 = (r // 2) * 2, (r // 2) * 2 + 1
        maps[r]["modv"] = ra[r]["modv"]
        maps[r]["ycT"] = ra[r]["ycT"]
        maps[r]["ab_ctx"] = ra[r]["ab_ctx"]
        maps[r]["ab_full"] = np.concatenate([ra[p0]["ab_own"], ra[p1]["ab_own"]], 1)
    rb = _launch("B", maps)
    for r in range(8):
        p0, p1 = (r // 2) * 2, (r // 2) * 2 + 1
        for k in ["h_lat", "qT", "kT_ctx", "v_ctx"]:
            maps[r][k] = rb[r][k]
        maps[r]["kT_full"] = np.concatenate([rb[p0]["kT_own"], rb[p1]["kT_own"]], 2)
        maps[r]["v_full"] = np.concatenate([rb[p0]["v_own"], rb[p1]["v_own"]], 1)
    rc = _launch("C", maps)
    out = np.zeros((4, 4096, 2048), np.float32)
    for r in range(8):
        out[r // 2, (r % 2) * 2048:(r % 2 + 1) * 2048] = rc[r]["out"]
    return out
```
